# Optimizing a Trainium2 kernel written in Bass

```python
import jax, jax.numpy as jnp
from jax import lax
import numpy as np

D_MODEL = 1024
BATCH = 4
SEQ = 8192
DEPTH = 1

EPS = 1e-6
NEG_INF = -1e30
SGU_CHUNK = 128
SGU_GROUPS = 8
SGU_GROUP_DIM = 128
SGU_WIDTH = SGU_GROUPS * SGU_GROUP_DIM
ATT_HEADS = 8
ATT_HEAD_DIM = 64
ATT_PATTERNS = ((128, 1), (512, 4), (2048, 16))
ATT_GROUPS = len(ATT_PATTERNS)
ATT_Q_WIDTH = ATT_GROUPS * ATT_HEADS * ATT_HEAD_DIM
ATT_KV_WIDTH = ATT_HEADS * ATT_HEAD_DIM
N_BRANCHES = 2
IN_SPLITS = (SGU_WIDTH, SGU_WIDTH, ATT_Q_WIDTH, ATT_KV_WIDTH, ATT_KV_WIDTH, D_MODEL, D_MODEL)
IN_WIDTH = sum(IN_SPLITS)
PEER_HEADS = 8
PEER_N_KEYS = 128
PEER_N_EXPERTS = PEER_N_KEYS * PEER_N_KEYS
PEER_KEY_DIM = 256
PEER_HALF = PEER_KEY_DIM // 2
PEER_TOPK = 16
PEER_TOKEN_BLOCK = 128

kernel_name = 'hybrid_sgu_dilated_attn_peer_block'


def rmsnorm(x, g):
    xf = x.astype(jnp.float32)
    y = xf * lax.rsqrt(jnp.mean(xf * xf, axis=-1, keepdims=True) + EPS) * g.astype(jnp.float32)
    return y.astype(x.dtype)


def alibi_slopes(n_heads):
    return 2.0 ** (-8.0 * jnp.arange(1, n_heads + 1, dtype=jnp.float32) / n_heads)


def spatial_gating_mixer(z, norm_g, w_s, b_s):
    B, S, _ = z.shape
    u, v = jnp.split(z, 2, axis=-1)
    v = rmsnorm(v, norm_g).reshape(B, S // SGU_CHUNK, SGU_CHUNK, SGU_GROUPS, SGU_GROUP_DIM)
    v = jnp.einsum('gts,bnsgc->bntgc', w_s, v) + b_s.T[None, None, :, :, None]
    return u * v.reshape(B, S, SGU_WIDTH)


def dilated_band_attention(q, k, v, dilation, n_side, slopes):
    B, S, H, E = q.shape
    span = dilation * n_side
    Sp = -(-S // span) * span
    pad = [(0, 0), (0, Sp - S), (0, 0), (0, 0)]
    q, k, v = (jnp.pad(t.astype(jnp.float32), pad) for t in (q, k, v))
    M = Sp // dilation
    nb = M // n_side
    qb = q.reshape(B, nb, n_side, dilation, H, E)

    def windows(t):
        t = t.reshape(B, M, dilation, H, E)
        t = jnp.pad(t, [(0, 0), (n_side, n_side), (0, 0), (0, 0), (0, 0)])
        t = t.reshape(B, nb + 2, n_side, dilation, H, E)
        return jnp.concatenate([t[:, :-2], t[:, 1:-1], t[:, 2:]], axis=2)

    kw, vw = windows(k), windows(v)
    rel = jnp.arange(3 * n_side)[None, :] - n_side - jnp.arange(n_side)[:, None]
    band = jnp.abs(rel) <= n_side
    m_k = jnp.arange(nb)[:, None, None] * n_side + jnp.arange(3 * n_side)[None, None, :] - n_side
    pos_k = m_k * dilation + jnp.arange(dilation)[None, :, None]
    key_ok = (m_k >= 0) & (pos_k < S)
    mask = band[None, None] & key_ok[:, :, None, :]
    bias = -slopes[:, None, None] * (jnp.abs(rel) * dilation).astype(jnp.float32)[None]
    s = jnp.einsum('bnidhe,bnjdhe->bndhij', qb, kw) * (E ** -0.5) + bias
    s = jnp.where(mask[None, :, :, None], s, NEG_INF)
    lse = jax.nn.logsumexp(s, axis=-1)
    p = jnp.exp(s - lse[..., None])
    o = jnp.einsum('bndhij,bnjdhe->bnidhe', p, vw).reshape(B, Sp, H, E)[:, :S]
    lse = lse.transpose(0, 1, 4, 2, 3).reshape(B, Sp, H)[:, :S]
    return o, lse


def dilated_attention_mixer(q_all, k, v):
    B, S = q_all.shape[:2]
    slopes = alibi_slopes(ATT_HEADS)
    outs, lses = [], []
    for g, (window, dilation) in enumerate(ATT_PATTERNS):
        o, l = dilated_band_attention(q_all[:, :, g], k, v, dilation, window // (2 * dilation), slopes)
        outs.append(o)
        lses.append(l)
    wts = jax.nn.softmax(jnp.stack(lses, axis=0), axis=0)
    o = jnp.einsum('gbsh,gbshe->bshe', wts, jnp.stack(outs, axis=0))
    return o.reshape(B, S, ATT_KV_WIDTH)


def peer_ffn(h, w_q, sub_keys, u_tab, v_tab):
    B, S, D = h.shape
    q = (h @ w_q).reshape(B, S, PEER_HEADS, 2, PEER_HALF).astype(jnp.float32)
    s = jnp.einsum('bshpk,hpnk->bshpn', q, sub_keys.astype(jnp.float32))
    top_s, top_i = lax.top_k(s, PEER_TOPK)
    cand_s = top_s[..., 0, :, None] + top_s[..., 1, None, :]
    cand_i = top_i[..., 0, :, None] * PEER_N_KEYS + top_i[..., 1, None, :]
    best_s, best_j = lax.top_k(cand_s.reshape(B, S, PEER_HEADS, PEER_TOPK * PEER_TOPK), PEER_TOPK)
    expert = jnp.take_along_axis(cand_i.reshape(B, S, PEER_HEADS, PEER_TOPK * PEER_TOPK), best_j, axis=-1)
    gate = jax.nn.softmax(best_s, axis=-1)
    n_blocks = (B * S) // PEER_TOKEN_BLOCK
    xs = (h.reshape(n_blocks, PEER_TOKEN_BLOCK, D),
          expert.reshape(n_blocks, PEER_TOKEN_BLOCK, PEER_HEADS * PEER_TOPK),
          gate.reshape(n_blocks, PEER_TOKEN_BLOCK, PEER_HEADS * PEER_TOPK))

    def block(args):
        hb, eb, gb = args
        a = jnp.einsum('ckd,cd->ck', u_tab[eb], hb)
        act = jax.nn.gelu(a.astype(jnp.float32), approximate=False) * gb
        return jnp.einsum('ck,ckd->cd', act.astype(hb.dtype), v_tab[eb])

    return lax.map(block, xs).reshape(B, S, D)


def setup_inputs(seed: int = 0) -> dict:
    key = jax.random.key(seed)
    ks = jax.random.split(key, 16)
    f32 = jnp.float32
    L, D = DEPTH, D_MODEL
    nrm = lambda k, shape, scale: jax.random.normal(k, shape, f32) * scale
    return {
        'x': jax.random.normal(ks[0], (BATCH, SEQ, D), f32),
        'norm_mix_g': 1.0 + nrm(ks[1], (L, D), 0.02),
        'w_in': nrm(ks[2], (L, D, IN_WIDTH), D ** -0.5),
        'sgu_norm_g': 1.0 + nrm(ks[3], (L, SGU_WIDTH), 0.02),
        'sgu_w': nrm(ks[4], (L, SGU_GROUPS, SGU_CHUNK, SGU_CHUNK), SGU_CHUNK ** -0.5),
        'sgu_b': 1.0 + nrm(ks[5], (L, SGU_GROUPS, SGU_CHUNK), 0.02),
        'w_branch_a': nrm(ks[6], (L, SGU_WIDTH, D), SGU_WIDTH ** -0.5),
        'w_branch_b': nrm(ks[7], (L, ATT_KV_WIDTH, D), ATT_KV_WIDTH ** -0.5),
        'w_out': nrm(ks[8], (L, D, D), D ** -0.5),
        'norm_ffn_g': 1.0 + nrm(ks[9], (L, D), 0.02),
        'peer_wq': nrm(ks[10], (L, D, PEER_HEADS * PEER_KEY_DIM), D ** -0.5),
        'peer_subkeys': nrm(ks[11], (L, PEER_HEADS, 2, PEER_N_KEYS, PEER_HALF), PEER_HALF ** -0.5),
        'peer_u': nrm(ks[12], (L, PEER_N_EXPERTS, D), D ** -0.5),
        'peer_v': nrm(ks[13], (L, PEER_N_EXPERTS, D), PEER_HEADS ** -0.5),
        'norm_final_g': 1.0 + nrm(ks[14], (D,), 0.02),
    }


def reference(x, norm_mix_g, w_in, sgu_norm_g, sgu_w, sgu_b, w_branch_a, w_branch_b, w_out,
              norm_ffn_g, peer_wq, peer_subkeys, peer_u, peer_v, norm_final_g):
    B, S, _ = x.shape
    split_at = np.cumsum(IN_SPLITS)[:-1].tolist()
    for l in range(DEPTH):
        h = rmsnorm(x, norm_mix_g[l])
        proj = h @ w_in[l]
        za_u, za_v, q_all, k, v, gate_a, gate_b = jnp.split(proj, split_at, axis=-1)
        z_a = jax.nn.gelu(jnp.concatenate([za_u, za_v], axis=-1), approximate=False)
        y_a = spatial_gating_mixer(z_a, sgu_norm_g[l], sgu_w[l], sgu_b[l])
        y_b = dilated_attention_mixer(
            q_all.reshape(B, S, ATT_GROUPS, ATT_HEADS, ATT_HEAD_DIM),
            k.reshape(B, S, ATT_HEADS, ATT_HEAD_DIM),
            v.reshape(B, S, ATT_HEADS, ATT_HEAD_DIM)).astype(x.dtype)
        merged = (jax.nn.sigmoid(gate_a) * (y_a @ w_branch_a[l])
                  + jax.nn.sigmoid(gate_b) * (y_b @ w_branch_b[l]))
        x = x + merged @ w_out[l]
        x = x + peer_ffn(rmsnorm(x, norm_ffn_g[l]), peer_wq[l], peer_subkeys[l], peer_u[l], peer_v[l])
    return rmsnorm(x, norm_final_g)
```

```python
from contextlib import ExitStack
import numpy as np
import concourse.bass as bass
import concourse.mybir as mybir
from concourse.bass_utils import run_bass_kernel_spmd

F32 = mybir.dt.float32
BF16 = mybir.dt.bfloat16
U32 = mybir.dt.uint32
AF = mybir.ActivationFunctionType
ALU = mybir.AluOpType
AX = mybir.AxisListType

D = 1024
SEQ = 8192
NTOK = 4096
NSEG = 48
EPS = 1e-6
DIL = (1, 4, 16)
ENG = ("pe", "act", "dve", "pool", "sp")


class Prog:
    def __init__(self, semobj):
        self.semobj = semobj
        self.cnt = {e: 0 for e in ENG}
        self.dcnt = {}
        self.q = {e: [] for e in ENG}
        self.lastw = {}
        self.readers = {}
        self.known = {e: {} for e in ENG}
        self._rec = None

    def capture(self, fn, *args):
        saved = self._rec
        self._rec = []
        fn(*args)
        rec, self._rec = self._rec, saved
        return rec

    def replay(self, rec):
        for kind, a in rec:
            if kind == "op":
                self.op(*a)
            else:
                self.dma(*a)

    def mark(self):
        if self._rec is not None:
            self._rec.append(("mark", None))

    @staticmethod
    def split(rec):
        out = [[]]
        for it in rec:
            if it[0] == "mark":
                out.append([])
            else:
                out[-1].append(it)
        return out

    @staticmethod
    def interleave_n(lists, spans=None):
        items = []
        for li, L in enumerate(lists):
            n = len(L)
            f = 1.0 if spans is None else spans[li]
            for i, it in enumerate(L):
                items.append(((i + 0.5) / n * f, li, i, it))
        items.sort(key=lambda t: (t[0], t[1], t[2]))
        return [t[3] for t in items]

    def pipeline(self, stages_by_item, spans=None):
        n = len(stages_by_item)
        S = max(len(x) for x in stages_by_item)
        for step in range(n + S - 1):
            sts = [st for st in range(S - 1, -1, -1) if 0 <= step - st < n and st < len(stages_by_item[step - st])]
            lists = [stages_by_item[step - st][st] for st in sts]
            self.replay(self.interleave_n(lists, None if spans is None else [spans[st] for st in sts]))

    @staticmethod
    def interleave(A, B):
        out = []
        na, nb = len(A), len(B)
        ia = ib = 0
        while ia < na or ib < nb:
            if ib >= nb or (ia < na and ia * nb <= ib * na):
                out.append(A[ia])
                ia += 1
            else:
                out.append(B[ib])
                ib += 1
        return out

    def _deps(self, eng, reads, writes):
        need = {}

        def add(tok):
            if tok is None:
                return
            s, v = tok
            if need.get(s, 0) < v:
                need[s] = v

        def addw(k):
            for s, v in self.lastw.get(k, {}).items():
                add((s, v))
        for k in reads:
            addw(k)
        for k in writes:
            addw(k)
            for t in self.readers.get(k, ()):
                add(t)
        out = []
        kn = self.known[eng]
        for s, v in need.items():
            if kn.get(s, 0) < v:
                kn[s] = v
                out.append((s, v))
        return out

    def _commit(self, tok, reads, writes):
        for k in reads:
            if not k.startswith("C:"):
                self.readers.setdefault(k, []).append(tok)
        for k in writes:
            if k.startswith("C:"):
                self.lastw.setdefault(k, {})[tok[0]] = tok[1]
            else:
                self.lastw[k] = {tok[0]: tok[1]}
            self.readers[k] = []

    def op(self, eng, fn, reads=(), writes=()):
        if self._rec is not None:
            self._rec.append(("op", (eng, fn, reads, writes)))
            return None
        waits = self._deps(eng, reads, writes)
        self.cnt[eng] += 1
        tok = (eng, self.cnt[eng])
        self.q[eng].append((waits, fn, (eng, 1)))
        self._commit(tok, reads, writes)
        return tok

    def dma(self, eng, fn, semkey, reads=(), writes=()):
        if self._rec is not None:
            self._rec.append(("dma", (eng, fn, semkey, reads, writes)))
            return None
        waits = self._deps(eng, reads, writes)
        self.dcnt[semkey] = self.dcnt.get(semkey, 0) + 16
        tok = (semkey, self.dcnt[semkey])
        self.q[eng].append((waits, fn, (semkey, 16)))
        self._commit(tok, reads, writes)
        return tok

    def drain_dma(self, eng="sp"):
        waits = []
        for s, v in self.dcnt.items():
            if self.known[eng].get(s, 0) < v:
                self.known[eng][s] = v
                waits.append((s, v))
        if waits:
            self.q[eng].append((waits, None, None))

    def emit(self, nc, name):
        q = self.q
        semobj = self.semobj

        def run(eng_name):
            def f(e):
                for waits, fn, inc in q[eng_name]:
                    for s, v in waits:
                        e.wait_ge(semobj[s], v)
                    if fn is not None:
                        ins = fn(e)
                        ins.then_inc(semobj[inc[0]], inc[1])
            return f
        with nc.Block() as block:
            block.tensor(run("pe"))
            block.scalar(run("act"))
            block.vector(run("dve"))
            block.gpsimd(run("pool"))
            block.sync(run("sp"))
        self.q = {e: [] for e in ENG}
        self.lastw = {}
        self.readers = {}


def _slopes():
    return [2.0 ** (-(h + 1)) for h in range(8)]


def build_program(cfg=None):
    cfg = cfg or {}
    segs = cfg.get("segs", list(range(NSEG)))
    nseg = len(segs)
    nblk = cfg.get("nblk", 16)
    ntile = cfg.get("ntile", 32)
    nslot = cfg.get("nslot", 128)
    debug = cfg.get("debug", False)
    pool_rms = cfg.get("pool_rms", 4)
    scratch_kind = "ExternalOutput" if debug else "Internal"

    nc = bass.Bass("TRN2", target_bir_lowering=False)
    dt = nc.dram_tensor
    xseg = dt("xseg", [NSEG, 128, 8, 384], F32, kind="ExternalInput").ap()
    kval_h = dt("kval", [128, NSEG * 3], F32, kind="ExternalInput").ap()
    xtok_h = dt("xtok", [NTOK, D], F32, kind="ExternalInput").ap()
    w_in_h = dt("w_in_l", [128, 8, 6656], F32, kind="ExternalInput").ap()
    gmix_h = dt("gmix", [128, 8], F32, kind="ExternalInput").ap()
    gsgu_h = dt("gsgu_bc", [128, 1024], F32, kind="ExternalInput").ap()
    wsT_h = dt("wsT", [128, 8, 128], F32, kind="ExternalInput").ap()
    bsbc_h = dt("bsgu_bc", [128, 1024], F32, kind="ExternalInput").ap()
    wa_h = dt("wa_l", [128, 8, 1024], F32, kind="ExternalInput").ap()
    wb_h = dt("wb_l", [128, 4, 1024], F32, kind="ExternalInput").ap()
    wo_h = dt("wo_l", [128, 8, 1024], F32, kind="ExternalInput").ap()
    gffn_h = dt("gffn_bc", [128, 1024], F32, kind="ExternalInput").ap()
    gfin_h = dt("gfin_bc", [128, 1024], F32, kind="ExternalInput").ap()
    wq_h = dt("wq_l", [128, 8, 2048], F32, kind="ExternalInput").ap()
    skT_h = dt("skT", [128, 16, 128], F32, kind="ExternalInput").ap()
    uv_h = dt("uv_tab", [16384, 2048], F32, kind="ExternalInput").ap()
    negabs_h = dt("negabs", [128, 512], F32, kind="ExternalInput").ap()
    ident_h = dt("ident", [128, 128], F32, kind="ExternalInput").ap()
    iota_h = dt("iota16", [128, 16], F32, kind="ExternalInput").ap()
    thr_h = dt("thr16", [128, 16], F32, kind="ExternalInput").ap()
    y_h = dt("y", [NTOK, D], F32, kind="ExternalOutput").ap()
    att_d = dt("att_d", [3, NTOK, 520], F32, kind=scratch_kind).ap()
    at_d = dt("at_d", [128, 8, NTOK], F32, kind=scratch_kind).ap()
    x2_d = dt("x2_d", [NTOK, D], F32, kind=scratch_kind).ap()
    uv16_d = dt("uv16_d", [16384, 2048], BF16, kind="Internal").ap()
    rstd_d = dt("rstd_d", [128, NTOK], F32, kind="Internal").ap()

    slopes = _slopes()

    with ExitStack() as top:
        semobj = {}
        for e in ENG:
            semobj[e] = top.enter_context(nc.semaphore("s_" + e))

        def dsem(name, eng="sp"):
            name = name + "@" + eng
            if name not in semobj:
                semobj[name] = top.enter_context(nc.semaphore("d_" + name))
            return name
        P = Prog(semobj)

        def load_cast(dst, src, eng="pool", sem="w", piece=1024):
            n = src.shape[-1]
            pre = (slice(None),) * (len(src.shape) - 1)
            for a in range(0, n, piece):
                b = min(n, a + piece)
                P.dma(eng, (lambda e, d_=dst[pre + (slice(a, b),)], s_=src[pre + (slice(a, b),)]: e.dma_start(out=d_, in_=s_)),
                      dsem(sem, eng), writes=("C:W",))

        def rmsnorm_block(es_bufs, src_ap, bi, n, tagp, rs_src=None, rs_loaded=False):
            xst, sq, rstd, xn, ps_ss, gmix, ones_bf, rtmp = es_bufs[:8]
            mhalf = None
            kx = f"{tagp}xst{bi}"
            if src_ap is not None:
                P.dma("sp", lambda e: e.dma_start(out=xst[bi][:, :, 0:n], in_=src_ap), dsem(kx), writes=(kx,))
                if rs_src is not None:
                    P.dma("sp", lambda e: e.dma_start(out=rstd[bi][:, 0:n], in_=rs_src), dsem(f"{tagp}rsl{bi}"),
                          writes=(f"{tagp}rs{bi}",))
                return None
            if rs_loaded:
                return scale_chunks(es_bufs, bi, n, tagp)
            P.op("act", lambda e: e.activation(out=sq[bi][:, :, 0:n], in_=xst[bi][:, :, 0:n], func=AF.Square),
                 reads=(kx,), writes=(f"{tagp}sq{bi}",))

            def mm_ss(e):
                ins = None
                for c in range(8):
                    ins = e.matmul(ps_ss[:, 0:n], lhsT=ones_bf[:, :], rhs=sq[bi][:, c, 0:n], start=(c == 0), stop=(c == 7))
                return ins
            P.op("pe", mm_ss, reads=(f"{tagp}sq{bi}", "C:W", "C:ones"), writes=(f"{tagp}ps_ss",))
            P.op("dve", lambda e: e.tensor_scalar(out=rstd[bi][:, 0:n], in0=ps_ss[:, 0:n], scalar1=1.0 / D, scalar2=EPS,
                                                  op0=ALU.mult, op1=ALU.add),
                 reads=(f"{tagp}ps_ss",), writes=(f"{tagp}rs{bi}",))
            if mhalf is not None:
                P.op("pool", lambda e: e.tensor_tensor(out=rstd[bi][:, 0:n], in0=rstd[bi][:, 0:n], in1=mhalf[:, 0:n], op=ALU.pow),
                     reads=(f"{tagp}rs{bi}", "C:mhalf"), writes=(f"{tagp}rs{bi}",))
            else:
                P.op("act", lambda e: e.activation(out=rstd[bi][:, 0:n], in_=rstd[bi][:, 0:n], func=AF.Ln),
                     reads=(f"{tagp}rs{bi}",), writes=(f"{tagp}rs{bi}",))
                P.op("act", lambda e: e.activation(out=rstd[bi][:, 0:n], in_=rstd[bi][:, 0:n], func=AF.Exp, scale=-0.5),
                     reads=(f"{tagp}rs{bi}",), writes=(f"{tagp}rs{bi}",))
            return scale_chunks(es_bufs, bi, n, tagp)

        def scale_chunks(es_bufs, bi, n, tagp):
            xst, sq, rstd, xn, ps_ss, gmix, ones_bf, rtmp = es_bufs[:8]
            kx = f"{tagp}xst{bi}"
            for c in range(8):
                if c < pool_rms:
                    tb = c % 2
                    P.op("pool", lambda e, c=c, tb=tb: e.tensor_tensor(
                        out=rtmp[tb][:, 0:n], in0=xst[bi][:, c, 0:n], in1=rstd[bi][:, 0:n], op=ALU.mult),
                        reads=(kx, f"{tagp}rs{bi}"), writes=(f"{tagp}rtmp{tb}",))
                    P.op("pool", lambda e, c=c, tb=tb: e.tensor_scalar(
                        out=xn[bi][:, c, 0:n], in0=rtmp[tb][:, 0:n], scalar1=gmix[:, c:c + 1], scalar2=1.0,
                        op0=ALU.mult, op1=ALU.mult),
                        reads=(f"{tagp}rtmp{tb}", "C:W"), writes=(f"{tagp}xn{bi}.{c}",))
                    continue
                P.op("dve", lambda e, c=c: e.scalar_tensor_tensor(
                    out=xn[bi][:, c, 0:n], in0=xst[bi][:, c, 0:n], scalar=gmix[:, c:c + 1], in1=rstd[bi][:, 0:n],
                    op0=ALU.mult, op1=ALU.mult),
                    reads=(kx, f"{tagp}rs{bi}", "C:W"), writes=(f"{tagp}xn{bi}.{c}",))
            return [f"{tagp}xn{bi}.{c}" for c in range(8)]

        if nseg > 0:
            with ExitStack() as es:
                sb = lambda name, shape, dtp: es.enter_context(nc.sbuf_tensor("sb_" + name, shape, dtp))
                ps = lambda name, shape, dtp=F32: es.enter_context(nc.psum_tensor("pp_" + name, shape, dtp))
                wqkv = sb("wqkv", [128, 8, 2560], BF16)
                gmix = sb("gmix1", [128, 8], F32)
                negabs = sb("negabs", [128, 512], F32)
                ones_bf = sb("ones1", [128, 128], BF16)
                ones8 = sb("ones8", [128, 8, 1], F32)
                kval = sb("kval", [128, NSEG * 3], F32)
                xst = [sb(f"xst{i}", [128, 8, 384], F32) for i in range(2)]
                sq = [sb(f"sq{i}", [128, 8, 384], BF16) for i in range(2)]
                rstd = [sb(f"rstd{i}", [128, 384], F32) for i in range(2)]
                xn = [sb(f"xn{i}", [128, 8, 384], BF16) for i in range(2)]
                kT = [sb(f"kT{i}", [128, 4, 384], BF16) for i in range(2)]
                qT = [sb(f"qT{i}", [128, 4, 256], BF16) for i in range(2)]
                vaug = [sb(f"vaug{i}", [128, 3, 8, 65], BF16) for i in range(2)]
                tt = [sb(f"tt{i}", [128, 512], F32) for i in range(2)]
                pT = [sb(f"pT{i}", [128, 512], BF16) for i in range(2)]
                osb = [sb(f"osb{i}", [128, 2, 520], F32) for i in range(2)]
                cst = [sb(f"cst{i}", [128, 2, 2048], BF16) for i in range(2)]
                castn = [0 if cfg.get("cast", True) else 64]
                ps_ss1 = ps("ps_ss1", [128, 512])
                ps_pr = [ps(f"ps_pr{i}", [128, 512]) for i in range(2)]
                ps_S = [ps(f"ps_S{i}", [128, 512]) for i in range(2)]
                ps_O = [ps(f"ps_O{i}", [128, 512]) for i in range(3)]
                ocnt = [0]

                load_cast(wqkv[:, :, :], w_in_h[:, :, 2048:4608], piece=640)
                P.dma("sp", lambda e: e.dma_start(out=gmix[:, :], in_=gmix_h), dsem("w"), writes=("C:W",))
                P.dma("sp", lambda e: e.dma_start(out=negabs[:, :], in_=negabs_h), dsem("w"), writes=("C:W",))
                P.dma("sp", lambda e: e.dma_start(out=kval[:, :], in_=kval_h), dsem("w"), writes=("C:W",))
                P.op("dve", lambda e: e.memset(ones_bf[:, :], 1.0), writes=("C:ones",))
                P.op("dve", lambda e: e.memset(ones8[:, :, :], 1.0), writes=("C:ones8",))

                prn = [0]

                def next_pr():
                    prn[0] += 1
                    return prn[0] % 2

                rtmp = [sb(f"rtmp{i}", [128, 384], F32) for i in range(2)]
                bufs1 = (xst, sq, rstd, xn, ps_ss1, gmix, ones_bf, rtmp)
                evn = [0]

                def evac_copy(out_ap, in_ap, reads, writes):
                    evn[0] += 1
                    if evn[0] % 2:
                        P.op("act", lambda e: e.copy(out=out_ap, in_=in_ap), reads=reads, writes=writes)
                    else:
                        P.op("dve", lambda e: e.tensor_copy(out=out_ap, in_=in_ap), reads=reads, writes=writes)

                def seg_geo(si, s):
                    g = s // 16
                    d = DIL[g]
                    sp_ = s % 16
                    nsub = 16 // d
                    return g, d, sp_ // nsub, sp_ % nsub, si % 2

                def cast_chunk(ci):
                    cb = ci % 2
                    srcv = uv_h[256 * ci:256 * (ci + 1), :].rearrange("(p r) c -> p r c", r=2)
                    dstv = uv16_d[256 * ci:256 * (ci + 1), :].rearrange("(p r) c -> p r c", r=2)
                    P.dma("pool", lambda e: e.dma_start(out=cst[cb][:, :, :], in_=srcv), dsem(f"cst{cb}", "pool"), writes=(f"cst{cb}",))
                    P.dma("sp", lambda e: e.dma_start(out=dstv, in_=cst[cb][:, :, :]), dsem(f"cso{cb}"), reads=(f"cst{cb}",))

                def body1a(si, s):
                    g, d, r, sub, bi = seg_geo(si, s)
                    if si == 0:
                        rmsnorm_block(bufs1, xseg[s], bi, 384, "a")
                    if si + 1 < nseg:
                        rmsnorm_block(bufs1, xseg[segs[si + 1]], (si + 1) % 2, 384, "a")
                    for _ in range(2 if si % 3 == 0 else 1):
                        if castn[0] < 64:
                            cast_chunk(castn[0])
                            castn[0] += 1
                    xk = rmsnorm_block(bufs1, None, bi, 384, "a")
                    P.mark()
                    if g == 0:
                        P.dma("sp", lambda e: e.dma_start(out=rstd_d[:, 256 * s:256 * (s + 1)], in_=rstd[bi][:, 64:320]),
                              dsem(f"rsd{bi}"), reads=(f"ars{bi}",))

                    for j in range(4):
                        pj = next_pr()

                        def mm_k(e, j=j, pj=pj):
                            ins = None
                            for c in range(8):
                                ins = e.matmul(ps_pr[pj][:, 0:384], lhsT=wqkv[:, c, 1536 + 128 * j:1536 + 128 * (j + 1)],
                                               rhs=xn[bi][:, c, :], start=(c == 0), stop=(c == 7))
                            return ins
                        P.op("pe", mm_k, reads=tuple(xk) + ("C:W",), writes=(f"ps_pr{pj}",))
                        evac_copy(kT[bi][:, j, :], ps_pr[pj][:, 0:384], (f"ps_pr{pj}",), (f"kT{bi}.{j}",))
                    for j in range(4):
                        pj = next_pr()

                        def mm_q(e, j=j, pj=pj):
                            ins = None
                            for c in range(8):
                                ins = e.matmul(ps_pr[pj][:, 0:256], lhsT=wqkv[:, c, g * 512 + 128 * j:g * 512 + 128 * (j + 1)],
                                               rhs=xn[bi][:, c, 64:320], start=(c == 0), stop=(c == 7))
                            return ins
                        P.op("pe", mm_q, reads=tuple(xk) + ("C:W",), writes=(f"ps_pr{pj}",))
                        evac_copy(qT[bi][:, j, :], ps_pr[pj][:, 0:256], (f"ps_pr{pj}",), (f"qT{bi}.{j}",))
                    for j in range(3):
                        pj = next_pr()

                        def mm_v(e, j=j, pj=pj):
                            ins = None
                            for c in range(8):
                                ins = e.matmul(ps_pr[pj][:, :], lhsT=xn[bi][:, c, 128 * j:128 * (j + 1)],
                                               rhs=wqkv[:, c, 2048:2560], start=(c == 0), stop=(c == 7))
                            return ins
                        P.op("pe", mm_v, reads=tuple(xk) + ("C:W",), writes=(f"ps_pr{pj}",))
                        col = 3 * s + j
                        P.op("act", lambda e, j=j, pj=pj, col=col: e.activation(
                            out=vaug[bi][:, j, :, 0:64], in_=ps_pr[pj][:, :].rearrange("p (h e) -> p h e", h=8),
                            func=AF.Copy, scale=kval[:, col:col + 1]),
                            reads=(f"ps_pr{pj}", "C:W"), writes=(f"va{bi}.{j}",))
                        P.op("dve", lambda e, j=j, col=col: e.tensor_scalar(
                            out=vaug[bi][:, j, :, 64:65], in0=ones8[:, :, :],
                            scalar1=kval[:, col:col + 1], scalar2=None, op0=ALU.mult),
                            reads=("C:W", "C:ones8"), writes=(f"vb{bi}.{j}",))

                def body1b(si, s):
                    g, d, r, sub, bi = seg_geo(si, s)
                    obm = {}

                    def front(h):
                        jc, pb = h // 2, 64 * (h % 2)
                        ri = h % 2
                        coef = slopes[h] * d * 8.0

                        def mm_s(e, jc=jc, pb=pb, ri=ri):
                            kk = kT[bi][pb:pb + 64, jc, :]
                            qq = qT[bi][pb:pb + 64, jc, :]
                            e.matmul(ps_S[ri][:, 0:128], lhsT=kk[:, 0:128], rhs=qq[:, 0:128], start=True, stop=True)
                            e.matmul(ps_S[ri][:, 128:384], lhsT=kk[:, 128:256], rhs=qq[:, 0:256], start=True, stop=True)
                            return e.matmul(ps_S[ri][:, 384:512], lhsT=kk[:, 256:384], rhs=qq[:, 128:256], start=True, stop=True)
                        P.op("pe", mm_s, reads=(f"kT{bi}.{jc}", f"qT{bi}.{jc}"), writes=(f"ps_S{ri}",))
                        P.op("dve", lambda e, ri=ri, coef=coef: e.scalar_tensor_tensor(
                            out=tt[ri][:, :], in0=negabs[:, :], scalar=coef, in1=ps_S[ri][:, :], op0=ALU.mult, op1=ALU.add),
                            reads=(f"ps_S{ri}", "C:W"), writes=(f"tt{ri}",))
                        P.op("act", lambda e, ri=ri: e.activation(out=pT[ri][:, :], in_=tt[ri][:, :], func=AF.Exp, scale=0.125),
                             reads=(f"tt{ri}",), writes=(f"pT{ri}",))

                    def back(h):
                        ri = h % 2
                        hg = h // 4
                        if h % 4 == 0:
                            for qt in range(2):
                                ocnt[0] += 1
                                obm[qt] = ocnt[0] % 3
                        for qt in range(2):
                            ob = obm[qt]
                            c0 = (h % 4) * 65

                            def mm_o(e, qt=qt, ob=ob, c0=c0, h=h, ri=ri):
                                e.matmul(ps_O[ob][:, c0:c0 + 65], lhsT=pT[ri][:, 256 * qt:256 * qt + 128],
                                         rhs=vaug[bi][:, qt, h, :], start=True, stop=False)
                                return e.matmul(ps_O[ob][:, c0:c0 + 65], lhsT=pT[ri][:, 256 * qt + 128:256 * qt + 256],
                                                rhs=vaug[bi][:, qt + 1, h, :], start=False, stop=True)
                            P.op("pe", mm_o, reads=(f"pT{ri}", f"va{bi}.{qt}", f"vb{bi}.{qt}", f"va{bi}.{qt + 1}", f"vb{bi}.{qt + 1}"),
                                 writes=(f"ps_O{ob}",))
                        if h % 4 == 3:
                            for qt in range(2):
                                ob = obm[qt]
                                evac_copy(osb[bi][:, qt, 260 * hg:260 * (hg + 1)], ps_O[ob][:, 0:260],
                                          (f"ps_O{ob}",), (f"osb{bi}.{qt}.{hg}",))

                    front(0)
                    for h in range(8):
                        if h + 1 < 8:
                            front(h + 1)
                        back(h)
                    P.mark()
                    for qt in range(2):
                        m0 = 256 * sub + 128 * qt
                        lo = d * m0 + r
                        dst = att_d[g][lo:lo + d * 127 + 1:d, :]
                        P.dma("sp", lambda e, dst=dst, qt=qt: e.dma_start(out=dst, in_=osb[bi][:, qt, :]),
                              dsem(f"osb{bi}.{qt}"), reads=(f"osb{bi}.{qt}.0", f"osb{bi}.{qt}.1"), writes=())
                recA = [P.split(P.capture(body1a, si_, s_)) for si_, s_ in enumerate(segs)]
                extras = []
                while castn[0] < 64:
                    extras.append(P.capture(cast_chunk, castn[0]))
                    castn[0] += 1
                recB = [P.split(P.capture(body1b, si_, s_)) for si_, s_ in enumerate(segs)]
                P.pipeline([recA[i] + recB[i] for i in range(nseg)])
                for extra in extras:
                    P.replay(extra)
                P.drain_dma("sp")
                P.drain_dma("pool")
                P.emit(nc, "p1")

        if nblk > 0:
            with ExitStack() as es:
                sb = lambda name, shape, dtp: es.enter_context(nc.sbuf_tensor("sb_" + name, shape, dtp))
                ps = lambda name, shape, dtp=F32: es.enter_context(nc.psum_tensor("pp_" + name, shape, dtp))
                w_uv = sb("w_uv", [128, 8, 2048], BF16)
                w_a = sb("w_a", [128, 8, 1024], BF16)
                wsT = sb("wsT", [128, 8, 128], BF16)
                bsbc = sb("bsbc", [128, 1024], F32)
                mxb = [sb(f"mxb{i}", [128, 512], F32) for i in range(2)]
                gsgu = sb("gsgu", [128, 1024], F32)
                gmix = sb("gmix2", [128, 8], F32)
                ones_bf = sb("ones2", [128, 128], BF16)
                xst = [sb(f"bxst{i}", [128, 8, 256], F32) for i in range(2)]
                sq = [sb(f"bsq{i}", [128, 8, 256], BF16) for i in range(2)]
                rstd = [sb(f"brstd{i}", [128, 256], F32) for i in range(2)]
                xn = [sb(f"bxn{i}", [128, 8, 256], BF16) for i in range(2)]
                uT = [sb(f"uT{i}", [128, 8, 256], BF16) for i in range(2)]
                vg = [sb(f"vg{i}", [128, 1024], F32) for i in range(4)]
                junkr = [sb(f"junk{i}", [128, 1024], BF16) for i in range(4)]
                jn = [0]

                def nj():
                    jn[0] += 1
                    return jn[0] % 4
                ssv4 = sb("ssv4", [128, 4], F32)
                vn = [sb(f"vn{i}", [128, 1024], BF16) for i in range(4)]
                yaT = [sb(f"yaT{i}", [128, 8, 256], BF16) for i in range(2)]
                atsb = [sb(f"atsb{i}", [128, 8, 256], F32) for i in range(2)]
                ps_ss = ps("b_ss", [128, 512])
                ps_pr = [ps(f"b_pr{i}", [128, 512]) for i in range(2)]
                ps_mx = [ps(f"b_mx{i}", [128, 512]) for i in range(2)]
                ps_at = [ps(f"b_at{i}", [128, 512]) for i in range(2)]

                load_cast(w_uv[:, :, :], w_in_h[:, :, 0:2048], piece=1024)
                load_cast(w_a[:, :, :], wa_h[:, :, :], piece=1024)
                load_cast(wsT[:, :, :], wsT_h[:, :, :], piece=128)
                for dst_, src_ in ((bsbc[:, :], bsbc_h), (gsgu[:, :], gsgu_h), (gmix[:, :], gmix_h)):
                    P.dma("sp", lambda e, d_=dst_, s_=src_: e.dma_start(out=d_, in_=s_), dsem("w"), writes=("C:W",))
                P.op("dve", lambda e: e.memset(ones_bf[:, :], 1.0), writes=("C:ones",))
                prn = [0]
                rtmp = [sb(f"brtmp{i}", [128, 256], F32) for i in range(2)]
                bufs2 = (xst, sq, rstd, xn, ps_ss, gmix, ones_bf, rtmp)

                def body2(nb):
                    bi = nb % 2
                    if nb == 0:
                        rmsnorm_block(bufs2, xseg[nb][:, :, 64:320], bi, 256, "b", rs_src=rstd_d[:, 256 * nb:256 * (nb + 1)])
                    if nb + 1 < nblk:
                        rmsnorm_block(bufs2, xseg[nb + 1][:, :, 64:320], (nb + 1) % 2, 256, "b",
                                      rs_src=rstd_d[:, 256 * (nb + 1):256 * (nb + 2)])
                    xk = rmsnorm_block(bufs2, None, bi, 256, "b", rs_loaded=True)
                    P.mark()
                    for j in range(8):
                        prn[0] += 1
                        pj = prn[0] % 2

                        def mm_u(e, j=j, pj=pj):
                            ins = None
                            for c in range(8):
                                ins = e.matmul(ps_pr[pj][:, 0:256], lhsT=w_uv[:, c, 128 * j:128 * (j + 1)],
                                               rhs=xn[bi][:, c, :], start=(c == 0), stop=(c == 7))
                            return ins
                        P.op("pe", mm_u, reads=tuple(xk) + ("C:W",), writes=(f"b_pr{pj}",))
                        P.op("act", lambda e, j=j, pj=pj: e.activation(out=uT[bi][:, j, :], in_=ps_pr[pj][:, 0:256], func=AF.Gelu),
                             reads=(f"b_pr{pj}",), writes=(f"uT{bi}.{j}",))
                    for t2 in range(2):
                        vi = (nb * 2 + t2) % 4
                        for hh in range(2):
                            prn[0] += 1
                            pj = prn[0] % 2

                            def mm_v(e, t2=t2, hh=hh, pj=pj):
                                ins = None
                                for c in range(8):
                                    ins = e.matmul(ps_pr[pj][:, :], lhsT=xn[bi][:, c, 128 * t2:128 * (t2 + 1)],
                                                   rhs=w_uv[:, c, 1024 + 512 * hh:1024 + 512 * (hh + 1)],
                                                   start=(c == 0), stop=(c == 7))
                                return ins
                            P.op("pe", mm_v, reads=tuple(xk) + ("C:W",), writes=(f"b_pr{pj}",))
                            P.op("act", lambda e, hh=hh, pj=pj, vi=vi: e.activation(
                                out=vg[vi][:, 512 * hh:512 * (hh + 1)], in_=ps_pr[pj][:, :], func=AF.Gelu),
                                reads=(f"b_pr{pj}",), writes=(f"vg{vi}.{hh}",))
                        ji = nj()
                        P.op("dve", lambda e, vi=vi, ji=ji: e.scalar_tensor_tensor(
                            out=junkr[ji][:, :], in0=vg[vi][:, :], scalar=1.0, in1=vg[vi][:, :], op0=ALU.mult, op1=ALU.mult,
                            accum_out=ssv4[:, vi:vi + 1]),
                            reads=(f"vg{vi}.0", f"vg{vi}.1"), writes=(f"ssv{vi}", f"junk{ji}"))
                    v0 = (nb * 2) % 4
                    sk = (f"ssv{v0}", f"ssv{v0 + 1}")
                    P.op("dve", lambda e: e.tensor_scalar(out=ssv4[:, v0:v0 + 2], in0=ssv4[:, v0:v0 + 2], scalar1=1.0 / D, scalar2=EPS,
                                                          op0=ALU.mult, op1=ALU.add), reads=sk, writes=sk)
                    P.op("act", lambda e: e.activation(out=ssv4[:, v0:v0 + 2], in_=ssv4[:, v0:v0 + 2], func=AF.Ln), reads=sk, writes=sk)
                    P.op("act", lambda e: e.activation(out=ssv4[:, v0:v0 + 2], in_=ssv4[:, v0:v0 + 2], func=AF.Exp, scale=-0.5),
                         reads=sk, writes=sk)
                    for t2 in range(2):
                        vi = (nb * 2 + t2) % 4
                        P.op("dve", lambda e, vi=vi: e.scalar_tensor_tensor(
                            out=vn[vi][:, :], in0=vg[vi][:, :], scalar=ssv4[:, vi:vi + 1], in1=gsgu[:, :], op0=ALU.mult, op1=ALU.mult),
                            reads=(f"vg{vi}.0", f"vg{vi}.1", f"ssv{vi}", "C:W"), writes=(f"vn{vi}",))
                    P.mark()
                    for t2 in range(2):
                        vi = (nb * 2 + t2) % 4
                        for gq in range(2):
                            mi = gq

                            def mm_mix(e, gq=gq, mi=mi, vi=vi):
                                ins = None
                                for g4 in range(4):
                                    gg = gq * 4 + g4
                                    ins = e.matmul(ps_mx[mi][:, 128 * g4:128 * (g4 + 1)], lhsT=vn[vi][:, 128 * gg:128 * (gg + 1)],
                                                   rhs=wsT[:, gg, :], start=True, stop=True)
                                return ins
                            P.op("pe", mm_mix, reads=(f"vn{vi}", "C:W"), writes=(f"b_mx{mi}",))
                            P.op("dve", lambda e, gq=gq, mi=mi: e.tensor_tensor(
                                out=mxb[mi][:, :], in0=ps_mx[mi][:, :], in1=bsbc[:, 512 * gq:512 * (gq + 1)], op=ALU.add),
                                reads=(f"b_mx{mi}", "C:W"), writes=(f"mxb{mi}",))
                            P.op("dve", lambda e, gq=gq, mi=mi, t2=t2: e.tensor_tensor(
                                out=yaT[bi][:, 4 * gq:4 * gq + 4, 128 * t2:128 * (t2 + 1)],
                                in0=mxb[mi][:, :].rearrange("p (g t) -> p g t", g=4),
                                in1=uT[bi][:, 4 * gq:4 * gq + 4, 128 * t2:128 * (t2 + 1)], op=ALU.mult),
                                reads=(f"mxb{mi}",) + tuple(f"uT{bi}.{4 * gq + q}" for q in range(4)),
                                writes=(f"yaT{bi}.{gq}.{t2}",))
                    yk = tuple(f"yaT{bi}.{gq}.{t2}" for gq in range(2) for t2 in range(2))
                    for j in range(8):
                        pj = j % 2

                        def mm_a(e, j=j, pj=pj):
                            ins = None
                            for c in range(8):
                                ins = e.matmul(ps_at[pj][:, 0:256], lhsT=w_a[:, c, 128 * j:128 * (j + 1)],
                                               rhs=yaT[bi][:, c, :], start=(c == 0), stop=(c == 7))
                            return ins
                        P.op("pe", mm_a, reads=yk + ("C:W",), writes=(f"b_at{pj}",))
                        if j % 2:
                            P.op("act", lambda e, j=j, pj=pj: e.copy(out=atsb[bi][:, j, :], in_=ps_at[pj][:, 0:256]),
                                 reads=(f"b_at{pj}",), writes=(f"atsb{bi}.{j}",))
                        else:
                            P.op("dve", lambda e, j=j, pj=pj: e.tensor_copy(out=atsb[bi][:, j, :], in_=ps_at[pj][:, 0:256]),
                                 reads=(f"b_at{pj}",), writes=(f"atsb{bi}.{j}",))
                    P.mark()
                    P.dma("sp", lambda e, nb=nb: e.dma_start(out=at_d[:, :, 256 * nb:256 * (nb + 1)], in_=atsb[bi][:, :, :]),
                          dsem(f"atsb{bi}"), reads=tuple(f"atsb{bi}.{j}" for j in range(8)), writes=())
                P.pipeline([P.split(P.capture(body2, nb_)) for nb_ in range(nblk)])
                P.drain_dma("sp")
                P.drain_dma("pool")
                P.emit(nc, "p2a")

        if nblk > 0:
            with ExitStack() as es:
                sb = lambda name, shape, dtp: es.enter_context(nc.sbuf_tensor("sb_" + name, shape, dtp))
                ps = lambda name, shape, dtp=F32: es.enter_context(nc.psum_tensor("pp_" + name, shape, dtp))
                w_g = sb("w_g", [128, 8, 2048], BF16)
                w_b = sb("w_b", [128, 4, 1024], BF16)
                w_o = sb("w_o", [128, 8, 1024], BF16)
                ident = sb("identb", [128, 128], BF16)
                gmix = sb("gmix3", [128, 8], F32)
                ones_bf = sb("ones3", [128, 128], BF16)
                xst = [sb(f"cxst{i}", [128, 8, 256], F32) for i in range(2)]
                sq = [sb(f"csq{i}", [128, 8, 256], BF16) for i in range(2)]
                rstd = [sb(f"crstd{i}", [128, 256], F32) for i in range(2)]
                xn = [sb(f"cxn{i}", [128, 8, 256], BF16) for i in range(2)]
                sg = [sb(f"sg{i}", [128, 16, 256], BF16) for i in range(2)]
                a3 = [sb(f"a3{i}", [128, 3, 520], F32) for i in range(2)]
                s2 = [sb(f"s2{i}", [128, 520], F32) for i in range(2)]
                rden = [sb(f"rden{i}", [128, 8, 1], F32) for i in range(2)]
                yb = [sb(f"yb{i}", [128, 512], BF16) for i in range(2)]
                ybT = [sb(f"ybT{i}", [128, 4, 256], BF16) for i in range(2)]
                atl = [sb(f"atl{i}", [128, 8, 256], F32) for i in range(2)]
                tmpa = [sb(f"tmpa{i}", [128, 256], F32) for i in range(2)]
                tmpb = [sb(f"tmpb{i}", [128, 256], F32) for i in range(2)]
                mT = [sb(f"mT{i}", [128, 8, 256], BF16) for i in range(2)]
                xtk = [sb(f"xtk{i}", [128, 1024], F32) for i in range(4)]
                x2 = [sb(f"x2{i}", [128, 1024], F32) for i in range(4)]
                ps_ss = ps("c_ss", [128, 512])
                ps_pr = [ps(f"c_pr{i}", [128, 512]) for i in range(2)]
                ps_T = ps("c_T", [128, 4, 128], BF16)
                ps_B = [ps(f"c_B{i}", [128, 512]) for i in range(2)]
                ps_o = [ps(f"c_o{i}", [128, 512]) for i in range(2)]

                load_cast(w_g[:, :, :], w_in_h[:, :, 4608:6656], piece=1024)
                load_cast(w_b[:, :, :], wb_h[:, :, :], piece=1024)
                load_cast(w_o[:, :, :], wo_h[:, :, :], piece=1024)
                load_cast(ident[:, :], ident_h, piece=128)
                P.dma("sp", lambda e: e.dma_start(out=gmix[:, :], in_=gmix_h), dsem("w"), writes=("C:W",))
                P.op("dve", lambda e: e.memset(ones_bf[:, :], 1.0), writes=("C:ones",))
                prn = [0]
                rtmp = [sb(f"crtmp{i}", [128, 256], F32) for i in range(2)]
                bufs3 = (xst, sq, rstd, xn, ps_ss, gmix, ones_bf, rtmp)

                def body3(nb):
                    bi = nb % 2
                    if nb == 0:
                        rmsnorm_block(bufs3, xseg[nb][:, :, 64:320], bi, 256, "c", rs_src=rstd_d[:, 256 * nb:256 * (nb + 1)])
                    if nb + 1 < nblk:
                        rmsnorm_block(bufs3, xseg[nb + 1][:, :, 64:320], (nb + 1) % 2, 256, "c",
                                      rs_src=rstd_d[:, 256 * (nb + 1):256 * (nb + 2)])
                    xk = rmsnorm_block(bufs3, None, bi, 256, "c", rs_loaded=True)
                    P.mark()
                    for j in range(16):
                        prn[0] += 1
                        pj = prn[0] % 2

                        def mm_g(e, j=j, pj=pj):
                            ins = None
                            for c in range(8):
                                ins = e.matmul(ps_pr[pj][:, 0:256], lhsT=w_g[:, c, 128 * j:128 * (j + 1)],
                                               rhs=xn[bi][:, c, :], start=(c == 0), stop=(c == 7))
                            return ins
                        P.op("pe", mm_g, reads=tuple(xk) + ("C:W",), writes=(f"c_pr{pj}",))
                        P.op("act", lambda e, j=j, pj=pj: e.activation(out=sg[bi][:, j, :], in_=ps_pr[pj][:, 0:256], func=AF.Sigmoid),
                             reads=(f"c_pr{pj}",), writes=(f"sg{bi}.{j}",))
                    P.dma("sp", lambda e, nb=nb: e.dma_start(out=atl[bi][:, :, :], in_=at_d[:, :, 256 * nb:256 * (nb + 1)]),
                          dsem(f"atl{bi}"), writes=(f"atl{bi}",))
                    for t2 in range(2):
                        ti = (nb * 2 + t2) % 2
                        t0 = 256 * nb + 128 * t2
                        P.dma("sp", lambda e, ti=ti, t0=t0: e.dma_start(
                            out=a3[ti][:, :, :], in_=att_d[:, t0:t0 + 128, :].rearrange("g t c -> t g c")),
                            dsem(f"a3{ti}"), writes=(f"a3{ti}",))
                        xi = (nb * 2 + t2) % 4
                        P.dma("sp", lambda e, xi=xi, t0=t0: e.dma_start(out=xtk[xi][:, :], in_=xtok_h[t0:t0 + 128, :]),
                              dsem(f"xtk{xi}"), writes=(f"xtk{xi}",))
                        P.op("dve", lambda e, ti=ti: e.tensor_tensor(out=s2[ti][:, :], in0=a3[ti][:, 0, :], in1=a3[ti][:, 1, :], op=ALU.add),
                             reads=(f"a3{ti}",), writes=(f"s2{ti}",))
                        P.op("dve", lambda e, ti=ti: e.tensor_tensor(out=s2[ti][:, :], in0=s2[ti][:, :], in1=a3[ti][:, 2, :], op=ALU.add),
                             reads=(f"a3{ti}", f"s2{ti}"), writes=(f"s2{ti}",))
                        P.op("dve", lambda e, ti=ti: e.reciprocal(
                            out=rden[ti][:, :, :], in_=s2[ti][:, :].rearrange("p (h e) -> p h e", h=8)[:, :, 64:65]),
                            reads=(f"s2{ti}",), writes=(f"rden{ti}",))
                        P.op("dve", lambda e, ti=ti: e.tensor_tensor(
                            out=yb[ti][:, :].rearrange("p (h e) -> p h e", h=8),
                            in0=s2[ti][:, :].rearrange("p (h e) -> p h e", h=8)[:, :, 0:64],
                            in1=rden[ti][:, :, :].to_broadcast([128, 8, 64]), op=ALU.mult),
                            reads=(f"s2{ti}", f"rden{ti}"), writes=(f"yb{ti}",))

                        def tr_y(e, ti=ti):
                            ins = None
                            for j in range(4):
                                ins = e.transpose(ps_T[:, j, :], yb[ti][:, 128 * j:128 * (j + 1)], ident[:, :])
                            return ins
                        P.op("pe", tr_y, reads=(f"yb{ti}", "C:W"), writes=("c_T",))
                        P.op("act", lambda e, t2=t2: e.copy(out=ybT[bi][:, :, 128 * t2:128 * (t2 + 1)], in_=ps_T[:, :, :]),
                             reads=("c_T",), writes=(f"ybT{bi}.{t2}",))
                    P.mark()
                    for j in range(8):
                        pj = j % 2

                        def mm_b(e, j=j, pj=pj):
                            ins = None
                            for c in range(4):
                                ins = e.matmul(ps_B[pj][:, 0:256], lhsT=w_b[:, c, 128 * j:128 * (j + 1)],
                                               rhs=ybT[bi][:, c, :], start=(c == 0), stop=(c == 3))
                            return ins
                        P.op("pe", mm_b, reads=(f"ybT{bi}.0", f"ybT{bi}.1", "C:W"), writes=(f"c_B{pj}",))
                        P.op("dve", lambda e, j=j, pj=pj: e.tensor_tensor(out=tmpa[pj][:, :], in0=atl[bi][:, j, :], in1=sg[bi][:, j, :], op=ALU.mult),
                             reads=(f"atl{bi}", f"sg{bi}.{j}"), writes=(f"tmpa{pj}",))
                        P.op("dve", lambda e, j=j, pj=pj: e.tensor_tensor(out=tmpb[pj][:, :], in0=ps_B[pj][:, 0:256], in1=sg[bi][:, 8 + j, :], op=ALU.mult),
                             reads=(f"c_B{pj}", f"sg{bi}.{8 + j}"), writes=(f"tmpb{pj}",))
                        P.op("dve", lambda e, j=j, pj=pj: e.tensor_tensor(out=mT[bi][:, j, :], in0=tmpa[pj][:, :], in1=tmpb[pj][:, :], op=ALU.add),
                             reads=(f"tmpa{pj}", f"tmpb{pj}"), writes=(f"mT{bi}.{j}",))
                    mk = tuple(f"mT{bi}.{j}" for j in range(8))
                    for t2 in range(2):
                        ti = (nb * 2 + t2) % 2
                        t0 = 256 * nb + 128 * t2
                        for hh in range(2):
                            def mm_o2(e, t2=t2, hh=hh):
                                ins = None
                                for c in range(8):
                                    ins = e.matmul(ps_o[hh][:, :], lhsT=mT[bi][:, c, 128 * t2:128 * (t2 + 1)],
                                                   rhs=w_o[:, c, 512 * hh:512 * (hh + 1)], start=(c == 0), stop=(c == 7))
                                return ins
                            P.op("pe", mm_o2, reads=mk + ("C:W",), writes=(f"c_o{hh}",))
                            xi = (nb * 2 + t2) % 4
                            P.op("dve", lambda e, hh=hh, xi=xi: e.tensor_tensor(
                                out=x2[xi][:, 512 * hh:512 * (hh + 1)], in0=ps_o[hh][:, :], in1=xtk[xi][:, 512 * hh:512 * (hh + 1)], op=ALU.add),
                                reads=(f"c_o{hh}", f"xtk{xi}"), writes=(f"x2{xi}.{hh}",))
                    P.mark()
                    for t2 in range(2):
                        xi = (nb * 2 + t2) % 4
                        t0 = 256 * nb + 128 * t2
                        P.dma("sp", lambda e, xi=xi, t0=t0: e.dma_start(out=x2_d[t0:t0 + 128, :], in_=x2[xi][:, :]),
                              dsem(f"x2{xi}"), reads=(f"x2{xi}.0", f"x2{xi}.1"), writes=())
                P.pipeline([P.split(P.capture(body3, nb_)) for nb_ in range(nblk)])
                P.drain_dma("sp")
                P.drain_dma("pool")
                P.emit(nc, "p2b")

        if ntile > 0:
            with ExitStack() as es:
                sb = lambda name, shape, dtp: es.enter_context(nc.sbuf_tensor("sb_" + name, shape, dtp))
                ps = lambda name, shape, dtp=F32: es.enter_context(nc.psum_tensor("pp_" + name, shape, dtp))
                NB = 13
                NDG = 4
                wq = sb("wq", [128, 8, 2048], BF16)
                skT = sb("skT", [128, 16, 128], BF16)
                ident = sb("identp", [128, 128], BF16)
                gffn = sb("gffn", [128, 1024], F32)
                gfin = sb("gfin", [128, 1024], F32)
                iota16 = sb("iota16", [128, 16], F32)
                thr16 = sb("thr16", [128, 16], F32)
                posf = sb("posf", [128, 8, 16], F32)
                x2t = [sb(f"x2t{i}", [128, 1024], F32) for i in range(3)]
                hn = [sb(f"hn{i}", [128, 1024], F32) for i in range(3)]
                hnb = sb("hnb", [128, 1024], BF16)
                hnT = sb("hnT", [128, 8, 128], BF16)
                qTp = sb("qTp", [128, 16, 128], BF16)
                junkr = [sb(f"junkp{i}", [128, 1024], BF16) for i in range(2)]
                jn = [0]

                def nj():
                    jn[0] += 1
                    return jn[0] % 2
                st1 = sb("st1", [128, 1], F32)
                S_sbs = [sb(f"S_sb{i}", [128, 16, 128], F32) for i in range(2)]
                S2 = sb("S2", [128, 16, 128], F32)
                tops = sb("top", [128, 16, 16], F32)
                idxu = sb("idxu", [128, 16, 16], U32)
                idxf = sb("idxf", [128, 16, 16], F32)
                cand = sb("cand", [128, 8, 256], F32)
                cand2 = sb("cand2", [128, 8, 256], F32)
                best = sb("best", [128, 8, 16], F32)
                posu = sb("posu", [128, 8, 16], U32)
                pa_u = sb("pa_u", [128, 8, 16], U32)
                pb_u = sb("pb_u", [128, 8, 16], U32)
                pa_f = sb("pa_f", [128, 8, 16], F32)
                pb_f = sb("pb_f", [128, 8, 16], F32)
                eq = sb("eq", [128, 128, 16], F32)
                i1s = sb("i1s", [128, 128], F32)
                i2s = sb("i2s", [128, 128], F32)
                expf = sb("expf", [128, 128], F32)
                expu = [sb(f"expu{i}", [128, 128], U32) for i in range(2)]
                gate = [sb(f"gate{i}", [128, 8, 16], F32) for i in range(2)]
                gsum = sb("gsum", [128, 8, 1], F32)
                aval = sb("aval", [128, 128], F32)
                gval = sb("gval", [128, 128], F32)
                wval = sb("wval", [128, 128], F32)
                gbuf = [sb(f"gbuf{i}", [128, 2048], BF16) for i in range(NB)]
                dg = [sb(f"dg{i}", [128, 128], BF16) for i in range(NDG)]
                ident_f = sb("ident_f", [128, 128], F32)
                st2 = sb("st2", [128, 1], F32)
                x3 = sb("x3", [128, 1024], F32)
                yo = [sb(f"yo{i}", [128, 1024], F32) for i in range(2)]
                ps_T = ps("p_T", [128, 8, 128], BF16)
                ps_q = [ps(f"p_q{i}", [128, 512]) for i in range(2)]
                ps_Sc = [ps(f"p_S{i}", [128, 512]) for i in range(2)]
                ps_acc = [ps(f"p_acc{i}", [128, 512]) for i in range(2)]

                load_cast(wq[:, :, :], wq_h[:, :, :], piece=1024)
                load_cast(skT[:, :, :], skT_h[:, :, :], piece=128)
                load_cast(ident[:, :], ident_h, piece=128)
                for dst_, src_ in ((gffn[:, :], gffn_h), (gfin[:, :], gfin_h), (iota16[:, :], iota_h), (thr16[:, :], thr_h),
                                   (ident_f[:, :], ident_h)):
                    P.dma("sp", lambda e, d_=dst_, s_=src_: e.dma_start(out=d_, in_=s_), dsem("w"), writes=("C:W",))

                def rms_rstd(src, dst1, tag):
                    P.op("act", lambda e: e.activation(out=hnb[:, :], in_=src, func=AF.Square, accum_out=dst1),
                         reads=(tag,), writes=(tag + "r", "hnb"))
                    P.op("dve", lambda e: e.tensor_scalar(out=dst1, in0=dst1, scalar1=1.0 / D, scalar2=EPS, op0=ALU.mult, op1=ALU.add),
                         reads=(tag + "r",), writes=(tag + "r",))
                    P.op("act", lambda e: e.activation(out=dst1, in_=dst1, func=AF.Ln), reads=(tag + "r",), writes=(tag + "r",))
                    P.op("act", lambda e: e.activation(out=dst1, in_=dst1, func=AF.Exp, scale=-0.5), reads=(tag + "r",), writes=(tag + "r",))

                gcount = [0]
                def load_x2(tI):
                    P.dma("sp", lambda e, ti=tI % 3, t0=128 * tI: e.dma_start(out=x2t[ti][:, :], in_=x2_d[t0:t0 + 128, :]),
                          dsem(f"x2t{tI % 3}"), writes=(f"x2t{tI % 3}",))
                def body4a(tI):
                    ti = tI % 2
                    t3 = tI % 3
                    S_sb = S_sbs[tI % 2]
                    sk_ = f"S_sb{tI % 2}"
                    t0 = 128 * tI
                    load_x2(tI)
                    rms_rstd(x2t[t3][:, :], st1[:, :], f"x2t{t3}")
                    P.op("dve", lambda e: e.scalar_tensor_tensor(out=hn[t3][:, :], in0=x2t[t3][:, :], scalar=st1[:, 0:1], in1=gffn[:, :],
                                                                 op0=ALU.mult, op1=ALU.mult),
                         reads=(f"x2t{t3}", f"x2t{t3}r", "C:W"), writes=(f"hn{t3}",))
                    P.op("act", lambda e: e.copy(out=hnb[:, :], in_=hn[t3][:, :]), reads=(f"hn{t3}",), writes=("hnb",))

                    def tr_h(e):
                        ins = None
                        for c in range(8):
                            ins = e.transpose(ps_T[:, c, :], hnb[:, 128 * c:128 * (c + 1)], ident[:, :])
                        return ins
                    P.op("pe", tr_h, reads=("hnb", "C:W"), writes=("p_T",))
                    P.op("act", lambda e: e.copy(out=hnT[:, :, :], in_=ps_T[:, :, :]), reads=("p_T",), writes=("hnT",))
                    for qg in range(4):
                        pj = qg % 2

                        def mm_pq(e, qg=qg, pj=pj):
                            ins = None
                            for q4 in range(4):
                                hp = qg * 4 + q4
                                for c in range(8):
                                    ins = e.matmul(ps_q[pj][:, 128 * q4:128 * (q4 + 1)], lhsT=wq[:, c, 128 * hp:128 * (hp + 1)],
                                                   rhs=hnT[:, c, :], start=(c == 0), stop=(c == 7))
                            return ins
                        P.op("pe", mm_pq, reads=("hnT", "C:W"), writes=(f"p_q{pj}",))
                        P.op("act", lambda e, qg=qg, pj=pj: e.copy(out=qTp[:, 4 * qg:4 * qg + 4, :],
                                                                  in_=ps_q[pj][:, :].rearrange("p (a b) -> p a b", a=4)),
                             reads=(f"p_q{pj}",), writes=(f"qTp.{qg}",))
                    for qg in range(4):
                        def mm_ps(e, qg=qg):
                            ins = None
                            for q4 in range(4):
                                hp = qg * 4 + q4
                                ins = e.matmul(ps_Sc[qg % 2][:, 128 * q4:128 * (q4 + 1)], lhsT=qTp[:, hp, :], rhs=skT[:, hp, :],
                                               start=True, stop=True)
                            return ins
                        P.op("pe", mm_ps, reads=(f"qTp.{qg}", "C:W"), writes=(f"p_S{qg % 2}",))
                        P.op("act", lambda e, qg=qg: e.copy(out=S_sb[:, 4 * qg:4 * qg + 4, :],
                                                           in_=ps_Sc[qg % 2][:, :].rearrange("p (a b) -> p a b", a=4)),
                             reads=(f"p_S{qg % 2}",), writes=(f"{sk_}.{qg}",))
                    P.mark()
                    for hp in range(16):
                        kq = f"{sk_}.{hp // 4}"
                        P.op("dve", lambda e, hp=hp: e.max(out=tops[:, hp, 0:8], in_=S_sb[:, hp, :]), reads=(kq,), writes=(f"top{hp}a",))
                        P.op("dve", lambda e, hp=hp: e.match_replace(out=S2[:, hp, :], in_to_replace=tops[:, hp, 0:8], in_values=S_sb[:, hp, :],
                                                                    imm_value=-1e30),
                             reads=(kq, f"top{hp}a"), writes=(f"S2.{hp}",))
                        P.op("dve", lambda e, hp=hp: e.max(out=tops[:, hp, 8:16], in_=S2[:, hp, :]), reads=(f"S2.{hp}",), writes=(f"top{hp}b",))
                        P.op("dve", lambda e, hp=hp: e.max_index(out=idxu[:, hp, 0:8], in_max=tops[:, hp, 0:8], in_values=S_sb[:, hp, :]),
                             reads=(kq, f"top{hp}a"), writes=(f"idx{hp}a",))
                        P.op("dve", lambda e, hp=hp: e.max_index(out=idxu[:, hp, 8:16], in_max=tops[:, hp, 8:16], in_values=S_sb[:, hp, :]),
                             reads=(kq, f"top{hp}b"), writes=(f"idx{hp}b",))
                    allidx = tuple(f"idx{hp}{x}" for hp in range(16) for x in "ab")
                    alltop = tuple(f"top{hp}{x}" for hp in range(16) for x in "ab")
                    P.op("dve", lambda e: e.tensor_copy(out=idxf[:, :, :], in_=idxu[:, :, :]), reads=allidx, writes=("idxf",))
                    topv = tops[:, :, :].rearrange("p (h t) k -> p h t k", t=2)
                    P.op("dve", lambda e: e.tensor_tensor(
                        out=cand[:, :, :].rearrange("p h (a b) -> p h a b", a=16),
                        in0=topv[:, :, 0, :].unsqueeze(3).to_broadcast([128, 8, 16, 16]),
                        in1=topv[:, :, 1, :].unsqueeze(2).to_broadcast([128, 8, 16, 16]), op=ALU.add),
                        reads=alltop, writes=("cand",))
                    for h in range(8):
                        P.op("dve", lambda e, h=h: e.max(out=best[:, h, 0:8], in_=cand[:, h, :]), reads=("cand",), writes=(f"best{h}a",))
                        P.op("dve", lambda e, h=h: e.match_replace(out=cand2[:, h, :], in_to_replace=best[:, h, 0:8], in_values=cand[:, h, :],
                                                                  imm_value=-1e30),
                             reads=("cand", f"best{h}a"), writes=(f"cand2.{h}",))
                        P.op("dve", lambda e, h=h: e.max(out=best[:, h, 8:16], in_=cand2[:, h, :]), reads=(f"cand2.{h}",), writes=(f"best{h}b",))
                        P.op("dve", lambda e, h=h: e.max_index(out=posu[:, h, 0:8], in_max=best[:, h, 0:8], in_values=cand[:, h, :]),
                             reads=("cand", f"best{h}a"), writes=(f"pos{h}a",))
                        P.op("dve", lambda e, h=h: e.max_index(out=posu[:, h, 8:16], in_max=best[:, h, 8:16], in_values=cand[:, h, :]),
                             reads=("cand", f"best{h}b"), writes=(f"pos{h}b",))
                    allpos = tuple(f"pos{h}{x}" for h in range(8) for x in "ab")
                    allbest = tuple(f"best{h}{x}" for h in range(8) for x in "ab")
                    gi = tI % 2
                    P.op("dve", lambda e, gi=gi: e.tensor_tensor(out=gate[gi][:, :, :], in0=best[:, :, :],
                                                                in1=best[:, :, 0:1].to_broadcast([128, 8, 16]), op=ALU.subtract),
                         reads=allbest, writes=(f"gate{gi}",))
                    P.op("act", lambda e, gi=gi: e.activation(out=gate[gi][:, :, :], in_=gate[gi][:, :, :], func=AF.Exp),
                         reads=(f"gate{gi}",), writes=(f"gate{gi}",))
                    P.op("dve", lambda e, gi=gi: e.tensor_reduce(out=gsum[:, :, :], in_=gate[gi][:, :, :], axis=AX.X, op=ALU.add),
                         reads=(f"gate{gi}",), writes=("gsum",))
                    P.op("dve", lambda e: e.reciprocal(out=gsum[:, :, :], in_=gsum[:, :, :]), reads=("gsum",), writes=("gsum",))
                    P.op("dve", lambda e, gi=gi: e.tensor_tensor(out=gate[gi][:, :, :], in0=gate[gi][:, :, :],
                                                                in1=gsum[:, :, :].to_broadcast([128, 8, 16]), op=ALU.mult),
                         reads=(f"gate{gi}", "gsum"), writes=(f"gate{gi}",))
                    P.op("dve", lambda e: e.tensor_copy(out=posf[:, :, :], in_=posu[:, :, :]), reads=allpos, writes=("posf",))
                    P.op("dve", lambda e: e.tensor_tensor(
                        out=eq[:, :, :], in0=posf[:, :, :].rearrange("p h k -> p (h k)").unsqueeze(2).to_broadcast([128, 128, 16]),
                        in1=thr16[:, :].unsqueeze(1).to_broadcast([128, 128, 16]), op=ALU.is_ge),
                        reads=("posf", "C:W"), writes=("eq",))
                    P.op("dve", lambda e: e.tensor_reduce(out=pa_f[:, :, :].rearrange("p h k -> p (h k)"), in_=eq[:, :, :], axis=AX.X, op=ALU.add),
                         reads=("eq",), writes=("pa_f",))
                    P.op("dve", lambda e: e.scalar_tensor_tensor(out=pb_f[:, :, :], in0=pa_f[:, :, :], scalar=-16.0, in1=posf[:, :, :],
                                                                 op0=ALU.mult, op1=ALU.add),
                         reads=("pa_f", "posf"), writes=("pb_f",))
                    idxv = idxf[:, :, :].rearrange("p (h t) k -> p h t k", t=2)
                    for (pf, half, dstk, dst) in ((pa_f, 0, "i1s", i1s), (pb_f, 1, "i2s", i2s)):
                        pk = "pa_f" if half == 0 else "pb_f"
                        P.op("dve", lambda e, pf=pf: e.tensor_tensor(
                            out=eq[:, :, :], in0=pf[:, :, :].rearrange("p h k -> p (h k)").unsqueeze(2).to_broadcast([128, 128, 16]),
                            in1=iota16[:, :].unsqueeze(1).to_broadcast([128, 128, 16]), op=ALU.is_equal),
                            reads=(pk, "C:W"), writes=("eq",))
                        P.op("dve", lambda e, half=half: e.tensor_tensor(
                            out=eq[:, :, :].rearrange("p (h k) a -> p h k a", h=8),
                            in0=eq[:, :, :].rearrange("p (h k) a -> p h k a", h=8),
                            in1=idxv[:, :, half, :].unsqueeze(2).to_broadcast([128, 8, 16, 16]), op=ALU.mult),
                            reads=("eq", "idxf"), writes=("eq",))
                        P.op("dve", lambda e, dst=dst: e.tensor_reduce(out=dst[:, :], in_=eq[:, :, :], axis=AX.X, op=ALU.add),
                             reads=("eq",), writes=(dstk,))
                    P.op("dve", lambda e: e.scalar_tensor_tensor(out=expf[:, :], in0=i1s[:, :], scalar=128.0, in1=i2s[:, :], op0=ALU.mult, op1=ALU.add),
                         reads=("i1s", "i2s"), writes=("expf",))
                    P.op("dve", lambda e, gi=gi: e.tensor_copy(out=expu[gi][:, :], in_=expf[:, :]), reads=("expf",), writes=(f"expu{gi}",))

                def body4b(tI):
                    ti = tI % 2
                    gi = tI % 2
                    t3 = tI % 3
                    t0 = 128 * tI
                    for k in range(nslot):
                        gcount[0] += 1
                        b = gcount[0] % NB
                        db = gcount[0] % NDG
                        P.dma("pool", lambda e, b=b, k=k, gi=gi: e.indirect_dma_start(
                            out=gbuf[b][:, :], out_offset=None, in_=uv16_d,
                            in_offset=bass.IndirectOffsetOnAxis(ap=expu[gi][:, k:k + 1], axis=0)),
                            dsem(f"gb{b}", "pool"), reads=(f"expu{gi}",), writes=(f"gbuf{b}",))
                        ji = nj()
                        P.op("dve", lambda e, b=b, k=k, t3=t3, ji=ji: e.scalar_tensor_tensor(
                            out=junkr[ji][:, :], in0=gbuf[b][:, 0:1024], scalar=1.0, in1=hn[t3][:, :], op0=ALU.mult, op1=ALU.mult,
                            accum_out=aval[:, k:k + 1]),
                            reads=(f"gbuf{b}", f"hn{t3}"), writes=(f"av{k}", f"junk{ji}"))
                        P.op("act", lambda e, k=k: e.activation(out=gval[:, k:k + 1], in_=aval[:, k:k + 1], func=AF.Gelu),
                             reads=(f"av{k}",), writes=(f"gv{k}",))
                        P.op("act", lambda e, k=k, gi=gi: e.mul(out=wval[:, k:k + 1], in_=gval[:, k:k + 1],
                                                               mul=gate[gi][:, :, :].rearrange("p h k -> p (h k)")[:, k:k + 1]),
                             reads=(f"gv{k}", f"gate{gi}"), writes=(f"wv{k}",))
                        P.op("act", lambda e, k=k, db=db: e.activation(out=dg[db][:, :], in_=ident_f[:, :], func=AF.Copy,
                                                                      scale=wval[:, k:k + 1]),
                             reads=(f"wv{k}", "C:W"), writes=(f"dg{db}",))

                        def mm_acc(e, k=k, b=b, db=db):
                            ins = None
                            for hh in range(2):
                                ins = e.matmul(ps_acc[hh][:, :], lhsT=dg[db][:, :], rhs=gbuf[b][:, 1024 + 512 * hh:1024 + 512 * (hh + 1)],
                                               start=(k == 0), stop=(k == nslot - 1))
                            return ins
                        P.op("pe", mm_acc, reads=(f"dg{db}", f"gbuf{b}"), writes=("p_acc",))
                    for hh in range(2):
                        P.op("dve", lambda e, t3=t3, hh=hh: e.tensor_tensor(out=x3[:, 512 * hh:512 * (hh + 1)], in0=ps_acc[hh][:, :],
                                                                           in1=x2t[t3][:, 512 * hh:512 * (hh + 1)], op=ALU.add),
                             reads=(f"x2t{t3}", "p_acc"), writes=(f"x3.{hh}",))
                    P.op("act", lambda e, ti=ti: e.activation(out=yo[ti][:, :], in_=x3[:, :], func=AF.Square, accum_out=st2[:, :]),
                         reads=("x3.0", "x3.1"), writes=("st2", f"yo{ti}"))
                    P.op("dve", lambda e: e.tensor_scalar(out=st2[:, :], in0=st2[:, :], scalar1=1.0 / D, scalar2=EPS, op0=ALU.mult, op1=ALU.add),
                         reads=("st2",), writes=("st2",))
                    P.op("act", lambda e: e.activation(out=st2[:, :], in_=st2[:, :], func=AF.Ln), reads=("st2",), writes=("st2",))
                    P.op("act", lambda e: e.activation(out=st2[:, :], in_=st2[:, :], func=AF.Exp, scale=-0.5), reads=("st2",), writes=("st2",))
                    P.op("dve", lambda e, ti=ti: e.scalar_tensor_tensor(out=yo[ti][:, :], in0=x3[:, :], scalar=st2[:, 0:1], in1=gfin[:, :],
                                                                       op0=ALU.mult, op1=ALU.mult),
                         reads=("x3.0", "x3.1", "st2", "C:W"), writes=(f"yo{ti}",))
                    P.dma("sp", lambda e, ti=ti, t0=t0: e.dma_start(out=y_h[t0:t0 + 128, :], in_=yo[ti][:, :]),
                          dsem(f"yo{ti}"), reads=(f"yo{ti}",), writes=())

                recA = [P.split(P.capture(body4a, t_)) for t_ in range(ntile)]
                recB = [P.capture(body4b, t_) for t_ in range(ntile)]
                P.pipeline([recA[t_] + [recB[t_]] for t_ in range(ntile)], spans=[0.7, 0.7, 1.0])
                P.drain_dma("sp")
                P.drain_dma("pool")
                P.emit(nc, "p3")
    return nc


def _seg_tokens():
    out = np.zeros((NSEG, 384), np.int64)
    kk = np.arange(384)
    for s in range(NSEG):
        g, sp_ = s // 16, s % 16
        d = DIL[g]
        nsub = 16 // d
        r, sub = sp_ // nsub, sp_ % nsub
        m = 256 * sub - 64 + kk
        out[s] = d * m + r
    return out


def prepare_inputs(inp):
    f = lambda a: np.ascontiguousarray(np.asarray(a, dtype=np.float32))
    x = f(inp["x"])
    lay = lambda w, nch: np.ascontiguousarray(w.reshape(nch, 128, -1).transpose(1, 0, 2))
    shared = {
        "w_in_l": lay(f(inp["w_in"])[0], 8),
        "gmix": np.ascontiguousarray(f(inp["norm_mix_g"])[0].reshape(8, 128).T),
        "gsgu_bc": np.ascontiguousarray(np.broadcast_to(f(inp["sgu_norm_g"])[0][None, :], (128, 1024))),
        "wsT": np.ascontiguousarray(f(inp["sgu_w"])[0].transpose(2, 0, 1)),
        "bsgu_bc": np.ascontiguousarray(np.broadcast_to(f(inp["sgu_b"])[0].reshape(1, 1024), (128, 1024))),
        "wa_l": lay(f(inp["w_branch_a"])[0], 8),
        "wb_l": lay(f(inp["w_branch_b"])[0], 4),
        "wo_l": lay(f(inp["w_out"])[0], 8),
        "gffn_bc": np.ascontiguousarray(np.broadcast_to(f(inp["norm_ffn_g"])[0][None, :], (128, 1024))),
        "gfin_bc": np.ascontiguousarray(np.broadcast_to(f(inp["norm_final_g"])[None, :], (128, 1024))),
        "wq_l": lay(f(inp["peer_wq"])[0], 8),
        "skT": np.ascontiguousarray(f(inp["peer_subkeys"])[0].reshape(16, 128, 128).transpose(2, 0, 1)),
        "uv_tab": np.ascontiguousarray(np.concatenate([f(inp["peer_u"])[0], f(inp["peer_v"])[0]], axis=1)),
        "ident": np.eye(128, dtype=np.float32),
        "iota16": np.ascontiguousarray(np.broadcast_to(np.arange(16, dtype=np.float32)[None, :], (128, 16))),
        "thr16": np.ascontiguousarray(np.broadcast_to((16.0 * np.arange(1, 17, dtype=np.float32))[None, :], (128, 16))),
    }
    jl = np.arange(128)[:, None]
    il = np.arange(128)[None, :]
    relA = jl - il - 64
    relB = jl - il + 64
    A = np.where(np.abs(relA) <= 64, -np.abs(relA), -1e9).astype(np.float32)
    Bm = np.where(np.abs(relB) <= 64, -np.abs(relB), -1e9).astype(np.float32)
    shared["negabs"] = np.ascontiguousarray(np.concatenate([A, Bm, A, Bm], axis=1))
    segtok = _seg_tokens()
    maps = []
    for c in range(8):
        b, hf = c // 2, c % 2
        T0 = hf * NTOK
        gtok = segtok + T0
        valid = (gtok >= 0) & (gtok < SEQ)
        xg = x[b][np.clip(gtok, 0, SEQ - 1)]
        xg = xg * valid[..., None].astype(np.float32) if False else np.where(valid[..., None], xg, np.float32(0))
        xs = np.ascontiguousarray(xg.reshape(NSEG, 384, 8, 128).transpose(0, 3, 2, 1))
        kv = valid.reshape(NSEG, 3, 128).transpose(2, 0, 1).reshape(128, NSEG * 3).astype(np.float32)
        m = dict(shared)
        m["xseg"] = xs
        m["kval"] = np.ascontiguousarray(kv)
        m["xtok"] = np.ascontiguousarray(x[b, T0:T0 + NTOK])
        maps.append(m)
    return maps


def kernel(**inputs):
    maps = prepare_inputs(inputs)
    nc = build_program()
    res = run_bass_kernel_spmd(nc, maps, core_ids=list(range(8)))
    out = np.zeros((4, SEQ, D), np.float32)
    for c in range(8):
        b, hf = c // 2, c % 2
        out[b, hf * NTOK:(hf + 1) * NTOK] = res.results[c]["y"]
    return out
```

```python
from contextlib import ExitStack
import numpy as np
import concourse.bass as bass
import concourse.mybir as mybir
from concourse.bass_utils import run_bass_kernel_spmd

F32 = mybir.dt.float32
BF16 = mybir.dt.bfloat16
U32 = mybir.dt.uint32
AF = mybir.ActivationFunctionType
ALU = mybir.AluOpType
AX = mybir.AxisListType

D = 1024
SEQ = 8192
NTOK = 4096
NSEG = 48
EPS = 1e-6
DIL = (1, 4, 16)
ENG = ("pe", "act", "dve", "pool", "sp")


class Prog:
    def __init__(self, semobj):
        self.semobj = semobj
        self.cnt = {e: 0 for e in ENG}
        self.dcnt = {}
        self.q = {e: [] for e in ENG}
        self.lastw = {}
        self.readers = {}
        self.known = {e: {} for e in ENG}
        self._rec = None

    def capture(self, fn, *args):
        saved = self._rec
        self._rec = []
        fn(*args)
        rec, self._rec = self._rec, saved
        return rec

    def replay(self, rec):
        for kind, a in rec:
            if kind == "op":
                self.op(*a)
            else:
                self.dma(*a)

    def mark(self):
        if self._rec is not None:
            self._rec.append(("mark", None))

    @staticmethod
    def split(rec):
        out = [[]]
        for it in rec:
            if it[0] == "mark":
                out.append([])
            else:
                out[-1].append(it)
        return out

    @staticmethod
    def interleave_n(lists, spans=None):
        items = []
        for li, L in enumerate(lists):
            n = len(L)
            f = 1.0 if spans is None else spans[li]
            for i, it in enumerate(L):
                items.append(((i + 0.5) / n * f, li, i, it))
        items.sort(key=lambda t: (t[0], t[1], t[2]))
        return [t[3] for t in items]

    def pipeline(self, stages_by_item, spans=None):
        n = len(stages_by_item)
        S = max(len(x) for x in stages_by_item)
        for step in range(n + S - 1):
            sts = [st for st in range(S - 1, -1, -1) if 0 <= step - st < n and st < len(stages_by_item[step - st])]
            lists = [stages_by_item[step - st][st] for st in sts]
            self.replay(self.interleave_n(lists, None if spans is None else [spans[st] for st in sts]))

    @staticmethod
    def interleave(A, B):
        out = []
        na, nb = len(A), len(B)
        ia = ib = 0
        while ia < na or ib < nb:
            if ib >= nb or (ia < na and ia * nb <= ib * na):
                out.append(A[ia])
                ia += 1
            else:
                out.append(B[ib])
                ib += 1
        return out

    def _deps(self, eng, reads, writes):
        need = {}

        def add(tok):
            if tok is None:
                return
            s, v = tok
            if need.get(s, 0) < v:
                need[s] = v

        def addw(k):
            for s, v in self.lastw.get(k, {}).items():
                add((s, v))
        for k in reads:
            addw(k)
        for k in writes:
            addw(k)
            for t in self.readers.get(k, ()):
                add(t)
        out = []
        kn = self.known[eng]
        for s, v in need.items():
            if kn.get(s, 0) < v:
                kn[s] = v
                out.append((s, v))
        return out

    def _commit(self, tok, reads, writes):
        for k in reads:
            if not k.startswith("C:"):
                self.readers.setdefault(k, []).append(tok)
        for k in writes:
            if k.startswith("C:"):
                self.lastw.setdefault(k, {})[tok[0]] = tok[1]
            else:
                self.lastw[k] = {tok[0]: tok[1]}
            self.readers[k] = []

    def op(self, eng, fn, reads=(), writes=()):
        if self._rec is not None:
            self._rec.append(("op", (eng, fn, reads, writes)))
            return None
        waits = self._deps(eng, reads, writes)
        self.cnt[eng] += 1
        tok = (eng, self.cnt[eng])
        self.q[eng].append((waits, fn, (eng, 1)))
        self._commit(tok, reads, writes)
        return tok

    def dma(self, eng, fn, semkey, reads=(), writes=()):
        if self._rec is not None:
            self._rec.append(("dma", (eng, fn, semkey, reads, writes)))
            return None
        waits = self._deps(eng, reads, writes)
        self.dcnt[semkey] = self.dcnt.get(semkey, 0) + 16
        tok = (semkey, self.dcnt[semkey])
        self.q[eng].append((waits, fn, (semkey, 16)))
        self._commit(tok, reads, writes)
        return tok

    def drain_dma(self, eng="sp"):
        waits = []
        for s, v in self.dcnt.items():
            if self.known[eng].get(s, 0) < v:
                self.known[eng][s] = v
                waits.append((s, v))
        if waits:
            self.q[eng].append((waits, None, None))

    def emit(self, nc, name):
        q = self.q
        semobj = self.semobj

        def run(eng_name):
            def f(e):
                for waits, fn, inc in q[eng_name]:
                    for s, v in waits:
                        e.wait_ge(semobj[s], v)
                    if fn is not None:
                        ins = fn(e)
                        ins.then_inc(semobj[inc[0]], inc[1])
            return f
        with nc.Block() as block:
            block.tensor(run("pe"))
            block.scalar(run("act"))
            block.vector(run("dve"))
            block.gpsimd(run("pool"))
            block.sync(run("sp"))
        self.q = {e: [] for e in ENG}
        self.lastw = {}
        self.readers = {}


def _slopes():
    return [2.0 ** (-(h + 1)) for h in range(8)]


def build_program(cfg=None):
    cfg = cfg or {}
    segs = cfg.get("segs", list(range(NSEG)))
    nseg = len(segs)
    nblk = cfg.get("nblk", 16)
    ntile = cfg.get("ntile", 32)
    nslot = cfg.get("nslot", 128)
    debug = cfg.get("debug", False)
    pool_rms = cfg.get("pool_rms", 4)
    scratch_kind = "ExternalOutput" if debug else "Internal"

    nc = bass.Bass("TRN2", target_bir_lowering=False)
    dt = nc.dram_tensor
    xseg = dt("xseg", [NSEG, 128, 8, 384], F32, kind="ExternalInput").ap()
    kval_h = dt("kval", [128, NSEG * 3], F32, kind="ExternalInput").ap()
    xtok_h = dt("xtok", [NTOK, D], F32, kind="ExternalInput").ap()
    w_in_h = dt("w_in_l", [128, 8, 6656], F32, kind="ExternalInput").ap()
    gmix_h = dt("gmix", [128, 8], F32, kind="ExternalInput").ap()
    gsgu_h = dt("gsgu_bc", [128, 1024], F32, kind="ExternalInput").ap()
    wsT_h = dt("wsT", [128, 8, 128], F32, kind="ExternalInput").ap()
    bsbc_h = dt("bsgu_bc", [128, 1024], F32, kind="ExternalInput").ap()
    wa_h = dt("wa_l", [128, 8, 1024], F32, kind="ExternalInput").ap()
    wb_h = dt("wb_l", [128, 4, 1024], F32, kind="ExternalInput").ap()
    wo_h = dt("wo_l", [128, 8, 1024], F32, kind="ExternalInput").ap()
    gffn_h = dt("gffn_bc", [128, 1024], F32, kind="ExternalInput").ap()
    gfin_h = dt("gfin_bc", [128, 1024], F32, kind="ExternalInput").ap()
    wq_h = dt("wq_l", [128, 8, 2048], F32, kind="ExternalInput").ap()
    skT_h = dt("skT", [128, 16, 128], F32, kind="ExternalInput").ap()
    uv_h = dt("uv_tab", [16384, 2048], F32, kind="ExternalInput").ap()
    negabs_h = dt("negabs", [128, 512], F32, kind="ExternalInput").ap()
    ident_h = dt("ident", [128, 128], F32, kind="ExternalInput").ap()
    iota_h = dt("iota16", [128, 16], F32, kind="ExternalInput").ap()
    thr_h = dt("thr16", [128, 16], F32, kind="ExternalInput").ap()
    y_h = dt("y", [NTOK, D], F32, kind="ExternalOutput").ap()
    att_d = dt("att_d", [3, NTOK, 520], F32, kind=scratch_kind).ap()
    at_d = dt("at_d", [128, 8, NTOK], F32, kind=scratch_kind).ap()
    x2_d = dt("x2_d", [NTOK, D], F32, kind=scratch_kind).ap()
    uv16_d = dt("uv16_d", [16384, 2048], BF16, kind="Internal").ap()
    rstd_d = dt("rstd_d", [128, NTOK], F32, kind="Internal").ap()

    slopes = _slopes()

    with ExitStack() as top:
        semobj = {}
        for e in ENG:
            semobj[e] = top.enter_context(nc.semaphore("s_" + e))

        def dsem(name, eng="sp"):
            name = name + "@" + eng
            if name not in semobj:
                semobj[name] = top.enter_context(nc.semaphore("d_" + name))
            return name
        P = Prog(semobj)

        def load_cast(dst, src, eng="pool", sem="w", piece=1024):
            n = src.shape[-1]
            pre = (slice(None),) * (len(src.shape) - 1)
            for a in range(0, n, piece):
                b = min(n, a + piece)
                P.dma(eng, (lambda e, d_=dst[pre + (slice(a, b),)], s_=src[pre + (slice(a, b),)]: e.dma_start(out=d_, in_=s_)),
                      dsem(sem, eng), writes=("C:W",))

        def rmsnorm_block(es_bufs, src_ap, bi, n, tagp, rs_src=None, rs_loaded=False):
            xst, sq, rstd, xn, ps_ss, gmix, ones_bf, rtmp = es_bufs[:8]
            mhalf = None
            kx = f"{tagp}xst{bi}"
            if src_ap is not None:
                P.dma("sp", lambda e: e.dma_start(out=xst[bi][:, :, 0:n], in_=src_ap), dsem(kx), writes=(kx,))
                if rs_src is not None:
                    P.dma("sp", lambda e: e.dma_start(out=rstd[bi][:, 0:n], in_=rs_src), dsem(f"{tagp}rsl{bi}"),
                          writes=(f"{tagp}rs{bi}",))
                return None
            if rs_loaded:
                return scale_chunks(es_bufs, bi, n, tagp)
            P.op("act", lambda e: e.activation(out=sq[bi][:, :, 0:n], in_=xst[bi][:, :, 0:n], func=AF.Square),
                 reads=(kx,), writes=(f"{tagp}sq{bi}",))

            def mm_ss(e):
                ins = None
                for c in range(8):
                    ins = e.matmul(ps_ss[:, 0:n], lhsT=ones_bf[:, :], rhs=sq[bi][:, c, 0:n], start=(c == 0), stop=(c == 7))
                return ins
            P.op("pe", mm_ss, reads=(f"{tagp}sq{bi}", "C:W", "C:ones"), writes=(f"{tagp}ps_ss",))
            P.op("dve", lambda e: e.tensor_scalar(out=rstd[bi][:, 0:n], in0=ps_ss[:, 0:n], scalar1=1.0 / D, scalar2=EPS,
                                                  op0=ALU.mult, op1=ALU.add),
                 reads=(f"{tagp}ps_ss",), writes=(f"{tagp}rs{bi}",))
            if mhalf is not None:
                P.op("pool", lambda e: e.tensor_tensor(out=rstd[bi][:, 0:n], in0=rstd[bi][:, 0:n], in1=mhalf[:, 0:n], op=ALU.pow),
                     reads=(f"{tagp}rs{bi}", "C:mhalf"), writes=(f"{tagp}rs{bi}",))
            else:
                P.op("act", lambda e: e.activation(out=rstd[bi][:, 0:n], in_=rstd[bi][:, 0:n], func=AF.Ln),
                     reads=(f"{tagp}rs{bi}",), writes=(f"{tagp}rs{bi}",))
                P.op("act", lambda e: e.activation(out=rstd[bi][:, 0:n], in_=rstd[bi][:, 0:n], func=AF.Exp, scale=-0.5),
                     reads=(f"{tagp}rs{bi}",), writes=(f"{tagp}rs{bi}",))
            return scale_chunks(es_bufs, bi, n, tagp)

        def scale_chunks(es_bufs, bi, n, tagp):
            xst, sq, rstd, xn, ps_ss, gmix, ones_bf, rtmp = es_bufs[:8]
            kx = f"{tagp}xst{bi}"
            for c in range(8):
                if c < pool_rms:
                    tb = c % 2
                    P.op("pool", lambda e, c=c, tb=tb: e.tensor_tensor(
                        out=rtmp[tb][:, 0:n], in0=xst[bi][:, c, 0:n], in1=rstd[bi][:, 0:n], op=ALU.mult),
                        reads=(kx, f"{tagp}rs{bi}"), writes=(f"{tagp}rtmp{tb}",))
                    P.op("pool", lambda e, c=c, tb=tb: e.tensor_scalar(
                        out=xn[bi][:, c, 0:n], in0=rtmp[tb][:, 0:n], scalar1=gmix[:, c:c + 1], scalar2=1.0,
                        op0=ALU.mult, op1=ALU.mult),
                        reads=(f"{tagp}rtmp{tb}", "C:W"), writes=(f"{tagp}xn{bi}.{c}",))
                    continue
                P.op("dve", lambda e, c=c: e.scalar_tensor_tensor(
                    out=xn[bi][:, c, 0:n], in0=xst[bi][:, c, 0:n], scalar=gmix[:, c:c + 1], in1=rstd[bi][:, 0:n],
                    op0=ALU.mult, op1=ALU.mult),
                    reads=(kx, f"{tagp}rs{bi}", "C:W"), writes=(f"{tagp}xn{bi}.{c}",))
            return [f"{tagp}xn{bi}.{c}" for c in range(8)]

        if nseg > 0:
            with ExitStack() as es:
                sb = lambda name, shape, dtp: es.enter_context(nc.sbuf_tensor("sb_" + name, shape, dtp))
                ps = lambda name, shape, dtp=F32: es.enter_context(nc.psum_tensor("pp_" + name, shape, dtp))
                wqkv = sb("wqkv", [128, 8, 2560], BF16)
                gmix = sb("gmix1", [128, 8], F32)
                negabs = sb("negabs", [128, 512], F32)
                ones_bf = sb("ones1", [128, 128], BF16)
                ones8 = sb("ones8", [128, 8, 1], F32)
                kval = sb("kval", [128, NSEG * 3], F32)
                xst = [sb(f"xst{i}", [128, 8, 384], F32) for i in range(2)]
                sq = [sb(f"sq{i}", [128, 8, 384], BF16) for i in range(2)]
                rstd = [sb(f"rstd{i}", [128, 384], F32) for i in range(2)]
                xn = [sb(f"xn{i}", [128, 8, 384], BF16) for i in range(2)]
                kT = [sb(f"kT{i}", [128, 4, 384], BF16) for i in range(2)]
                qT = [sb(f"qT{i}", [128, 4, 256], BF16) for i in range(2)]
                vaug = [sb(f"vaug{i}", [128, 3, 8, 65], BF16) for i in range(2)]
                tt = [sb(f"tt{i}", [128, 512], F32) for i in range(2)]
                pT = [sb(f"pT{i}", [128, 512], BF16) for i in range(2)]
                osb = [sb(f"osb{i}", [128, 2, 520], F32) for i in range(2)]
                cst = [sb(f"cst{i}", [128, 2, 2048], BF16) for i in range(2)]
                castn = [0 if cfg.get("cast", True) else 64]
                ps_ss1 = ps("ps_ss1", [128, 512])
                ps_pr = [ps(f"ps_pr{i}", [128, 512]) for i in range(2)]
                ps_S = [ps(f"ps_S{i}", [128, 512]) for i in range(2)]
                ps_O = [ps(f"ps_O{i}", [128, 512]) for i in range(3)]
                ocnt = [0]

                load_cast(wqkv[:, :, :], w_in_h[:, :, 2048:4608], piece=640)
                P.dma("sp", lambda e: e.dma_start(out=gmix[:, :], in_=gmix_h), dsem("w"), writes=("C:W",))
                P.dma("sp", lambda e: e.dma_start(out=negabs[:, :], in_=negabs_h), dsem("w"), writes=("C:W",))
                P.dma("sp", lambda e: e.dma_start(out=kval[:, :], in_=kval_h), dsem("w"), writes=("C:W",))
                P.op("dve", lambda e: e.memset(ones_bf[:, :], 1.0), writes=("C:ones",))
                P.op("dve", lambda e: e.memset(ones8[:, :, :], 1.0), writes=("C:ones8",))

                prn = [0]

                def next_pr():
                    prn[0] += 1
                    return prn[0] % 2

                rtmp = [sb(f"rtmp{i}", [128, 384], F32) for i in range(2)]
                bufs1 = (xst, sq, rstd, xn, ps_ss1, gmix, ones_bf, rtmp)
                evn = [0]

                def evac_copy(out_ap, in_ap, reads, writes):
                    evn[0] += 1
                    if evn[0] % 2:
                        P.op("act", lambda e: e.copy(out=out_ap, in_=in_ap), reads=reads, writes=writes)
                    else:
                        P.op("dve", lambda e: e.tensor_copy(out=out_ap, in_=in_ap), reads=reads, writes=writes)

                def seg_geo(si, s):
                    g = s // 16
                    d = DIL[g]
                    sp_ = s % 16
                    nsub = 16 // d
                    return g, d, sp_ // nsub, sp_ % nsub, si % 2

                def cast_chunk(ci):
                    cb = ci % 2
                    srcv = uv_h[256 * ci:256 * (ci + 1), :].rearrange("(p r) c -> p r c", r=2)
                    dstv = uv16_d[256 * ci:256 * (ci + 1), :].rearrange("(p r) c -> p r c", r=2)
                    P.dma("pool", lambda e: e.dma_start(out=cst[cb][:, :, :], in_=srcv), dsem(f"cst{cb}", "pool"), writes=(f"cst{cb}",))
                    P.dma("sp", lambda e: e.dma_start(out=dstv, in_=cst[cb][:, :, :]), dsem(f"cso{cb}"), reads=(f"cst{cb}",))

                def body1a(si, s):
                    g, d, r, sub, bi = seg_geo(si, s)
                    if si == 0:
                        rmsnorm_block(bufs1, xseg[s], bi, 384, "a")
                    if si + 1 < nseg:
                        rmsnorm_block(bufs1, xseg[segs[si + 1]], (si + 1) % 2, 384, "a")
                    for _ in range(2 if si % 3 == 0 else 1):
                        if castn[0] < 64:
                            cast_chunk(castn[0])
                            castn[0] += 1
                    xk = rmsnorm_block(bufs1, None, bi, 384, "a")
                    P.mark()
                    if g == 0:
                        P.dma("sp", lambda e: e.dma_start(out=rstd_d[:, 256 * s:256 * (s + 1)], in_=rstd[bi][:, 64:320]),
                              dsem(f"rsd{bi}"), reads=(f"ars{bi}",))

                    for j in range(4):
                        pj = next_pr()

                        def mm_k(e, j=j, pj=pj):
                            ins = None
                            for c in range(8):
                                ins = e.matmul(ps_pr[pj][:, 0:384], lhsT=wqkv[:, c, 1536 + 128 * j:1536 + 128 * (j + 1)],
                                               rhs=xn[bi][:, c, :], start=(c == 0), stop=(c == 7))
                            return ins
                        P.op("pe", mm_k, reads=tuple(xk) + ("C:W",), writes=(f"ps_pr{pj}",))
                        evac_copy(kT[bi][:, j, :], ps_pr[pj][:, 0:384], (f"ps_pr{pj}",), (f"kT{bi}.{j}",))
                    for j in range(4):
                        pj = next_pr()

                        def mm_q(e, j=j, pj=pj):
                            ins = None
                            for c in range(8):
                                ins = e.matmul(ps_pr[pj][:, 0:256], lhsT=wqkv[:, c, g * 512 + 128 * j:g * 512 + 128 * (j + 1)],
                                               rhs=xn[bi][:, c, 64:320], start=(c == 0), stop=(c == 7))
                            return ins
                        P.op("pe", mm_q, reads=tuple(xk) + ("C:W",), writes=(f"ps_pr{pj}",))
                        evac_copy(qT[bi][:, j, :], ps_pr[pj][:, 0:256], (f"ps_pr{pj}",), (f"qT{bi}.{j}",))
                    for j in range(3):
                        pj = next_pr()

                        def mm_v(e, j=j, pj=pj):
                            ins = None
                            for c in range(8):
                                ins = e.matmul(ps_pr[pj][:, :], lhsT=xn[bi][:, c, 128 * j:128 * (j + 1)],
                                               rhs=wqkv[:, c, 2048:2560], start=(c == 0), stop=(c == 7))
                            return ins
                        P.op("pe", mm_v, reads=tuple(xk) + ("C:W",), writes=(f"ps_pr{pj}",))
                        col = 3 * s + j
                        P.op("act", lambda e, j=j, pj=pj, col=col: e.activation(
                            out=vaug[bi][:, j, :, 0:64], in_=ps_pr[pj][:, :].rearrange("p (h e) -> p h e", h=8),
                            func=AF.Copy, scale=kval[:, col:col + 1]),
                            reads=(f"ps_pr{pj}", "C:W"), writes=(f"va{bi}.{j}",))
                        P.op("dve", lambda e, j=j, col=col: e.tensor_scalar(
                            out=vaug[bi][:, j, :, 64:65], in0=ones8[:, :, :],
                            scalar1=kval[:, col:col + 1], scalar2=None, op0=ALU.mult),
                            reads=("C:W", "C:ones8"), writes=(f"vb{bi}.{j}",))

                def body1b(si, s):
                    g, d, r, sub, bi = seg_geo(si, s)
                    obm = {}

                    def front(h):
                        jc, pb = h // 2, 64 * (h % 2)
                        ri = h % 2
                        coef = slopes[h] * d * 8.0

                        def mm_s(e, jc=jc, pb=pb, ri=ri):
                            kk = kT[bi][pb:pb + 64, jc, :]
                            qq = qT[bi][pb:pb + 64, jc, :]
                            e.matmul(ps_S[ri][:, 0:128], lhsT=kk[:, 0:128], rhs=qq[:, 0:128], start=True, stop=True)
                            e.matmul(ps_S[ri][:, 128:384], lhsT=kk[:, 128:256], rhs=qq[:, 0:256], start=True, stop=True)
                            return e.matmul(ps_S[ri][:, 384:512], lhsT=kk[:, 256:384], rhs=qq[:, 128:256], start=True, stop=True)
                        P.op("pe", mm_s, reads=(f"kT{bi}.{jc}", f"qT{bi}.{jc}"), writes=(f"ps_S{ri}",))
                        P.op("dve", lambda e, ri=ri, coef=coef: e.scalar_tensor_tensor(
                            out=tt[ri][:, :], in0=negabs[:, :], scalar=coef, in1=ps_S[ri][:, :], op0=ALU.mult, op1=ALU.add),
                            reads=(f"ps_S{ri}", "C:W"), writes=(f"tt{ri}",))
                        P.op("act", lambda e, ri=ri: e.activation(out=pT[ri][:, :], in_=tt[ri][:, :], func=AF.Exp, scale=0.125),
                             reads=(f"tt{ri}",), writes=(f"pT{ri}",))

                    def back(h):
                        ri = h % 2
                        hg = h // 4
                        if h % 4 == 0:
                            for qt in range(2):
                                ocnt[0] += 1
                                obm[qt] = ocnt[0] % 3
                        for qt in range(2):
                            ob = obm[qt]
                            c0 = (h % 4) * 65

                            def mm_o(e, qt=qt, ob=ob, c0=c0, h=h, ri=ri):
                                e.matmul(ps_O[ob][:, c0:c0 + 65], lhsT=pT[ri][:, 256 * qt:256 * qt + 128],
                                         rhs=vaug[bi][:, qt, h, :], start=True, stop=False)
                                return e.matmul(ps_O[ob][:, c0:c0 + 65], lhsT=pT[ri][:, 256 * qt + 128:256 * qt + 256],
                                                rhs=vaug[bi][:, qt + 1, h, :], start=False, stop=True)
                            P.op("pe", mm_o, reads=(f"pT{ri}", f"va{bi}.{qt}", f"vb{bi}.{qt}", f"va{bi}.{qt + 1}", f"vb{bi}.{qt + 1}"),
                                 writes=(f"ps_O{ob}",))
                        if h % 4 == 3:
                            for qt in range(2):
                                ob = obm[qt]
                                evac_copy(osb[bi][:, qt, 260 * hg:260 * (hg + 1)], ps_O[ob][:, 0:260],
                                          (f"ps_O{ob}",), (f"osb{bi}.{qt}.{hg}",))

                    front(0)
                    for h in range(8):
                        if h + 1 < 8:
                            front(h + 1)
                        back(h)
                    P.mark()
                    for qt in range(2):
                        m0 = 256 * sub + 128 * qt
                        lo = d * m0 + r
                        dst = att_d[g][lo:lo + d * 127 + 1:d, :]
                        P.dma("sp", lambda e, dst=dst, qt=qt: e.dma_start(out=dst, in_=osb[bi][:, qt, :]),
                              dsem(f"osb{bi}.{qt}"), reads=(f"osb{bi}.{qt}.0", f"osb{bi}.{qt}.1"), writes=())
                recA = [P.split(P.capture(body1a, si_, s_)) for si_, s_ in enumerate(segs)]
                extras = []
                while castn[0] < 64:
                    extras.append(P.capture(cast_chunk, castn[0]))
                    castn[0] += 1
                recB = [P.split(P.capture(body1b, si_, s_)) for si_, s_ in enumerate(segs)]
                P.pipeline([recA[i] + recB[i] for i in range(nseg)], spans=[0.85, 0.85, 1.0, 1.0])
                for extra in extras:
                    P.replay(extra)
                P.drain_dma("sp")
                P.drain_dma("pool")
                P.emit(nc, "p1")

        if nblk > 0:
            with ExitStack() as es:
                sb = lambda name, shape, dtp: es.enter_context(nc.sbuf_tensor("sb_" + name, shape, dtp))
                ps = lambda name, shape, dtp=F32: es.enter_context(nc.psum_tensor("pp_" + name, shape, dtp))
                w_uv = sb("w_uv", [128, 8, 2048], BF16)
                w_a = sb("w_a", [128, 8, 1024], BF16)
                wsT = sb("wsT", [128, 8, 128], BF16)
                bsbc = sb("bsbc", [128, 1024], F32)
                mxb = [sb(f"mxb{i}", [128, 512], F32) for i in range(2)]
                gsgu = sb("gsgu", [128, 1024], F32)
                gmix = sb("gmix2", [128, 8], F32)
                ones_bf = sb("ones2", [128, 128], BF16)
                xst = [sb(f"bxst{i}", [128, 8, 256], F32) for i in range(2)]
                sq = [sb(f"bsq{i}", [128, 8, 256], BF16) for i in range(2)]
                rstd = [sb(f"brstd{i}", [128, 256], F32) for i in range(2)]
                xn = [sb(f"bxn{i}", [128, 8, 256], BF16) for i in range(2)]
                uT = [sb(f"uT{i}", [128, 8, 256], BF16) for i in range(2)]
                vg = [sb(f"vg{i}", [128, 1024], F32) for i in range(4)]
                junkr = [sb(f"junk{i}", [128, 1024], BF16) for i in range(4)]
                jn = [0]

                def nj():
                    jn[0] += 1
                    return jn[0] % 4
                ssv4 = sb("ssv4", [128, 4], F32)
                vn = [sb(f"vn{i}", [128, 1024], BF16) for i in range(4)]
                yaT = [sb(f"yaT{i}", [128, 8, 256], BF16) for i in range(2)]
                atsb = [sb(f"atsb{i}", [128, 8, 256], F32) for i in range(2)]
                ps_ss = ps("b_ss", [128, 512])
                ps_pr = [ps(f"b_pr{i}", [128, 512]) for i in range(2)]
                ps_mx = [ps(f"b_mx{i}", [128, 512]) for i in range(2)]
                ps_at = [ps(f"b_at{i}", [128, 512]) for i in range(2)]

                load_cast(w_uv[:, :, :], w_in_h[:, :, 0:2048], piece=1024)
                load_cast(w_a[:, :, :], wa_h[:, :, :], piece=1024)
                load_cast(wsT[:, :, :], wsT_h[:, :, :], piece=128)
                for dst_, src_ in ((bsbc[:, :], bsbc_h), (gsgu[:, :], gsgu_h), (gmix[:, :], gmix_h)):
                    P.dma("sp", lambda e, d_=dst_, s_=src_: e.dma_start(out=d_, in_=s_), dsem("w"), writes=("C:W",))
                P.op("dve", lambda e: e.memset(ones_bf[:, :], 1.0), writes=("C:ones",))
                prn = [0]
                rtmp = [sb(f"brtmp{i}", [128, 256], F32) for i in range(2)]
                bufs2 = (xst, sq, rstd, xn, ps_ss, gmix, ones_bf, rtmp)

                def body2(nb):
                    bi = nb % 2
                    if nb == 0:
                        rmsnorm_block(bufs2, xseg[nb][:, :, 64:320], bi, 256, "b", rs_src=rstd_d[:, 256 * nb:256 * (nb + 1)])
                    if nb + 1 < nblk:
                        rmsnorm_block(bufs2, xseg[nb + 1][:, :, 64:320], (nb + 1) % 2, 256, "b",
                                      rs_src=rstd_d[:, 256 * (nb + 1):256 * (nb + 2)])
                    xk = rmsnorm_block(bufs2, None, bi, 256, "b", rs_loaded=True)
                    P.mark()
                    for j in range(8):
                        prn[0] += 1
                        pj = prn[0] % 2

                        def mm_u(e, j=j, pj=pj):
                            ins = None
                            for c in range(8):
                                ins = e.matmul(ps_pr[pj][:, 0:256], lhsT=w_uv[:, c, 128 * j:128 * (j + 1)],
                                               rhs=xn[bi][:, c, :], start=(c == 0), stop=(c == 7))
                            return ins
                        P.op("pe", mm_u, reads=tuple(xk) + ("C:W",), writes=(f"b_pr{pj}",))
                        P.op("act", lambda e, j=j, pj=pj: e.activation(out=uT[bi][:, j, :], in_=ps_pr[pj][:, 0:256], func=AF.Gelu),
                             reads=(f"b_pr{pj}",), writes=(f"uT{bi}.{j}",))
                    for t2 in range(2):
                        vi = (nb * 2 + t2) % 4
                        for hh in range(2):
                            prn[0] += 1
                            pj = prn[0] % 2

                            def mm_v(e, t2=t2, hh=hh, pj=pj):
                                ins = None
                                for c in range(8):
                                    ins = e.matmul(ps_pr[pj][:, :], lhsT=xn[bi][:, c, 128 * t2:128 * (t2 + 1)],
                                                   rhs=w_uv[:, c, 1024 + 512 * hh:1024 + 512 * (hh + 1)],
                                                   start=(c == 0), stop=(c == 7))
                                return ins
                            P.op("pe", mm_v, reads=tuple(xk) + ("C:W",), writes=(f"b_pr{pj}",))
                            P.op("act", lambda e, hh=hh, pj=pj, vi=vi: e.activation(
                                out=vg[vi][:, 512 * hh:512 * (hh + 1)], in_=ps_pr[pj][:, :], func=AF.Gelu),
                                reads=(f"b_pr{pj}",), writes=(f"vg{vi}.{hh}",))
                        ji = nj()
                        P.op("dve", lambda e, vi=vi, ji=ji: e.scalar_tensor_tensor(
                            out=junkr[ji][:, :], in0=vg[vi][:, :], scalar=1.0, in1=vg[vi][:, :], op0=ALU.mult, op1=ALU.mult,
                            accum_out=ssv4[:, vi:vi + 1]),
                            reads=(f"vg{vi}.0", f"vg{vi}.1"), writes=(f"ssv{vi}", f"junk{ji}"))
                    v0 = (nb * 2) % 4
                    sk = (f"ssv{v0}", f"ssv{v0 + 1}")
                    P.op("dve", lambda e: e.tensor_scalar(out=ssv4[:, v0:v0 + 2], in0=ssv4[:, v0:v0 + 2], scalar1=1.0 / D, scalar2=EPS,
                                                          op0=ALU.mult, op1=ALU.add), reads=sk, writes=sk)
                    P.op("act", lambda e: e.activation(out=ssv4[:, v0:v0 + 2], in_=ssv4[:, v0:v0 + 2], func=AF.Ln), reads=sk, writes=sk)
                    P.op("act", lambda e: e.activation(out=ssv4[:, v0:v0 + 2], in_=ssv4[:, v0:v0 + 2], func=AF.Exp, scale=-0.5),
                         reads=sk, writes=sk)
                    for t2 in range(2):
                        vi = (nb * 2 + t2) % 4
                        P.op("dve", lambda e, vi=vi: e.scalar_tensor_tensor(
                            out=vn[vi][:, :], in0=vg[vi][:, :], scalar=ssv4[:, vi:vi + 1], in1=gsgu[:, :], op0=ALU.mult, op1=ALU.mult),
                            reads=(f"vg{vi}.0", f"vg{vi}.1", f"ssv{vi}", "C:W"), writes=(f"vn{vi}",))
                    P.mark()
                    for t2 in range(2):
                        vi = (nb * 2 + t2) % 4
                        for gq in range(2):
                            mi = gq

                            def mm_mix(e, gq=gq, mi=mi, vi=vi):
                                ins = None
                                for g4 in range(4):
                                    gg = gq * 4 + g4
                                    ins = e.matmul(ps_mx[mi][:, 128 * g4:128 * (g4 + 1)], lhsT=vn[vi][:, 128 * gg:128 * (gg + 1)],
                                                   rhs=wsT[:, gg, :], start=True, stop=True)
                                return ins
                            P.op("pe", mm_mix, reads=(f"vn{vi}", "C:W"), writes=(f"b_mx{mi}",))
                            P.op("dve", lambda e, gq=gq, mi=mi: e.tensor_tensor(
                                out=mxb[mi][:, :], in0=ps_mx[mi][:, :], in1=bsbc[:, 512 * gq:512 * (gq + 1)], op=ALU.add),
                                reads=(f"b_mx{mi}", "C:W"), writes=(f"mxb{mi}",))
                            P.op("dve", lambda e, gq=gq, mi=mi, t2=t2: e.tensor_tensor(
                                out=yaT[bi][:, 4 * gq:4 * gq + 4, 128 * t2:128 * (t2 + 1)],
                                in0=mxb[mi][:, :].rearrange("p (g t) -> p g t", g=4),
                                in1=uT[bi][:, 4 * gq:4 * gq + 4, 128 * t2:128 * (t2 + 1)], op=ALU.mult),
                                reads=(f"mxb{mi}",) + tuple(f"uT{bi}.{4 * gq + q}" for q in range(4)),
                                writes=(f"yaT{bi}.{gq}.{t2}",))
                    yk = tuple(f"yaT{bi}.{gq}.{t2}" for gq in range(2) for t2 in range(2))
                    for j in range(8):
                        pj = j % 2

                        def mm_a(e, j=j, pj=pj):
                            ins = None
                            for c in range(8):
                                ins = e.matmul(ps_at[pj][:, 0:256], lhsT=w_a[:, c, 128 * j:128 * (j + 1)],
                                               rhs=yaT[bi][:, c, :], start=(c == 0), stop=(c == 7))
                            return ins
                        P.op("pe", mm_a, reads=yk + ("C:W",), writes=(f"b_at{pj}",))
                        if j % 2:
                            P.op("act", lambda e, j=j, pj=pj: e.copy(out=atsb[bi][:, j, :], in_=ps_at[pj][:, 0:256]),
                                 reads=(f"b_at{pj}",), writes=(f"atsb{bi}.{j}",))
                        else:
                            P.op("dve", lambda e, j=j, pj=pj: e.tensor_copy(out=atsb[bi][:, j, :], in_=ps_at[pj][:, 0:256]),
                                 reads=(f"b_at{pj}",), writes=(f"atsb{bi}.{j}",))
                    P.mark()
                    P.dma("sp", lambda e, nb=nb: e.dma_start(out=at_d[:, :, 256 * nb:256 * (nb + 1)], in_=atsb[bi][:, :, :]),
                          dsem(f"atsb{bi}"), reads=tuple(f"atsb{bi}.{j}" for j in range(8)), writes=())
                P.pipeline([P.split(P.capture(body2, nb_)) for nb_ in range(nblk)], spans=[0.85, 0.85, 1.0, 1.0])
                P.drain_dma("sp")
                P.drain_dma("pool")
                P.emit(nc, "p2a")

        if nblk > 0:
            with ExitStack() as es:
                sb = lambda name, shape, dtp: es.enter_context(nc.sbuf_tensor("sb_" + name, shape, dtp))
                ps = lambda name, shape, dtp=F32: es.enter_context(nc.psum_tensor("pp_" + name, shape, dtp))
                w_g = sb("w_g", [128, 8, 2048], BF16)
                w_b = sb("w_b", [128, 4, 1024], BF16)
                w_o = sb("w_o", [128, 8, 1024], BF16)
                ident = sb("identb", [128, 128], BF16)
                gmix = sb("gmix3", [128, 8], F32)
                ones_bf = sb("ones3", [128, 128], BF16)
                xst = [sb(f"cxst{i}", [128, 8, 256], F32) for i in range(2)]
                sq = [sb(f"csq{i}", [128, 8, 256], BF16) for i in range(2)]
                rstd = [sb(f"crstd{i}", [128, 256], F32) for i in range(2)]
                xn = [sb(f"cxn{i}", [128, 8, 256], BF16) for i in range(2)]
                sg = [sb(f"sg{i}", [128, 16, 256], BF16) for i in range(2)]
                a3 = [sb(f"a3{i}", [128, 3, 520], F32) for i in range(2)]
                s2 = [sb(f"s2{i}", [128, 520], F32) for i in range(2)]
                rden = [sb(f"rden{i}", [128, 8, 1], F32) for i in range(2)]
                yb = [sb(f"yb{i}", [128, 512], BF16) for i in range(2)]
                ybT = [sb(f"ybT{i}", [128, 4, 256], BF16) for i in range(2)]
                atl = [sb(f"atl{i}", [128, 8, 256], F32) for i in range(2)]
                tmpa = [sb(f"tmpa{i}", [128, 256], F32) for i in range(2)]
                tmpb = [sb(f"tmpb{i}", [128, 256], F32) for i in range(2)]
                mT = [sb(f"mT{i}", [128, 8, 256], BF16) for i in range(2)]
                xtk = [sb(f"xtk{i}", [128, 1024], F32) for i in range(4)]
                x2 = [sb(f"x2{i}", [128, 1024], F32) for i in range(4)]
                ps_ss = ps("c_ss", [128, 512])
                ps_pr = [ps(f"c_pr{i}", [128, 512]) for i in range(2)]
                ps_T = ps("c_T", [128, 4, 128], BF16)
                ps_B = [ps(f"c_B{i}", [128, 512]) for i in range(2)]
                ps_o = [ps(f"c_o{i}", [128, 512]) for i in range(2)]

                load_cast(w_g[:, :, :], w_in_h[:, :, 4608:6656], piece=1024)
                load_cast(w_b[:, :, :], wb_h[:, :, :], piece=1024)
                load_cast(w_o[:, :, :], wo_h[:, :, :], piece=1024)
                load_cast(ident[:, :], ident_h, piece=128)
                P.dma("sp", lambda e: e.dma_start(out=gmix[:, :], in_=gmix_h), dsem("w"), writes=("C:W",))
                P.op("dve", lambda e: e.memset(ones_bf[:, :], 1.0), writes=("C:ones",))
                prn = [0]
                rtmp = [sb(f"crtmp{i}", [128, 256], F32) for i in range(2)]
                bufs3 = (xst, sq, rstd, xn, ps_ss, gmix, ones_bf, rtmp)

                def body3(nb):
                    bi = nb % 2
                    if nb == 0:
                        rmsnorm_block(bufs3, xseg[nb][:, :, 64:320], bi, 256, "c", rs_src=rstd_d[:, 256 * nb:256 * (nb + 1)])
                    if nb + 1 < nblk:
                        rmsnorm_block(bufs3, xseg[nb + 1][:, :, 64:320], (nb + 1) % 2, 256, "c",
                                      rs_src=rstd_d[:, 256 * (nb + 1):256 * (nb + 2)])
                    xk = rmsnorm_block(bufs3, None, bi, 256, "c", rs_loaded=True)
                    P.mark()
                    for j in range(16):
                        prn[0] += 1
                        pj = prn[0] % 2

                        def mm_g(e, j=j, pj=pj):
                            ins = None
                            for c in range(8):
                                ins = e.matmul(ps_pr[pj][:, 0:256], lhsT=w_g[:, c, 128 * j:128 * (j + 1)],
                                               rhs=xn[bi][:, c, :], start=(c == 0), stop=(c == 7))
                            return ins
                        P.op("pe", mm_g, reads=tuple(xk) + ("C:W",), writes=(f"c_pr{pj}",))
                        P.op("act", lambda e, j=j, pj=pj: e.activation(out=sg[bi][:, j, :], in_=ps_pr[pj][:, 0:256], func=AF.Sigmoid),
                             reads=(f"c_pr{pj}",), writes=(f"sg{bi}.{j}",))
                    P.dma("sp", lambda e, nb=nb: e.dma_start(out=atl[bi][:, :, :], in_=at_d[:, :, 256 * nb:256 * (nb + 1)]),
                          dsem(f"atl{bi}"), writes=(f"atl{bi}",))
                    for t2 in range(2):
                        ti = (nb * 2 + t2) % 2
                        t0 = 256 * nb + 128 * t2
                        P.dma("sp", lambda e, ti=ti, t0=t0: e.dma_start(
                            out=a3[ti][:, :, :], in_=att_d[:, t0:t0 + 128, :].rearrange("g t c -> t g c")),
                            dsem(f"a3{ti}"), writes=(f"a3{ti}",))
                        xi = (nb * 2 + t2) % 4
                        P.dma("sp", lambda e, xi=xi, t0=t0: e.dma_start(out=xtk[xi][:, :], in_=xtok_h[t0:t0 + 128, :]),
                              dsem(f"xtk{xi}"), writes=(f"xtk{xi}",))
                        P.op("dve", lambda e, ti=ti: e.tensor_tensor(out=s2[ti][:, :], in0=a3[ti][:, 0, :], in1=a3[ti][:, 1, :], op=ALU.add),
                             reads=(f"a3{ti}",), writes=(f"s2{ti}",))
                        P.op("dve", lambda e, ti=ti: e.tensor_tensor(out=s2[ti][:, :], in0=s2[ti][:, :], in1=a3[ti][:, 2, :], op=ALU.add),
                             reads=(f"a3{ti}", f"s2{ti}"), writes=(f"s2{ti}",))
                        P.op("dve", lambda e, ti=ti: e.reciprocal(
                            out=rden[ti][:, :, :], in_=s2[ti][:, :].rearrange("p (h e) -> p h e", h=8)[:, :, 64:65]),
                            reads=(f"s2{ti}",), writes=(f"rden{ti}",))
                        P.op("dve", lambda e, ti=ti: e.tensor_tensor(
                            out=yb[ti][:, :].rearrange("p (h e) -> p h e", h=8),
                            in0=s2[ti][:, :].rearrange("p (h e) -> p h e", h=8)[:, :, 0:64],
                            in1=rden[ti][:, :, :].to_broadcast([128, 8, 64]), op=ALU.mult),
                            reads=(f"s2{ti}", f"rden{ti}"), writes=(f"yb{ti}",))

                        def tr_y(e, ti=ti):
                            ins = None
                            for j in range(4):
                                ins = e.transpose(ps_T[:, j, :], yb[ti][:, 128 * j:128 * (j + 1)], ident[:, :])
                            return ins
                        P.op("pe", tr_y, reads=(f"yb{ti}", "C:W"), writes=("c_T",))
                        P.op("act", lambda e, t2=t2: e.copy(out=ybT[bi][:, :, 128 * t2:128 * (t2 + 1)], in_=ps_T[:, :, :]),
                             reads=("c_T",), writes=(f"ybT{bi}.{t2}",))
                    P.mark()
                    for j in range(8):
                        pj = j % 2

                        def mm_b(e, j=j, pj=pj):
                            ins = None
                            for c in range(4):
                                ins = e.matmul(ps_B[pj][:, 0:256], lhsT=w_b[:, c, 128 * j:128 * (j + 1)],
                                               rhs=ybT[bi][:, c, :], start=(c == 0), stop=(c == 3))
                            return ins
                        P.op("pe", mm_b, reads=(f"ybT{bi}.0", f"ybT{bi}.1", "C:W"), writes=(f"c_B{pj}",))
                        P.op("dve", lambda e, j=j, pj=pj: e.tensor_tensor(out=tmpa[pj][:, :], in0=atl[bi][:, j, :], in1=sg[bi][:, j, :], op=ALU.mult),
                             reads=(f"atl{bi}", f"sg{bi}.{j}"), writes=(f"tmpa{pj}",))
                        P.op("dve", lambda e, j=j, pj=pj: e.tensor_tensor(out=tmpb[pj][:, :], in0=ps_B[pj][:, 0:256], in1=sg[bi][:, 8 + j, :], op=ALU.mult),
                             reads=(f"c_B{pj}", f"sg{bi}.{8 + j}"), writes=(f"tmpb{pj}",))
                        P.op("dve", lambda e, j=j, pj=pj: e.tensor_tensor(out=mT[bi][:, j, :], in0=tmpa[pj][:, :], in1=tmpb[pj][:, :], op=ALU.add),
                             reads=(f"tmpa{pj}", f"tmpb{pj}"), writes=(f"mT{bi}.{j}",))
                    mk = tuple(f"mT{bi}.{j}" for j in range(8))
                    for t2 in range(2):
                        ti = (nb * 2 + t2) % 2
                        t0 = 256 * nb + 128 * t2
                        for hh in range(2):
                            def mm_o2(e, t2=t2, hh=hh):
                                ins = None
                                for c in range(8):
                                    ins = e.matmul(ps_o[hh][:, :], lhsT=mT[bi][:, c, 128 * t2:128 * (t2 + 1)],
                                                   rhs=w_o[:, c, 512 * hh:512 * (hh + 1)], start=(c == 0), stop=(c == 7))
                                return ins
                            P.op("pe", mm_o2, reads=mk + ("C:W",), writes=(f"c_o{hh}",))
                            xi = (nb * 2 + t2) % 4
                            P.op("dve", lambda e, hh=hh, xi=xi: e.tensor_tensor(
                                out=x2[xi][:, 512 * hh:512 * (hh + 1)], in0=ps_o[hh][:, :], in1=xtk[xi][:, 512 * hh:512 * (hh + 1)], op=ALU.add),
                                reads=(f"c_o{hh}", f"xtk{xi}"), writes=(f"x2{xi}.{hh}",))
                    P.mark()
                    for t2 in range(2):
                        xi = (nb * 2 + t2) % 4
                        t0 = 256 * nb + 128 * t2
                        P.dma("sp", lambda e, xi=xi, t0=t0: e.dma_start(out=x2_d[t0:t0 + 128, :], in_=x2[xi][:, :]),
                              dsem(f"x2{xi}"), reads=(f"x2{xi}.0", f"x2{xi}.1"), writes=())
                P.pipeline([P.split(P.capture(body3, nb_)) for nb_ in range(nblk)], spans=[0.85, 0.85, 1.0, 1.0])
                P.drain_dma("sp")
                P.drain_dma("pool")
                P.emit(nc, "p2b")

        if ntile > 0:
            with ExitStack() as es:
                sb = lambda name, shape, dtp: es.enter_context(nc.sbuf_tensor("sb_" + name, shape, dtp))
                ps = lambda name, shape, dtp=F32: es.enter_context(nc.psum_tensor("pp_" + name, shape, dtp))
                NB = 13
                NDG = 4
                wq = sb("wq", [128, 8, 2048], BF16)
                skT = sb("skT", [128, 16, 128], BF16)
                ident = sb("identp", [128, 128], BF16)
                gffn = sb("gffn", [128, 1024], F32)
                gfin = sb("gfin", [128, 1024], F32)
                iota16 = sb("iota16", [128, 16], F32)
                thr16 = sb("thr16", [128, 16], F32)
                posf = sb("posf", [128, 8, 16], F32)
                x2t = [sb(f"x2t{i}", [128, 1024], F32) for i in range(3)]
                hn = [sb(f"hn{i}", [128, 1024], F32) for i in range(3)]
                hnb = sb("hnb", [128, 1024], BF16)
                hnT = sb("hnT", [128, 8, 128], BF16)
                qTp = sb("qTp", [128, 16, 128], BF16)
                junkr = [sb(f"junkp{i}", [128, 1024], BF16) for i in range(2)]
                jn = [0]

                def nj():
                    jn[0] += 1
                    return jn[0] % 2
                st1 = sb("st1", [128, 1], F32)
                S_sbs = [sb(f"S_sb{i}", [128, 16, 128], F32) for i in range(2)]
                S2 = sb("S2", [128, 16, 128], F32)
                tops = sb("top", [128, 16, 16], F32)
                idxu = sb("idxu", [128, 16, 16], U32)
                idxf = sb("idxf", [128, 16, 16], F32)
                cand = sb("cand", [128, 8, 256], F32)
                cand2 = sb("cand2", [128, 8, 256], F32)
                best = sb("best", [128, 8, 16], F32)
                posu = sb("posu", [128, 8, 16], U32)
                pa_u = sb("pa_u", [128, 8, 16], U32)
                pb_u = sb("pb_u", [128, 8, 16], U32)
                pa_f = sb("pa_f", [128, 8, 16], F32)
                pb_f = sb("pb_f", [128, 8, 16], F32)
                eq = sb("eq", [128, 128, 16], F32)
                i1s = sb("i1s", [128, 128], F32)
                i2s = sb("i2s", [128, 128], F32)
                expf = sb("expf", [128, 128], F32)
                expu = [sb(f"expu{i}", [128, 128], U32) for i in range(2)]
                gate = [sb(f"gate{i}", [128, 8, 16], F32) for i in range(2)]
                gsum = sb("gsum", [128, 8, 1], F32)
                aval = sb("aval", [128, 128], F32)
                gval = sb("gval", [128, 128], F32)
                wval = sb("wval", [128, 128], F32)
                gbuf = [sb(f"gbuf{i}", [128, 2048], BF16) for i in range(NB)]
                dg = [sb(f"dg{i}", [128, 128], BF16) for i in range(NDG)]
                ident_f = sb("ident_f", [128, 128], F32)
                st2 = sb("st2", [128, 1], F32)
                x3 = sb("x3", [128, 1024], F32)
                yo = [sb(f"yo{i}", [128, 1024], F32) for i in range(2)]
                ps_T = ps("p_T", [128, 8, 128], BF16)
                ps_q = [ps(f"p_q{i}", [128, 512]) for i in range(2)]
                ps_Sc = [ps(f"p_S{i}", [128, 512]) for i in range(2)]
                ps_acc = [ps(f"p_acc{i}", [128, 512]) for i in range(2)]

                load_cast(wq[:, :, :], wq_h[:, :, :], piece=1024)
                load_cast(skT[:, :, :], skT_h[:, :, :], piece=128)
                load_cast(ident[:, :], ident_h, piece=128)
                for dst_, src_ in ((gffn[:, :], gffn_h), (gfin[:, :], gfin_h), (iota16[:, :], iota_h), (thr16[:, :], thr_h),
                                   (ident_f[:, :], ident_h)):
                    P.dma("sp", lambda e, d_=dst_, s_=src_: e.dma_start(out=d_, in_=s_), dsem("w"), writes=("C:W",))

                def rms_rstd(src, dst1, tag):
                    P.op("act", lambda e: e.activation(out=hnb[:, :], in_=src, func=AF.Square, accum_out=dst1),
                         reads=(tag,), writes=(tag + "r", "hnb"))
                    P.op("dve", lambda e: e.tensor_scalar(out=dst1, in0=dst1, scalar1=1.0 / D, scalar2=EPS, op0=ALU.mult, op1=ALU.add),
                         reads=(tag + "r",), writes=(tag + "r",))
                    P.op("act", lambda e: e.activation(out=dst1, in_=dst1, func=AF.Ln), reads=(tag + "r",), writes=(tag + "r",))
                    P.op("act", lambda e: e.activation(out=dst1, in_=dst1, func=AF.Exp, scale=-0.5), reads=(tag + "r",), writes=(tag + "r",))

                gcount = [0]
                def load_x2(tI):
                    P.dma("sp", lambda e, ti=tI % 3, t0=128 * tI: e.dma_start(out=x2t[ti][:, :], in_=x2_d[t0:t0 + 128, :]),
                          dsem(f"x2t{tI % 3}"), writes=(f"x2t{tI % 3}",))
                def body4a(tI):
                    ti = tI % 2
                    t3 = tI % 3
                    S_sb = S_sbs[tI % 2]
                    sk_ = f"S_sb{tI % 2}"
                    t0 = 128 * tI
                    load_x2(tI)
                    rms_rstd(x2t[t3][:, :], st1[:, :], f"x2t{t3}")
                    P.op("dve", lambda e: e.scalar_tensor_tensor(out=hn[t3][:, :], in0=x2t[t3][:, :], scalar=st1[:, 0:1], in1=gffn[:, :],
                                                                 op0=ALU.mult, op1=ALU.mult),
                         reads=(f"x2t{t3}", f"x2t{t3}r", "C:W"), writes=(f"hn{t3}",))
                    P.op("act", lambda e: e.copy(out=hnb[:, :], in_=hn[t3][:, :]), reads=(f"hn{t3}",), writes=("hnb",))

                    def tr_h(e):
                        ins = None
                        for c in range(8):
                            ins = e.transpose(ps_T[:, c, :], hnb[:, 128 * c:128 * (c + 1)], ident[:, :])
                        return ins
                    P.op("pe", tr_h, reads=("hnb", "C:W"), writes=("p_T",))
                    P.op("act", lambda e: e.copy(out=hnT[:, :, :], in_=ps_T[:, :, :]), reads=("p_T",), writes=("hnT",))
                    for qg in range(4):
                        pj = qg % 2

                        def mm_pq(e, qg=qg, pj=pj):
                            ins = None
                            for q4 in range(4):
                                hp = qg * 4 + q4
                                for c in range(8):
                                    ins = e.matmul(ps_q[pj][:, 128 * q4:128 * (q4 + 1)], lhsT=wq[:, c, 128 * hp:128 * (hp + 1)],
                                                   rhs=hnT[:, c, :], start=(c == 0), stop=(c == 7))
                            return ins
                        P.op("pe", mm_pq, reads=("hnT", "C:W"), writes=(f"p_q{pj}",))
                        P.op("act", lambda e, qg=qg, pj=pj: e.copy(out=qTp[:, 4 * qg:4 * qg + 4, :],
                                                                  in_=ps_q[pj][:, :].rearrange("p (a b) -> p a b", a=4)),
                             reads=(f"p_q{pj}",), writes=(f"qTp.{qg}",))
                    for qg in range(4):
                        def mm_ps(e, qg=qg):
                            ins = None
                            for q4 in range(4):
                                hp = qg * 4 + q4
                                ins = e.matmul(ps_Sc[qg % 2][:, 128 * q4:128 * (q4 + 1)], lhsT=qTp[:, hp, :], rhs=skT[:, hp, :],
                                               start=True, stop=True)
                            return ins
                        P.op("pe", mm_ps, reads=(f"qTp.{qg}", "C:W"), writes=(f"p_S{qg % 2}",))
                        P.op("act", lambda e, qg=qg: e.copy(out=S_sb[:, 4 * qg:4 * qg + 4, :],
                                                           in_=ps_Sc[qg % 2][:, :].rearrange("p (a b) -> p a b", a=4)),
                             reads=(f"p_S{qg % 2}",), writes=(f"{sk_}.{qg}",))
                    P.mark()
                    for hp in range(16):
                        kq = f"{sk_}.{hp // 4}"
                        P.op("dve", lambda e, hp=hp: e.max(out=tops[:, hp, 0:8], in_=S_sb[:, hp, :]), reads=(kq,), writes=(f"top{hp}a",))
                        P.op("dve", lambda e, hp=hp: e.match_replace(out=S2[:, hp, :], in_to_replace=tops[:, hp, 0:8], in_values=S_sb[:, hp, :],
                                                                    imm_value=-1e30),
                             reads=(kq, f"top{hp}a"), writes=(f"S2.{hp}",))
                        P.op("dve", lambda e, hp=hp: e.max(out=tops[:, hp, 8:16], in_=S2[:, hp, :]), reads=(f"S2.{hp}",), writes=(f"top{hp}b",))
                        P.op("dve", lambda e, hp=hp: e.max_index(out=idxu[:, hp, 0:8], in_max=tops[:, hp, 0:8], in_values=S_sb[:, hp, :]),
                             reads=(kq, f"top{hp}a"), writes=(f"idx{hp}a",))
                        P.op("dve", lambda e, hp=hp: e.max_index(out=idxu[:, hp, 8:16], in_max=tops[:, hp, 8:16], in_values=S_sb[:, hp, :]),
                             reads=(kq, f"top{hp}b"), writes=(f"idx{hp}b",))
                    allidx = tuple(f"idx{hp}{x}" for hp in range(16) for x in "ab")
                    alltop = tuple(f"top{hp}{x}" for hp in range(16) for x in "ab")
                    P.op("dve", lambda e: e.tensor_copy(out=idxf[:, :, :], in_=idxu[:, :, :]), reads=allidx, writes=("idxf",))
                    topv = tops[:, :, :].rearrange("p (h t) k -> p h t k", t=2)
                    P.op("dve", lambda e: e.tensor_tensor(
                        out=cand[:, :, :].rearrange("p h (a b) -> p h a b", a=16),
                        in0=topv[:, :, 0, :].unsqueeze(3).to_broadcast([128, 8, 16, 16]),
                        in1=topv[:, :, 1, :].unsqueeze(2).to_broadcast([128, 8, 16, 16]), op=ALU.add),
                        reads=alltop, writes=("cand",))
                    for h in range(8):
                        P.op("dve", lambda e, h=h: e.max(out=best[:, h, 0:8], in_=cand[:, h, :]), reads=("cand",), writes=(f"best{h}a",))
                        P.op("dve", lambda e, h=h: e.match_replace(out=cand2[:, h, :], in_to_replace=best[:, h, 0:8], in_values=cand[:, h, :],
                                                                  imm_value=-1e30),
                             reads=("cand", f"best{h}a"), writes=(f"cand2.{h}",))
                        P.op("dve", lambda e, h=h: e.max(out=best[:, h, 8:16], in_=cand2[:, h, :]), reads=(f"cand2.{h}",), writes=(f"best{h}b",))
                        P.op("dve", lambda e, h=h: e.max_index(out=posu[:, h, 0:8], in_max=best[:, h, 0:8], in_values=cand[:, h, :]),
                             reads=("cand", f"best{h}a"), writes=(f"pos{h}a",))
                        P.op("dve", lambda e, h=h: e.max_index(out=posu[:, h, 8:16], in_max=best[:, h, 8:16], in_values=cand[:, h, :]),
                             reads=("cand", f"best{h}b"), writes=(f"pos{h}b",))
                    allpos = tuple(f"pos{h}{x}" for h in range(8) for x in "ab")
                    allbest = tuple(f"best{h}{x}" for h in range(8) for x in "ab")
                    gi = tI % 2
                    P.op("dve", lambda e, gi=gi: e.tensor_tensor(out=gate[gi][:, :, :], in0=best[:, :, :],
                                                                in1=best[:, :, 0:1].to_broadcast([128, 8, 16]), op=ALU.subtract),
                         reads=allbest, writes=(f"gate{gi}",))
                    P.op("act", lambda e, gi=gi: e.activation(out=gate[gi][:, :, :], in_=gate[gi][:, :, :], func=AF.Exp),
                         reads=(f"gate{gi}",), writes=(f"gate{gi}",))
                    P.op("dve", lambda e, gi=gi: e.tensor_reduce(out=gsum[:, :, :], in_=gate[gi][:, :, :], axis=AX.X, op=ALU.add),
                         reads=(f"gate{gi}",), writes=("gsum",))
                    P.op("dve", lambda e: e.reciprocal(out=gsum[:, :, :], in_=gsum[:, :, :]), reads=("gsum",), writes=("gsum",))
                    P.op("dve", lambda e, gi=gi: e.tensor_tensor(out=gate[gi][:, :, :], in0=gate[gi][:, :, :],
                                                                in1=gsum[:, :, :].to_broadcast([128, 8, 16]), op=ALU.mult),
                         reads=(f"gate{gi}", "gsum"), writes=(f"gate{gi}",))
                    P.op("dve", lambda e: e.tensor_copy(out=posf[:, :, :], in_=posu[:, :, :]), reads=allpos, writes=("posf",))
                    P.op("dve", lambda e: e.tensor_tensor(
                        out=eq[:, :, :], in0=posf[:, :, :].rearrange("p h k -> p (h k)").unsqueeze(2).to_broadcast([128, 128, 16]),
                        in1=thr16[:, :].unsqueeze(1).to_broadcast([128, 128, 16]), op=ALU.is_ge),
                        reads=("posf", "C:W"), writes=("eq",))
                    P.op("dve", lambda e: e.tensor_reduce(out=pa_f[:, :, :].rearrange("p h k -> p (h k)"), in_=eq[:, :, :], axis=AX.X, op=ALU.add),
                         reads=("eq",), writes=("pa_f",))
                    P.op("dve", lambda e: e.scalar_tensor_tensor(out=pb_f[:, :, :], in0=pa_f[:, :, :], scalar=-16.0, in1=posf[:, :, :],
                                                                 op0=ALU.mult, op1=ALU.add),
                         reads=("pa_f", "posf"), writes=("pb_f",))
                    idxv = idxf[:, :, :].rearrange("p (h t) k -> p h t k", t=2)
                    for (pf, half, dstk, dst) in ((pa_f, 0, "i1s", i1s), (pb_f, 1, "i2s", i2s)):
                        pk = "pa_f" if half == 0 else "pb_f"
                        P.op("dve", lambda e, pf=pf: e.tensor_tensor(
                            out=eq[:, :, :], in0=pf[:, :, :].rearrange("p h k -> p (h k)").unsqueeze(2).to_broadcast([128, 128, 16]),
                            in1=iota16[:, :].unsqueeze(1).to_broadcast([128, 128, 16]), op=ALU.is_equal),
                            reads=(pk, "C:W"), writes=("eq",))
                        P.op("dve", lambda e, half=half: e.tensor_tensor(
                            out=eq[:, :, :].rearrange("p (h k) a -> p h k a", h=8),
                            in0=eq[:, :, :].rearrange("p (h k) a -> p h k a", h=8),
                            in1=idxv[:, :, half, :].unsqueeze(2).to_broadcast([128, 8, 16, 16]), op=ALU.mult),
                            reads=("eq", "idxf"), writes=("eq",))
                        P.op("dve", lambda e, dst=dst: e.tensor_reduce(out=dst[:, :], in_=eq[:, :, :], axis=AX.X, op=ALU.add),
                             reads=("eq",), writes=(dstk,))
                    P.op("dve", lambda e: e.scalar_tensor_tensor(out=expf[:, :], in0=i1s[:, :], scalar=128.0, in1=i2s[:, :], op0=ALU.mult, op1=ALU.add),
                         reads=("i1s", "i2s"), writes=("expf",))
                    P.op("dve", lambda e, gi=gi: e.tensor_copy(out=expu[gi][:, :], in_=expf[:, :]), reads=("expf",), writes=(f"expu{gi}",))

                def body4b(tI):
                    ti = tI % 2
                    gi = tI % 2
                    t3 = tI % 3
                    t0 = 128 * tI
                    for k in range(nslot):
                        gcount[0] += 1
                        b = gcount[0] % NB
                        db = gcount[0] % NDG
                        P.dma("pool", lambda e, b=b, k=k, gi=gi: e.indirect_dma_start(
                            out=gbuf[b][:, :], out_offset=None, in_=uv16_d,
                            in_offset=bass.IndirectOffsetOnAxis(ap=expu[gi][:, k:k + 1], axis=0)),
                            dsem(f"gb{b}", "pool"), reads=(f"expu{gi}",), writes=(f"gbuf{b}",))
                        ji = nj()
                        P.op("dve", lambda e, b=b, k=k, t3=t3, ji=ji: e.scalar_tensor_tensor(
                            out=junkr[ji][:, :], in0=gbuf[b][:, 0:1024], scalar=1.0, in1=hn[t3][:, :], op0=ALU.mult, op1=ALU.mult,
                            accum_out=aval[:, k:k + 1]),
                            reads=(f"gbuf{b}", f"hn{t3}"), writes=(f"av{k}", f"junk{ji}"))
                        P.op("act", lambda e, k=k: e.activation(out=gval[:, k:k + 1], in_=aval[:, k:k + 1], func=AF.Gelu),
                             reads=(f"av{k}",), writes=(f"gv{k}",))
                        P.op("act", lambda e, k=k, gi=gi: e.mul(out=wval[:, k:k + 1], in_=gval[:, k:k + 1],
                                                               mul=gate[gi][:, :, :].rearrange("p h k -> p (h k)")[:, k:k + 1]),
                             reads=(f"gv{k}", f"gate{gi}"), writes=(f"wv{k}",))
                        P.op("act", lambda e, k=k, db=db: e.activation(out=dg[db][:, :], in_=ident_f[:, :], func=AF.Copy,
                                                                      scale=wval[:, k:k + 1]),
                             reads=(f"wv{k}", "C:W"), writes=(f"dg{db}",))

                        def mm_acc(e, k=k, b=b, db=db):
                            ins = None
                            for hh in range(2):
                                ins = e.matmul(ps_acc[hh][:, :], lhsT=dg[db][:, :], rhs=gbuf[b][:, 1024 + 512 * hh:1024 + 512 * (hh + 1)],
                                               start=(k == 0), stop=(k == nslot - 1))
                            return ins
                        P.op("pe", mm_acc, reads=(f"dg{db}", f"gbuf{b}"), writes=("p_acc",))
                    for hh in range(2):
                        P.op("dve", lambda e, t3=t3, hh=hh: e.tensor_tensor(out=x3[:, 512 * hh:512 * (hh + 1)], in0=ps_acc[hh][:, :],
                                                                           in1=x2t[t3][:, 512 * hh:512 * (hh + 1)], op=ALU.add),
                             reads=(f"x2t{t3}", "p_acc"), writes=(f"x3.{hh}",))
                    P.op("act", lambda e, ti=ti: e.activation(out=yo[ti][:, :], in_=x3[:, :], func=AF.Square, accum_out=st2[:, :]),
                         reads=("x3.0", "x3.1"), writes=("st2", f"yo{ti}"))
                    P.op("dve", lambda e: e.tensor_scalar(out=st2[:, :], in0=st2[:, :], scalar1=1.0 / D, scalar2=EPS, op0=ALU.mult, op1=ALU.add),
                         reads=("st2",), writes=("st2",))
                    P.op("act", lambda e: e.activation(out=st2[:, :], in_=st2[:, :], func=AF.Ln), reads=("st2",), writes=("st2",))
                    P.op("act", lambda e: e.activation(out=st2[:, :], in_=st2[:, :], func=AF.Exp, scale=-0.5), reads=("st2",), writes=("st2",))
                    P.op("dve", lambda e, ti=ti: e.scalar_tensor_tensor(out=yo[ti][:, :], in0=x3[:, :], scalar=st2[:, 0:1], in1=gfin[:, :],
                                                                       op0=ALU.mult, op1=ALU.mult),
                         reads=("x3.0", "x3.1", "st2", "C:W"), writes=(f"yo{ti}",))
                    P.dma("sp", lambda e, ti=ti, t0=t0: e.dma_start(out=y_h[t0:t0 + 128, :], in_=yo[ti][:, :]),
                          dsem(f"yo{ti}"), reads=(f"yo{ti}",), writes=())

                recA = [P.split(P.capture(body4a, t_)) for t_ in range(ntile)]
                recB = [P.capture(body4b, t_) for t_ in range(ntile)]
                P.pipeline([recA[t_] + [recB[t_]] for t_ in range(ntile)], spans=[0.85, 0.85, 1.0])
                P.drain_dma("sp")
                P.drain_dma("pool")
                P.emit(nc, "p3")
    return nc


def _seg_tokens():
    out = np.zeros((NSEG, 384), np.int64)
    kk = np.arange(384)
    for s in range(NSEG):
        g, sp_ = s // 16, s % 16
        d = DIL[g]
        nsub = 16 // d
        r, sub = sp_ // nsub, sp_ % nsub
        m = 256 * sub - 64 + kk
        out[s] = d * m + r
    return out


def prepare_inputs(inp):
    f = lambda a: np.ascontiguousarray(np.asarray(a, dtype=np.float32))
    x = f(inp["x"])
    lay = lambda w, nch: np.ascontiguousarray(w.reshape(nch, 128, -1).transpose(1, 0, 2))
    shared = {
        "w_in_l": lay(f(inp["w_in"])[0], 8),
        "gmix": np.ascontiguousarray(f(inp["norm_mix_g"])[0].reshape(8, 128).T),
        "gsgu_bc": np.ascontiguousarray(np.broadcast_to(f(inp["sgu_norm_g"])[0][None, :], (128, 1024))),
        "wsT": np.ascontiguousarray(f(inp["sgu_w"])[0].transpose(2, 0, 1)),
        "bsgu_bc": np.ascontiguousarray(np.broadcast_to(f(inp["sgu_b"])[0].reshape(1, 1024), (128, 1024))),
        "wa_l": lay(f(inp["w_branch_a"])[0], 8),
        "wb_l": lay(f(inp["w_branch_b"])[0], 4),
        "wo_l": lay(f(inp["w_out"])[0], 8),
        "gffn_bc": np.ascontiguousarray(np.broadcast_to(f(inp["norm_ffn_g"])[0][None, :], (128, 1024))),
        "gfin_bc": np.ascontiguousarray(np.broadcast_to(f(inp["norm_final_g"])[None, :], (128, 1024))),
        "wq_l": lay(f(inp["peer_wq"])[0], 8),
        "skT": np.ascontiguousarray(f(inp["peer_subkeys"])[0].reshape(16, 128, 128).transpose(2, 0, 1)),
        "uv_tab": np.ascontiguousarray(np.concatenate([f(inp["peer_u"])[0], f(inp["peer_v"])[0]], axis=1)),
        "ident": np.eye(128, dtype=np.float32),
        "iota16": np.ascontiguousarray(np.broadcast_to(np.arange(16, dtype=np.float32)[None, :], (128, 16))),
        "thr16": np.ascontiguousarray(np.broadcast_to((16.0 * np.arange(1, 17, dtype=np.float32))[None, :], (128, 16))),
    }
    jl = np.arange(128)[:, None]
    il = np.arange(128)[None, :]
    relA = jl - il - 64
    relB = jl - il + 64
    A = np.where(np.abs(relA) <= 64, -np.abs(relA), -1e9).astype(np.float32)
    Bm = np.where(np.abs(relB) <= 64, -np.abs(relB), -1e9).astype(np.float32)
    shared["negabs"] = np.ascontiguousarray(np.concatenate([A, Bm, A, Bm], axis=1))
    segtok = _seg_tokens()
    maps = []
    for c in range(8):
        b, hf = c // 2, c % 2
        T0 = hf * NTOK
        gtok = segtok + T0
        valid = (gtok >= 0) & (gtok < SEQ)
        xg = x[b][np.clip(gtok, 0, SEQ - 1)]
        xg = xg * valid[..., None].astype(np.float32) if False else np.where(valid[..., None], xg, np.float32(0))
        xs = np.ascontiguousarray(xg.reshape(NSEG, 384, 8, 128).transpose(0, 3, 2, 1))
        kv = valid.reshape(NSEG, 3, 128).transpose(2, 0, 1).reshape(128, NSEG * 3).astype(np.float32)
        m = dict(shared)
        m["xseg"] = xs
        m["kval"] = np.ascontiguousarray(kv)
        m["xtok"] = np.ascontiguousarray(x[b, T0:T0 + NTOK])
        maps.append(m)
    return maps


def kernel(**inputs):
    maps = prepare_inputs(inputs)
    nc = build_program()
    res = run_bass_kernel_spmd(nc, maps, core_ids=list(range(8)))
    out = np.zeros((4, SEQ, D), np.float32)
    for c in range(8):
        b, hf = c // 2, c % 2
        out[b, hf * NTOK:(hf + 1) * NTOK] = res.results[c]["y"]
    return out
```

```python
from contextlib import ExitStack
import numpy as np
import concourse.bass as bass
import concourse.mybir as mybir
from concourse.bass_utils import run_bass_kernel_spmd

F32 = mybir.dt.float32
BF16 = mybir.dt.bfloat16
U32 = mybir.dt.uint32
AF = mybir.ActivationFunctionType
ALU = mybir.AluOpType
AX = mybir.AxisListType

D = 1024
SEQ = 8192
NTOK = 4096
NSEG = 48
EPS = 1e-6
DIL = (1, 4, 16)
ENG = ("pe", "act", "dve", "pool", "sp")


class Prog:
    def __init__(self, semobj):
        self.semobj = semobj
        self.cnt = {e: 0 for e in ENG}
        self.dcnt = {}
        self.q = {e: [] for e in ENG}
        self.lastw = {}
        self.readers = {}
        self.known = {e: {} for e in ENG}
        self._rec = None

    def capture(self, fn, *args):
        saved = self._rec
        self._rec = []
        fn(*args)
        rec, self._rec = self._rec, saved
        return rec

    def replay(self, rec):
        for kind, a in rec:
            if kind == "op":
                self.op(*a)
            else:
                self.dma(*a)

    def mark(self):
        if self._rec is not None:
            self._rec.append(("mark", None))

    @staticmethod
    def split(rec):
        out = [[]]
        for it in rec:
            if it[0] == "mark":
                out.append([])
            else:
                out[-1].append(it)
        return out

    @staticmethod
    def interleave_n(lists, spans=None):
        items = []
        for li, L in enumerate(lists):
            n = len(L)
            f = 1.0 if spans is None else spans[li]
            for i, it in enumerate(L):
                items.append(((i + 0.5) / n * f, li, i, it))
        items.sort(key=lambda t: (t[0], t[1], t[2]))
        return [t[3] for t in items]

    def pipeline(self, stages_by_item, spans=None):
        n = len(stages_by_item)
        S = max(len(x) for x in stages_by_item)
        for step in range(n + S - 1):
            sts = [st for st in range(S - 1, -1, -1) if 0 <= step - st < n and st < len(stages_by_item[step - st])]
            lists = [stages_by_item[step - st][st] for st in sts]
            self.replay(self.interleave_n(lists, None if spans is None else [spans[st] for st in sts]))

    @staticmethod
    def interleave(A, B):
        out = []
        na, nb = len(A), len(B)
        ia = ib = 0
        while ia < na or ib < nb:
            if ib >= nb or (ia < na and ia * nb <= ib * na):
                out.append(A[ia])
                ia += 1
            else:
                out.append(B[ib])
                ib += 1
        return out

    def _deps(self, eng, reads, writes):
        need = {}

        def add(tok):
            if tok is None:
                return
            s, v = tok
            if need.get(s, 0) < v:
                need[s] = v

        def addw(k):
            for s, v in self.lastw.get(k, {}).items():
                add((s, v))
        for k in reads:
            addw(k)
        for k in writes:
            addw(k)
            for t in self.readers.get(k, ()):
                add(t)
        out = []
        kn = self.known[eng]
        for s, v in need.items():
            if kn.get(s, 0) < v:
                kn[s] = v
                out.append((s, v))
        return out

    def _commit(self, tok, reads, writes):
        for k in reads:
            if not k.startswith("C:"):
                self.readers.setdefault(k, []).append(tok)
        for k in writes:
            if k.startswith("C:"):
                self.lastw.setdefault(k, {})[tok[0]] = tok[1]
            else:
                self.lastw[k] = {tok[0]: tok[1]}
            self.readers[k] = []

    def op(self, eng, fn, reads=(), writes=()):
        if self._rec is not None:
            self._rec.append(("op", (eng, fn, reads, writes)))
            return None
        waits = self._deps(eng, reads, writes)
        self.cnt[eng] += 1
        tok = (eng, self.cnt[eng])
        self.q[eng].append((waits, fn, (eng, 1)))
        self._commit(tok, reads, writes)
        return tok

    def dma(self, eng, fn, semkey, reads=(), writes=()):
        if self._rec is not None:
            self._rec.append(("dma", (eng, fn, semkey, reads, writes)))
            return None
        waits = self._deps(eng, reads, writes)
        self.dcnt[semkey] = self.dcnt.get(semkey, 0) + 16
        tok = (semkey, self.dcnt[semkey])
        self.q[eng].append((waits, fn, (semkey, 16)))
        self._commit(tok, reads, writes)
        return tok

    def drain_dma(self, eng="sp"):
        waits = []
        for s, v in self.dcnt.items():
            if self.known[eng].get(s, 0) < v:
                self.known[eng][s] = v
                waits.append((s, v))
        if waits:
            self.q[eng].append((waits, None, None))

    def emit(self, nc, name):
        q = self.q
        semobj = self.semobj

        def run(eng_name):
            def f(e):
                for waits, fn, inc in q[eng_name]:
                    for s, v in waits:
                        e.wait_ge(semobj[s], v)
                    if fn is not None:
                        ins = fn(e)
                        ins.then_inc(semobj[inc[0]], inc[1])
            return f
        with nc.Block() as block:
            block.tensor(run("pe"))
            block.scalar(run("act"))
            block.vector(run("dve"))
            block.gpsimd(run("pool"))
            block.sync(run("sp"))
        self.q = {e: [] for e in ENG}
        self.lastw = {}
        self.readers = {}


def _slopes():
    return [2.0 ** (-(h + 1)) for h in range(8)]


def build_program(cfg=None):
    cfg = cfg or {}
    segs = cfg.get("segs", list(range(NSEG)))
    nseg = len(segs)
    nblk = cfg.get("nblk", 16)
    ntile = cfg.get("ntile", 32)
    nslot = cfg.get("nslot", 128)
    debug = cfg.get("debug", False)
    pool_rms = cfg.get("pool_rms", 4)
    scratch_kind = "ExternalOutput" if debug else "Internal"

    nc = bass.Bass("TRN2", target_bir_lowering=False)
    dt = nc.dram_tensor
    xseg = dt("xseg", [NSEG, 128, 8, 384], F32, kind="ExternalInput").ap()
    kval_h = dt("kval", [128, NSEG * 3], F32, kind="ExternalInput").ap()
    xtok_h = dt("xtok", [NTOK, D], F32, kind="ExternalInput").ap()
    w_in_h = dt("w_in_l", [128, 8, 6656], F32, kind="ExternalInput").ap()
    gmix_h = dt("gmix", [128, 8], F32, kind="ExternalInput").ap()
    gsgu_h = dt("gsgu_bc", [128, 1024], F32, kind="ExternalInput").ap()
    wsT_h = dt("wsT", [128, 8, 128], F32, kind="ExternalInput").ap()
    bsbc_h = dt("bsgu_bc", [128, 1024], F32, kind="ExternalInput").ap()
    wa_h = dt("wa_l", [128, 8, 1024], F32, kind="ExternalInput").ap()
    wb_h = dt("wb_l", [128, 4, 1024], F32, kind="ExternalInput").ap()
    wo_h = dt("wo_l", [128, 8, 1024], F32, kind="ExternalInput").ap()
    gffn_h = dt("gffn_bc", [128, 1024], F32, kind="ExternalInput").ap()
    gfin_h = dt("gfin_bc", [128, 1024], F32, kind="ExternalInput").ap()
    wq_h = dt("wq_l", [128, 8, 2048], F32, kind="ExternalInput").ap()
    skT_h = dt("skT", [128, 16, 128], F32, kind="ExternalInput").ap()
    uv_h = dt("uv_tab", [16384, 2048], F32, kind="ExternalInput").ap()
    negabs_h = dt("negabs", [128, 512], F32, kind="ExternalInput").ap()
    ident_h = dt("ident", [128, 128], F32, kind="ExternalInput").ap()
    iota_h = dt("iota16", [128, 16], F32, kind="ExternalInput").ap()
    thr_h = dt("thr16", [128, 16], F32, kind="ExternalInput").ap()
    y_h = dt("y", [NTOK, D], F32, kind="ExternalOutput").ap()
    att_d = dt("att_d", [3, NTOK, 520], F32, kind=scratch_kind).ap()
    at_d = dt("at_d", [128, 8, NTOK], F32, kind=scratch_kind).ap()
    x2_d = dt("x2_d", [NTOK, D], F32, kind=scratch_kind).ap()
    uv16_d = dt("uv16_d", [16384, 2048], BF16, kind="Internal").ap()
    rstd_d = dt("rstd_d", [128, NTOK], F32, kind="Internal").ap()

    slopes = _slopes()

    with ExitStack() as top:
        semobj = {}
        for e in ENG:
            semobj[e] = top.enter_context(nc.semaphore("s_" + e))

        def dsem(name, eng="sp"):
            name = name + "@" + eng
            if name not in semobj:
                semobj[name] = top.enter_context(nc.semaphore("d_" + name))
            return name
        P = Prog(semobj)

        def load_cast(dst, src, eng="pool", sem="w", piece=1024):
            n = src.shape[-1]
            pre = (slice(None),) * (len(src.shape) - 1)
            for a in range(0, n, piece):
                b = min(n, a + piece)
                P.dma(eng, (lambda e, d_=dst[pre + (slice(a, b),)], s_=src[pre + (slice(a, b),)]: e.dma_start(out=d_, in_=s_)),
                      dsem(sem, eng), writes=("C:W",))

        def rmsnorm_block(es_bufs, src_ap, bi, n, tagp, rs_src=None, rs_loaded=False):
            xst, sq, rstd, xn, ps_ss, gmix, ones_bf, rtmp = es_bufs[:8]
            mhalf = None
            kx = f"{tagp}xst{bi}"
            if src_ap is not None:
                P.dma("sp", lambda e: e.dma_start(out=xst[bi][:, :, 0:n], in_=src_ap), dsem(kx), writes=(kx,))
                if rs_src is not None:
                    P.dma("sp", lambda e: e.dma_start(out=rstd[bi][:, 0:n], in_=rs_src), dsem(f"{tagp}rsl{bi}"),
                          writes=(f"{tagp}rs{bi}",))
                return None
            if rs_loaded:
                return scale_chunks(es_bufs, bi, n, tagp)
            P.op("act", lambda e: e.activation(out=sq[bi][:, :, 0:n], in_=xst[bi][:, :, 0:n], func=AF.Square),
                 reads=(kx,), writes=(f"{tagp}sq{bi}",))

            def mm_ss(e):
                ins = None
                for c in range(8):
                    ins = e.matmul(ps_ss[:, 0:n], lhsT=ones_bf[:, :], rhs=sq[bi][:, c, 0:n], start=(c == 0), stop=(c == 7))
                return ins
            P.op("pe", mm_ss, reads=(f"{tagp}sq{bi}", "C:W", "C:ones"), writes=(f"{tagp}ps_ss",))
            P.op("dve", lambda e: e.tensor_scalar(out=rstd[bi][:, 0:n], in0=ps_ss[:, 0:n], scalar1=1.0 / D, scalar2=EPS,
                                                  op0=ALU.mult, op1=ALU.add),
                 reads=(f"{tagp}ps_ss",), writes=(f"{tagp}rs{bi}",))
            if mhalf is not None:
                P.op("pool", lambda e: e.tensor_tensor(out=rstd[bi][:, 0:n], in0=rstd[bi][:, 0:n], in1=mhalf[:, 0:n], op=ALU.pow),
                     reads=(f"{tagp}rs{bi}", "C:mhalf"), writes=(f"{tagp}rs{bi}",))
            else:
                P.op("act", lambda e: e.activation(out=rstd[bi][:, 0:n], in_=rstd[bi][:, 0:n], func=AF.Ln),
                     reads=(f"{tagp}rs{bi}",), writes=(f"{tagp}rs{bi}",))
                P.op("act", lambda e: e.activation(out=rstd[bi][:, 0:n], in_=rstd[bi][:, 0:n], func=AF.Exp, scale=-0.5),
                     reads=(f"{tagp}rs{bi}",), writes=(f"{tagp}rs{bi}",))
            return scale_chunks(es_bufs, bi, n, tagp)

        def scale_chunks(es_bufs, bi, n, tagp):
            xst, sq, rstd, xn, ps_ss, gmix, ones_bf, rtmp = es_bufs[:8]
            kx = f"{tagp}xst{bi}"
            for c in range(8):
                if c < pool_rms:
                    tb = c % 2
                    P.op("pool", lambda e, c=c, tb=tb: e.tensor_tensor(
                        out=rtmp[tb][:, 0:n], in0=xst[bi][:, c, 0:n], in1=rstd[bi][:, 0:n], op=ALU.mult),
                        reads=(kx, f"{tagp}rs{bi}"), writes=(f"{tagp}rtmp{tb}",))
                    P.op("pool", lambda e, c=c, tb=tb: e.tensor_scalar(
                        out=xn[bi][:, c, 0:n], in0=rtmp[tb][:, 0:n], scalar1=gmix[:, c:c + 1], scalar2=1.0,
                        op0=ALU.mult, op1=ALU.mult),
                        reads=(f"{tagp}rtmp{tb}", "C:W"), writes=(f"{tagp}xn{bi}.{c}",))
                    continue
                P.op("dve", lambda e, c=c: e.scalar_tensor_tensor(
                    out=xn[bi][:, c, 0:n], in0=xst[bi][:, c, 0:n], scalar=gmix[:, c:c + 1], in1=rstd[bi][:, 0:n],
                    op0=ALU.mult, op1=ALU.mult),
                    reads=(kx, f"{tagp}rs{bi}", "C:W"), writes=(f"{tagp}xn{bi}.{c}",))
            return [f"{tagp}xn{bi}.{c}" for c in range(8)]

        if nseg > 0:
            with ExitStack() as es:
                sb = lambda name, shape, dtp: es.enter_context(nc.sbuf_tensor("sb_" + name, shape, dtp))
                ps = lambda name, shape, dtp=F32: es.enter_context(nc.psum_tensor("pp_" + name, shape, dtp))
                wqkv = sb("wqkv", [128, 8, 2560], BF16)
                gmix = sb("gmix1", [128, 8], F32)
                negabs = sb("negabs", [128, 512], F32)
                ones_bf = sb("ones1", [128, 128], BF16)
                ones8 = sb("ones8", [128, 8, 1], F32)
                kval = sb("kval", [128, NSEG * 3], F32)
                xst = [sb(f"xst{i}", [128, 8, 384], F32) for i in range(2)]
                sq = [sb(f"sq{i}", [128, 8, 384], BF16) for i in range(2)]
                rstd = [sb(f"rstd{i}", [128, 384], F32) for i in range(2)]
                xn = [sb(f"xn{i}", [128, 8, 384], BF16) for i in range(2)]
                kT = [sb(f"kT{i}", [128, 4, 384], BF16) for i in range(2)]
                qT = [sb(f"qT{i}", [128, 4, 256], BF16) for i in range(2)]
                vaug = [sb(f"vaug{i}", [128, 3, 8, 65], BF16) for i in range(2)]
                tt = [sb(f"tt{i}", [128, 512], F32) for i in range(2)]
                pT = [sb(f"pT{i}", [128, 512], BF16) for i in range(2)]
                osb = [sb(f"osb{i}", [128, 2, 520], F32) for i in range(2)]
                cst = [sb(f"cst{i}", [128, 2, 2048], BF16) for i in range(2)]
                castn = [0 if cfg.get("cast", True) else 64]
                ps_ss1 = ps("ps_ss1", [128, 512])
                ps_pr = [ps(f"ps_pr{i}", [128, 512]) for i in range(2)]
                ps_S = [ps(f"ps_S{i}", [128, 512]) for i in range(2)]
                ps_O = [ps(f"ps_O{i}", [128, 512]) for i in range(3)]
                ocnt = [0]

                load_cast(wqkv[:, :, :], w_in_h[:, :, 2048:4608], piece=640)
                P.dma("sp", lambda e: e.dma_start(out=gmix[:, :], in_=gmix_h), dsem("w"), writes=("C:W",))
                P.dma("sp", lambda e: e.dma_start(out=negabs[:, :], in_=negabs_h), dsem("w"), writes=("C:W",))
                P.dma("sp", lambda e: e.dma_start(out=kval[:, :], in_=kval_h), dsem("w"), writes=("C:W",))
                P.op("dve", lambda e: e.memset(ones_bf[:, :], 1.0), writes=("C:ones",))
                P.op("dve", lambda e: e.memset(ones8[:, :, :], 1.0), writes=("C:ones8",))

                prn = [0]

                def next_pr():
                    prn[0] += 1
                    return prn[0] % 2

                rtmp = [sb(f"rtmp{i}", [128, 384], F32) for i in range(2)]
                bufs1 = (xst, sq, rstd, xn, ps_ss1, gmix, ones_bf, rtmp)
                evn = [0]

                def evac_copy(out_ap, in_ap, reads, writes):
                    evn[0] += 1
                    if evn[0] % 2:
                        P.op("act", lambda e: e.copy(out=out_ap, in_=in_ap), reads=reads, writes=writes)
                    else:
                        P.op("dve", lambda e: e.tensor_copy(out=out_ap, in_=in_ap), reads=reads, writes=writes)

                def seg_geo(si, s):
                    g = s // 16
                    d = DIL[g]
                    sp_ = s % 16
                    nsub = 16 // d
                    return g, d, sp_ // nsub, sp_ % nsub, si % 2

                def cast_chunk(ci):
                    cb = ci % 2
                    srcv = uv_h[256 * ci:256 * (ci + 1), :].rearrange("(p r) c -> p r c", r=2)
                    dstv = uv16_d[256 * ci:256 * (ci + 1), :].rearrange("(p r) c -> p r c", r=2)
                    P.dma("pool", lambda e: e.dma_start(out=cst[cb][:, :, :], in_=srcv), dsem(f"cst{cb}", "pool"), writes=(f"cst{cb}",))
                    P.dma("sp", lambda e: e.dma_start(out=dstv, in_=cst[cb][:, :, :]), dsem(f"cso{cb}"), reads=(f"cst{cb}",))

                def body1a(si, s):
                    g, d, r, sub, bi = seg_geo(si, s)
                    if si == 0:
                        rmsnorm_block(bufs1, xseg[s], bi, 384, "a")
                    if si + 1 < nseg:
                        rmsnorm_block(bufs1, xseg[segs[si + 1]], (si + 1) % 2, 384, "a")
                    for _ in range(2 if si % 3 == 0 else 1):
                        if castn[0] < 64:
                            cast_chunk(castn[0])
                            castn[0] += 1
                    xk = rmsnorm_block(bufs1, None, bi, 384, "a")
                    P.mark()
                    if g == 0:
                        P.dma("sp", lambda e: e.dma_start(out=rstd_d[:, 256 * s:256 * (s + 1)], in_=rstd[bi][:, 64:320]),
                              dsem(f"rsd{bi}"), reads=(f"ars{bi}",))

                    for j in range(4):
                        pj = next_pr()

                        def mm_k(e, j=j, pj=pj):
                            ins = None
                            for c in range(8):
                                ins = e.matmul(ps_pr[pj][:, 0:384], lhsT=wqkv[:, c, 1536 + 128 * j:1536 + 128 * (j + 1)],
                                               rhs=xn[bi][:, c, :], start=(c == 0), stop=(c == 7))
                            return ins
                        P.op("pe", mm_k, reads=tuple(xk) + ("C:W",), writes=(f"ps_pr{pj}",))
                        evac_copy(kT[bi][:, j, :], ps_pr[pj][:, 0:384], (f"ps_pr{pj}",), (f"kT{bi}.{j}",))
                    for j in range(4):
                        pj = next_pr()

                        def mm_q(e, j=j, pj=pj):
                            ins = None
                            for c in range(8):
                                ins = e.matmul(ps_pr[pj][:, 0:256], lhsT=wqkv[:, c, g * 512 + 128 * j:g * 512 + 128 * (j + 1)],
                                               rhs=xn[bi][:, c, 64:320], start=(c == 0), stop=(c == 7))
                            return ins
                        P.op("pe", mm_q, reads=tuple(xk) + ("C:W",), writes=(f"ps_pr{pj}",))
                        evac_copy(qT[bi][:, j, :], ps_pr[pj][:, 0:256], (f"ps_pr{pj}",), (f"qT{bi}.{j}",))
                    for j in range(3):
                        pj = next_pr()

                        def mm_v(e, j=j, pj=pj):
                            ins = None
                            for c in range(8):
                                ins = e.matmul(ps_pr[pj][:, :], lhsT=xn[bi][:, c, 128 * j:128 * (j + 1)],
                                               rhs=wqkv[:, c, 2048:2560], start=(c == 0), stop=(c == 7))
                            return ins
                        P.op("pe", mm_v, reads=tuple(xk) + ("C:W",), writes=(f"ps_pr{pj}",))
                        col = 3 * s + j
                        P.op("act", lambda e, j=j, pj=pj, col=col: e.activation(
                            out=vaug[bi][:, j, :, 0:64], in_=ps_pr[pj][:, :].rearrange("p (h e) -> p h e", h=8),
                            func=AF.Copy, scale=kval[:, col:col + 1]),
                            reads=(f"ps_pr{pj}", "C:W"), writes=(f"va{bi}.{j}",))
                        P.op("dve", lambda e, j=j, col=col: e.tensor_scalar(
                            out=vaug[bi][:, j, :, 64:65], in0=ones8[:, :, :],
                            scalar1=kval[:, col:col + 1], scalar2=None, op0=ALU.mult),
                            reads=("C:W", "C:ones8"), writes=(f"vb{bi}.{j}",))

                def body1b(si, s):
                    g, d, r, sub, bi = seg_geo(si, s)
                    obm = {}

                    def front(h):
                        jc, pb = h // 2, 64 * (h % 2)
                        ri = h % 2
                        coef = slopes[h] * d * 8.0

                        def mm_s(e, jc=jc, pb=pb, ri=ri):
                            kk = kT[bi][pb:pb + 64, jc, :]
                            qq = qT[bi][pb:pb + 64, jc, :]
                            e.matmul(ps_S[ri][:, 0:128], lhsT=kk[:, 0:128], rhs=qq[:, 0:128], start=True, stop=True)
                            e.matmul(ps_S[ri][:, 128:384], lhsT=kk[:, 128:256], rhs=qq[:, 0:256], start=True, stop=True)
                            return e.matmul(ps_S[ri][:, 384:512], lhsT=kk[:, 256:384], rhs=qq[:, 128:256], start=True, stop=True)
                        P.op("pe", mm_s, reads=(f"kT{bi}.{jc}", f"qT{bi}.{jc}"), writes=(f"ps_S{ri}",))
                        P.op("dve", lambda e, ri=ri, coef=coef: e.scalar_tensor_tensor(
                            out=tt[ri][:, :], in0=negabs[:, :], scalar=coef, in1=ps_S[ri][:, :], op0=ALU.mult, op1=ALU.add),
                            reads=(f"ps_S{ri}", "C:W"), writes=(f"tt{ri}",))
                        P.op("act", lambda e, ri=ri: e.activation(out=pT[ri][:, :], in_=tt[ri][:, :], func=AF.Exp, scale=0.125),
                             reads=(f"tt{ri}",), writes=(f"pT{ri}",))

                    def back(h):
                        ri = h % 2
                        hg = h // 4
                        if h % 4 == 0:
                            for qt in range(2):
                                ocnt[0] += 1
                                obm[qt] = ocnt[0] % 3
                        for qt in range(2):
                            ob = obm[qt]
                            c0 = (h % 4) * 65

                            def mm_o(e, qt=qt, ob=ob, c0=c0, h=h, ri=ri):
                                e.matmul(ps_O[ob][:, c0:c0 + 65], lhsT=pT[ri][:, 256 * qt:256 * qt + 128],
                                         rhs=vaug[bi][:, qt, h, :], start=True, stop=False)
                                return e.matmul(ps_O[ob][:, c0:c0 + 65], lhsT=pT[ri][:, 256 * qt + 128:256 * qt + 256],
                                                rhs=vaug[bi][:, qt + 1, h, :], start=False, stop=True)
                            P.op("pe", mm_o, reads=(f"pT{ri}", f"va{bi}.{qt}", f"vb{bi}.{qt}", f"va{bi}.{qt + 1}", f"vb{bi}.{qt + 1}"),
                                 writes=(f"ps_O{ob}",))
                        if h % 4 == 3:
                            for qt in range(2):
                                ob = obm[qt]
                                evac_copy(osb[bi][:, qt, 260 * hg:260 * (hg + 1)], ps_O[ob][:, 0:260],
                                          (f"ps_O{ob}",), (f"osb{bi}.{qt}.{hg}",))

                    front(0)
                    for h in range(8):
                        if h + 1 < 8:
                            front(h + 1)
                        back(h)
                    P.mark()
                    for qt in range(2):
                        m0 = 256 * sub + 128 * qt
                        lo = d * m0 + r
                        dst = att_d[g][lo:lo + d * 127 + 1:d, :]
                        P.dma("sp", lambda e, dst=dst, qt=qt: e.dma_start(out=dst, in_=osb[bi][:, qt, :]),
                              dsem(f"osb{bi}.{qt}"), reads=(f"osb{bi}.{qt}.0", f"osb{bi}.{qt}.1"), writes=())
                recA = [P.split(P.capture(body1a, si_, s_)) for si_, s_ in enumerate(segs)]
                extras = []
                while castn[0] < 64:
                    extras.append(P.capture(cast_chunk, castn[0]))
                    castn[0] += 1
                recB = [P.split(P.capture(body1b, si_, s_)) for si_, s_ in enumerate(segs)]
                P.pipeline([recA[i] + recB[i] for i in range(nseg)])
                for extra in extras:
                    P.replay(extra)
                P.drain_dma("sp")
                P.drain_dma("pool")
                P.emit(nc, "p1")

        if nblk > 0:
            with ExitStack() as es:
                sb = lambda name, shape, dtp: es.enter_context(nc.sbuf_tensor("sb_" + name, shape, dtp))
                ps = lambda name, shape, dtp=F32: es.enter_context(nc.psum_tensor("pp_" + name, shape, dtp))
                w_uv = sb("w_uv", [128, 8, 2048], BF16)
                w_a = sb("w_a", [128, 8, 1024], BF16)
                wsT = sb("wsT", [128, 8, 128], BF16)
                bsbc = sb("bsbc", [128, 1024], F32)
                mxb = [sb(f"mxb{i}", [128, 512], F32) for i in range(2)]
                gsgu = sb("gsgu", [128, 1024], F32)
                gmix = sb("gmix2", [128, 8], F32)
                ones_bf = sb("ones2", [128, 128], BF16)
                xst = [sb(f"bxst{i}", [128, 8, 256], F32) for i in range(2)]
                sq = [sb(f"bsq{i}", [128, 8, 256], BF16) for i in range(2)]
                rstd = [sb(f"brstd{i}", [128, 256], F32) for i in range(2)]
                xn = [sb(f"bxn{i}", [128, 8, 256], BF16) for i in range(2)]
                uT = [sb(f"uT{i}", [128, 8, 256], BF16) for i in range(2)]
                vg = [sb(f"vg{i}", [128, 1024], F32) for i in range(4)]
                junkr = [sb(f"junk{i}", [128, 1024], BF16) for i in range(4)]
                jn = [0]

                def nj():
                    jn[0] += 1
                    return jn[0] % 4
                ssv4 = sb("ssv4", [128, 4], F32)
                vn = [sb(f"vn{i}", [128, 1024], BF16) for i in range(4)]
                yaT = [sb(f"yaT{i}", [128, 8, 256], BF16) for i in range(2)]
                atsb = [sb(f"atsb{i}", [128, 8, 256], F32) for i in range(2)]
                ps_ss = ps("b_ss", [128, 512])
                ps_pr = [ps(f"b_pr{i}", [128, 512]) for i in range(2)]
                ps_mx = [ps(f"b_mx{i}", [128, 512]) for i in range(2)]
                ps_at = [ps(f"b_at{i}", [128, 512]) for i in range(2)]

                load_cast(w_uv[:, :, :], w_in_h[:, :, 0:2048], piece=1024)
                load_cast(w_a[:, :, :], wa_h[:, :, :], piece=1024)
                load_cast(wsT[:, :, :], wsT_h[:, :, :], piece=128)
                for dst_, src_ in ((bsbc[:, :], bsbc_h), (gsgu[:, :], gsgu_h), (gmix[:, :], gmix_h)):
                    P.dma("sp", lambda e, d_=dst_, s_=src_: e.dma_start(out=d_, in_=s_), dsem("w"), writes=("C:W",))
                P.op("dve", lambda e: e.memset(ones_bf[:, :], 1.0), writes=("C:ones",))
                prn = [0]
                rtmp = [sb(f"brtmp{i}", [128, 256], F32) for i in range(2)]
                bufs2 = (xst, sq, rstd, xn, ps_ss, gmix, ones_bf, rtmp)

                def body2(nb):
                    bi = nb % 2
                    if nb == 0:
                        rmsnorm_block(bufs2, xseg[nb][:, :, 64:320], bi, 256, "b", rs_src=rstd_d[:, 256 * nb:256 * (nb + 1)])
                    if nb + 1 < nblk:
                        rmsnorm_block(bufs2, xseg[nb + 1][:, :, 64:320], (nb + 1) % 2, 256, "b",
                                      rs_src=rstd_d[:, 256 * (nb + 1):256 * (nb + 2)])
                    xk = rmsnorm_block(bufs2, None, bi, 256, "b", rs_loaded=True)
                    P.mark()
                    for j in range(8):
                        prn[0] += 1
                        pj = prn[0] % 2

                        def mm_u(e, j=j, pj=pj):
                            ins = None
                            for c in range(8):
                                ins = e.matmul(ps_pr[pj][:, 0:256], lhsT=w_uv[:, c, 128 * j:128 * (j + 1)],
                                               rhs=xn[bi][:, c, :], start=(c == 0), stop=(c == 7))
                            return ins
                        P.op("pe", mm_u, reads=tuple(xk) + ("C:W",), writes=(f"b_pr{pj}",))
                        P.op("act", lambda e, j=j, pj=pj: e.activation(out=uT[bi][:, j, :], in_=ps_pr[pj][:, 0:256], func=AF.Gelu),
                             reads=(f"b_pr{pj}",), writes=(f"uT{bi}.{j}",))
                    for t2 in range(2):
                        vi = (nb * 2 + t2) % 4
                        for hh in range(2):
                            prn[0] += 1
                            pj = prn[0] % 2

                            def mm_v(e, t2=t2, hh=hh, pj=pj):
                                ins = None
                                for c in range(8):
                                    ins = e.matmul(ps_pr[pj][:, :], lhsT=xn[bi][:, c, 128 * t2:128 * (t2 + 1)],
                                                   rhs=w_uv[:, c, 1024 + 512 * hh:1024 + 512 * (hh + 1)],
                                                   start=(c == 0), stop=(c == 7))
                                return ins
                            P.op("pe", mm_v, reads=tuple(xk) + ("C:W",), writes=(f"b_pr{pj}",))
                            P.op("act", lambda e, hh=hh, pj=pj, vi=vi: e.activation(
                                out=vg[vi][:, 512 * hh:512 * (hh + 1)], in_=ps_pr[pj][:, :], func=AF.Gelu),
                                reads=(f"b_pr{pj}",), writes=(f"vg{vi}.{hh}",))
                        ji = nj()
                        P.op("dve", lambda e, vi=vi, ji=ji: e.scalar_tensor_tensor(
                            out=junkr[ji][:, :], in0=vg[vi][:, :], scalar=1.0, in1=vg[vi][:, :], op0=ALU.mult, op1=ALU.mult,
                            accum_out=ssv4[:, vi:vi + 1]),
                            reads=(f"vg{vi}.0", f"vg{vi}.1"), writes=(f"ssv{vi}", f"junk{ji}"))
                    v0 = (nb * 2) % 4
                    sk = (f"ssv{v0}", f"ssv{v0 + 1}")
                    P.op("dve", lambda e: e.tensor_scalar(out=ssv4[:, v0:v0 + 2], in0=ssv4[:, v0:v0 + 2], scalar1=1.0 / D, scalar2=EPS,
                                                          op0=ALU.mult, op1=ALU.add), reads=sk, writes=sk)
                    P.op("act", lambda e: e.activation(out=ssv4[:, v0:v0 + 2], in_=ssv4[:, v0:v0 + 2], func=AF.Ln), reads=sk, writes=sk)
                    P.op("act", lambda e: e.activation(out=ssv4[:, v0:v0 + 2], in_=ssv4[:, v0:v0 + 2], func=AF.Exp, scale=-0.5),
                         reads=sk, writes=sk)
                    for t2 in range(2):
                        vi = (nb * 2 + t2) % 4
                        P.op("dve", lambda e, vi=vi: e.scalar_tensor_tensor(
                            out=vn[vi][:, :], in0=vg[vi][:, :], scalar=ssv4[:, vi:vi + 1], in1=gsgu[:, :], op0=ALU.mult, op1=ALU.mult),
                            reads=(f"vg{vi}.0", f"vg{vi}.1", f"ssv{vi}", "C:W"), writes=(f"vn{vi}",))
                    P.mark()
                    for t2 in range(2):
                        vi = (nb * 2 + t2) % 4
                        for gq in range(2):
                            mi = gq

                            def mm_mix(e, gq=gq, mi=mi, vi=vi):
                                ins = None
                                for g4 in range(4):
                                    gg = gq * 4 + g4
                                    ins = e.matmul(ps_mx[mi][:, 128 * g4:128 * (g4 + 1)], lhsT=vn[vi][:, 128 * gg:128 * (gg + 1)],
                                                   rhs=wsT[:, gg, :], start=True, stop=True)
                                return ins
                            P.op("pe", mm_mix, reads=(f"vn{vi}", "C:W"), writes=(f"b_mx{mi}",))
                            P.op("dve", lambda e, gq=gq, mi=mi: e.tensor_tensor(
                                out=mxb[mi][:, :], in0=ps_mx[mi][:, :], in1=bsbc[:, 512 * gq:512 * (gq + 1)], op=ALU.add),
                                reads=(f"b_mx{mi}", "C:W"), writes=(f"mxb{mi}",))
                            P.op("dve", lambda e, gq=gq, mi=mi, t2=t2: e.tensor_tensor(
                                out=yaT[bi][:, 4 * gq:4 * gq + 4, 128 * t2:128 * (t2 + 1)],
                                in0=mxb[mi][:, :].rearrange("p (g t) -> p g t", g=4),
                                in1=uT[bi][:, 4 * gq:4 * gq + 4, 128 * t2:128 * (t2 + 1)], op=ALU.mult),
                                reads=(f"mxb{mi}",) + tuple(f"uT{bi}.{4 * gq + q}" for q in range(4)),
                                writes=(f"yaT{bi}.{gq}.{t2}",))
                    yk = tuple(f"yaT{bi}.{gq}.{t2}" for gq in range(2) for t2 in range(2))
                    for j in range(8):
                        pj = j % 2

                        def mm_a(e, j=j, pj=pj):
                            ins = None
                            for c in range(8):
                                ins = e.matmul(ps_at[pj][:, 0:256], lhsT=w_a[:, c, 128 * j:128 * (j + 1)],
                                               rhs=yaT[bi][:, c, :], start=(c == 0), stop=(c == 7))
                            return ins
                        P.op("pe", mm_a, reads=yk + ("C:W",), writes=(f"b_at{pj}",))
                        if j % 2:
                            P.op("act", lambda e, j=j, pj=pj: e.copy(out=atsb[bi][:, j, :], in_=ps_at[pj][:, 0:256]),
                                 reads=(f"b_at{pj}",), writes=(f"atsb{bi}.{j}",))
                        else:
                            P.op("dve", lambda e, j=j, pj=pj: e.tensor_copy(out=atsb[bi][:, j, :], in_=ps_at[pj][:, 0:256]),
                                 reads=(f"b_at{pj}",), writes=(f"atsb{bi}.{j}",))
                    P.mark()
                    P.dma("sp", lambda e, nb=nb: e.dma_start(out=at_d[:, :, 256 * nb:256 * (nb + 1)], in_=atsb[bi][:, :, :]),
                          dsem(f"atsb{bi}"), reads=tuple(f"atsb{bi}.{j}" for j in range(8)), writes=())
                P.pipeline([P.split(P.capture(body2, nb_)) for nb_ in range(nblk)])
                P.drain_dma("sp")
                P.drain_dma("pool")
                P.emit(nc, "p2a")

        if nblk > 0:
            with ExitStack() as es:
                sb = lambda name, shape, dtp: es.enter_context(nc.sbuf_tensor("sb_" + name, shape, dtp))
                ps = lambda name, shape, dtp=F32: es.enter_context(nc.psum_tensor("pp_" + name, shape, dtp))
                w_g = sb("w_g", [128, 8, 2048], BF16)
                w_b = sb("w_b", [128, 4, 1024], BF16)
                w_o = sb("w_o", [128, 8, 1024], BF16)
                ident = sb("identb", [128, 128], BF16)
                gmix = sb("gmix3", [128, 8], F32)
                ones_bf = sb("ones3", [128, 128], BF16)
                xst = [sb(f"cxst{i}", [128, 8, 256], F32) for i in range(2)]
                sq = [sb(f"csq{i}", [128, 8, 256], BF16) for i in range(2)]
                rstd = [sb(f"crstd{i}", [128, 256], F32) for i in range(2)]
                xn = [sb(f"cxn{i}", [128, 8, 256], BF16) for i in range(2)]
                sg = [sb(f"sg{i}", [128, 16, 256], BF16) for i in range(2)]
                a3 = [sb(f"a3{i}", [128, 3, 520], F32) for i in range(2)]
                s2 = [sb(f"s2{i}", [128, 520], F32) for i in range(2)]
                rden = [sb(f"rden{i}", [128, 8, 1], F32) for i in range(2)]
                yb = [sb(f"yb{i}", [128, 512], BF16) for i in range(2)]
                ybT = [sb(f"ybT{i}", [128, 4, 256], BF16) for i in range(2)]
                atl = [sb(f"atl{i}", [128, 8, 256], F32) for i in range(2)]
                tmpa = [sb(f"tmpa{i}", [128, 256], F32) for i in range(2)]
                tmpb = [sb(f"tmpb{i}", [128, 256], F32) for i in range(2)]
                mT = [sb(f"mT{i}", [128, 8, 256], BF16) for i in range(2)]
                xtk = [sb(f"xtk{i}", [128, 1024], F32) for i in range(4)]
                x2 = [sb(f"x2{i}", [128, 1024], F32) for i in range(4)]
                ps_ss = ps("c_ss", [128, 512])
                ps_pr = [ps(f"c_pr{i}", [128, 512]) for i in range(2)]
                ps_T = ps("c_T", [128, 4, 128], BF16)
                ps_B = [ps(f"c_B{i}", [128, 512]) for i in range(2)]
                ps_o = [ps(f"c_o{i}", [128, 512]) for i in range(2)]

                load_cast(w_g[:, :, :], w_in_h[:, :, 4608:6656], piece=1024)
                load_cast(w_b[:, :, :], wb_h[:, :, :], piece=1024)
                load_cast(w_o[:, :, :], wo_h[:, :, :], piece=1024)
                load_cast(ident[:, :], ident_h, piece=128)
                P.dma("sp", lambda e: e.dma_start(out=gmix[:, :], in_=gmix_h), dsem("w"), writes=("C:W",))
                P.op("dve", lambda e: e.memset(ones_bf[:, :], 1.0), writes=("C:ones",))
                prn = [0]
                rtmp = [sb(f"crtmp{i}", [128, 256], F32) for i in range(2)]
                bufs3 = (xst, sq, rstd, xn, ps_ss, gmix, ones_bf, rtmp)

                def body3(nb):
                    bi = nb % 2
                    if nb == 0:
                        rmsnorm_block(bufs3, xseg[nb][:, :, 64:320], bi, 256, "c", rs_src=rstd_d[:, 256 * nb:256 * (nb + 1)])
                    if nb + 1 < nblk:
                        rmsnorm_block(bufs3, xseg[nb + 1][:, :, 64:320], (nb + 1) % 2, 256, "c",
                                      rs_src=rstd_d[:, 256 * (nb + 1):256 * (nb + 2)])
                    xk = rmsnorm_block(bufs3, None, bi, 256, "c", rs_loaded=True)
                    P.mark()
                    for j in range(16):
                        prn[0] += 1
                        pj = prn[0] % 2

                        def mm_g(e, j=j, pj=pj):
                            ins = None
                            for c in range(8):
                                ins = e.matmul(ps_pr[pj][:, 0:256], lhsT=w_g[:, c, 128 * j:128 * (j + 1)],
                                               rhs=xn[bi][:, c, :], start=(c == 0), stop=(c == 7))
                            return ins
                        P.op("pe", mm_g, reads=tuple(xk) + ("C:W",), writes=(f"c_pr{pj}",))
                        P.op("act", lambda e, j=j, pj=pj: e.activation(out=sg[bi][:, j, :], in_=ps_pr[pj][:, 0:256], func=AF.Sigmoid),
                             reads=(f"c_pr{pj}",), writes=(f"sg{bi}.{j}",))
                    P.dma("sp", lambda e, nb=nb: e.dma_start(out=atl[bi][:, :, :], in_=at_d[:, :, 256 * nb:256 * (nb + 1)]),
                          dsem(f"atl{bi}"), writes=(f"atl{bi}",))
                    for t2 in range(2):
                        ti = (nb * 2 + t2) % 2
                        t0 = 256 * nb + 128 * t2
                        P.dma("sp", lambda e, ti=ti, t0=t0: e.dma_start(
                            out=a3[ti][:, :, :], in_=att_d[:, t0:t0 + 128, :].rearrange("g t c -> t g c")),
                            dsem(f"a3{ti}"), writes=(f"a3{ti}",))
                        xi = (nb * 2 + t2) % 4
                        P.dma("sp", lambda e, xi=xi, t0=t0: e.dma_start(out=xtk[xi][:, :], in_=xtok_h[t0:t0 + 128, :]),
                              dsem(f"xtk{xi}"), writes=(f"xtk{xi}",))
                        P.op("dve", lambda e, ti=ti: e.tensor_tensor(out=s2[ti][:, :], in0=a3[ti][:, 0, :], in1=a3[ti][:, 1, :], op=ALU.add),
                             reads=(f"a3{ti}",), writes=(f"s2{ti}",))
                        P.op("dve", lambda e, ti=ti: e.tensor_tensor(out=s2[ti][:, :], in0=s2[ti][:, :], in1=a3[ti][:, 2, :], op=ALU.add),
                             reads=(f"a3{ti}", f"s2{ti}"), writes=(f"s2{ti}",))
                        P.op("dve", lambda e, ti=ti: e.reciprocal(
                            out=rden[ti][:, :, :], in_=s2[ti][:, :].rearrange("p (h e) -> p h e", h=8)[:, :, 64:65]),
                            reads=(f"s2{ti}",), writes=(f"rden{ti}",))
                        P.op("dve", lambda e, ti=ti: e.tensor_tensor(
                            out=yb[ti][:, :].rearrange("p (h e) -> p h e", h=8),
                            in0=s2[ti][:, :].rearrange("p (h e) -> p h e", h=8)[:, :, 0:64],
                            in1=rden[ti][:, :, :].to_broadcast([128, 8, 64]), op=ALU.mult),
                            reads=(f"s2{ti}", f"rden{ti}"), writes=(f"yb{ti}",))

                        def tr_y(e, ti=ti):
                            ins = None
                            for j in range(4):
                                ins = e.transpose(ps_T[:, j, :], yb[ti][:, 128 * j:128 * (j + 1)], ident[:, :])
                            return ins
                        P.op("pe", tr_y, reads=(f"yb{ti}", "C:W"), writes=("c_T",))
                        P.op("act", lambda e, t2=t2: e.copy(out=ybT[bi][:, :, 128 * t2:128 * (t2 + 1)], in_=ps_T[:, :, :]),
                             reads=("c_T",), writes=(f"ybT{bi}.{t2}",))
                    P.mark()
                    for j in range(8):
                        pj = j % 2

                        def mm_b(e, j=j, pj=pj):
                            ins = None
                            for c in range(4):
                                ins = e.matmul(ps_B[pj][:, 0:256], lhsT=w_b[:, c, 128 * j:128 * (j + 1)],
                                               rhs=ybT[bi][:, c, :], start=(c == 0), stop=(c == 3))
                            return ins
                        P.op("pe", mm_b, reads=(f"ybT{bi}.0", f"ybT{bi}.1", "C:W"), writes=(f"c_B{pj}",))
                        P.op("dve", lambda e, j=j, pj=pj: e.tensor_tensor(out=tmpa[pj][:, :], in0=atl[bi][:, j, :], in1=sg[bi][:, j, :], op=ALU.mult),
                             reads=(f"atl{bi}", f"sg{bi}.{j}"), writes=(f"tmpa{pj}",))
                        P.op("dve", lambda e, j=j, pj=pj: e.tensor_tensor(out=tmpb[pj][:, :], in0=ps_B[pj][:, 0:256], in1=sg[bi][:, 8 + j, :], op=ALU.mult),
                             reads=(f"c_B{pj}", f"sg{bi}.{8 + j}"), writes=(f"tmpb{pj}",))
                        P.op("dve", lambda e, j=j, pj=pj: e.tensor_tensor(out=mT[bi][:, j, :], in0=tmpa[pj][:, :], in1=tmpb[pj][:, :], op=ALU.add),
                             reads=(f"tmpa{pj}", f"tmpb{pj}"), writes=(f"mT{bi}.{j}",))
                    mk = tuple(f"mT{bi}.{j}" for j in range(8))
                    for t2 in range(2):
                        ti = (nb * 2 + t2) % 2
                        t0 = 256 * nb + 128 * t2
                        for hh in range(2):
                            def mm_o2(e, t2=t2, hh=hh):
                                ins = None
                                for c in range(8):
                                    ins = e.matmul(ps_o[hh][:, :], lhsT=mT[bi][:, c, 128 * t2:128 * (t2 + 1)],
                                                   rhs=w_o[:, c, 512 * hh:512 * (hh + 1)], start=(c == 0), stop=(c == 7))
                                return ins
                            P.op("pe", mm_o2, reads=mk + ("C:W",), writes=(f"c_o{hh}",))
                            xi = (nb * 2 + t2) % 4
                            P.op("dve", lambda e, hh=hh, xi=xi: e.tensor_tensor(
                                out=x2[xi][:, 512 * hh:512 * (hh + 1)], in0=ps_o[hh][:, :], in1=xtk[xi][:, 512 * hh:512 * (hh + 1)], op=ALU.add),
                                reads=(f"c_o{hh}", f"xtk{xi}"), writes=(f"x2{xi}.{hh}",))
                    P.mark()
                    for t2 in range(2):
                        xi = (nb * 2 + t2) % 4
                        t0 = 256 * nb + 128 * t2
                        P.dma("sp", lambda e, xi=xi, t0=t0: e.dma_start(out=x2_d[t0:t0 + 128, :], in_=x2[xi][:, :]),
                              dsem(f"x2{xi}"), reads=(f"x2{xi}.0", f"x2{xi}.1"), writes=())
                P.pipeline([P.split(P.capture(body3, nb_)) for nb_ in range(nblk)])
                P.drain_dma("sp")
                P.drain_dma("pool")
                P.emit(nc, "p2b")

        if ntile > 0:
            with ExitStack() as es:
                sb = lambda name, shape, dtp: es.enter_context(nc.sbuf_tensor("sb_" + name, shape, dtp))
                ps = lambda name, shape, dtp=F32: es.enter_context(nc.psum_tensor("pp_" + name, shape, dtp))
                NB = 13
                NDG = 4
                wq = sb("wq", [128, 8, 2048], BF16)
                skT = sb("skT", [128, 16, 128], BF16)
                ident = sb("identp", [128, 128], BF16)
                gffn = sb("gffn", [128, 1024], F32)
                gfin = sb("gfin", [128, 1024], F32)
                iota16 = sb("iota16", [128, 16], F32)
                thr16 = sb("thr16", [128, 16], F32)
                posf = sb("posf", [128, 8, 16], F32)
                x2t = [sb(f"x2t{i}", [128, 1024], F32) for i in range(3)]
                hn = [sb(f"hn{i}", [128, 1024], F32) for i in range(3)]
                hnb = sb("hnb", [128, 1024], BF16)
                hnT = sb("hnT", [128, 8, 128], BF16)
                qTp = sb("qTp", [128, 16, 128], BF16)
                junkr = [sb(f"junkp{i}", [128, 1024], BF16) for i in range(2)]
                jn = [0]

                def nj():
                    jn[0] += 1
                    return jn[0] % 2
                st1 = sb("st1", [128, 1], F32)
                S_sbs = [sb(f"S_sb{i}", [128, 16, 128], F32) for i in range(2)]
                S2 = sb("S2", [128, 16, 128], F32)
                tops = sb("top", [128, 16, 16], F32)
                idxu = sb("idxu", [128, 16, 16], U32)
                idxf = sb("idxf", [128, 16, 16], F32)
                cand = sb("cand", [128, 8, 256], F32)
                cand2 = sb("cand2", [128, 8, 256], F32)
                best = sb("best", [128, 8, 16], F32)
                posu = sb("posu", [128, 8, 16], U32)
                pa_u = sb("pa_u", [128, 8, 16], U32)
                pb_u = sb("pb_u", [128, 8, 16], U32)
                pa_f = sb("pa_f", [128, 8, 16], F32)
                pb_f = sb("pb_f", [128, 8, 16], F32)
                eq = sb("eq", [128, 128, 16], F32)
                i1s = sb("i1s", [128, 128], F32)
                i2s = sb("i2s", [128, 128], F32)
                expf = sb("expf", [128, 128], F32)
                expu = [sb(f"expu{i}", [128, 128], U32) for i in range(2)]
                gate = [sb(f"gate{i}", [128, 8, 16], F32) for i in range(2)]
                gsum = sb("gsum", [128, 8, 1], F32)
                aval = sb("aval", [128, 128], F32)
                gval = sb("gval", [128, 128], F32)
                wval = sb("wval", [128, 128], F32)
                gbuf = [sb(f"gbuf{i}", [128, 2048], BF16) for i in range(NB)]
                dg = [sb(f"dg{i}", [128, 128], BF16) for i in range(NDG)]
                ident_f = sb("ident_f", [128, 128], F32)
                st2 = sb("st2", [128, 1], F32)
                x3 = sb("x3", [128, 1024], F32)
                yo = [sb(f"yo{i}", [128, 1024], F32) for i in range(2)]
                ps_T = ps("p_T", [128, 8, 128], BF16)
                ps_q = [ps(f"p_q{i}", [128, 512]) for i in range(2)]
                ps_Sc = [ps(f"p_S{i}", [128, 512]) for i in range(2)]
                ps_acc = [ps(f"p_acc{i}", [128, 512]) for i in range(2)]

                load_cast(wq[:, :, :], wq_h[:, :, :], piece=1024)
                load_cast(skT[:, :, :], skT_h[:, :, :], piece=128)
                load_cast(ident[:, :], ident_h, piece=128)
                for dst_, src_ in ((gffn[:, :], gffn_h), (gfin[:, :], gfin_h), (iota16[:, :], iota_h), (thr16[:, :], thr_h),
                                   (ident_f[:, :], ident_h)):
                    P.dma("sp", lambda e, d_=dst_, s_=src_: e.dma_start(out=d_, in_=s_), dsem("w"), writes=("C:W",))

                def rms_rstd(src, dst1, tag):
                    P.op("act", lambda e: e.activation(out=hnb[:, :], in_=src, func=AF.Square, accum_out=dst1),
                         reads=(tag,), writes=(tag + "r", "hnb"))
                    P.op("dve", lambda e: e.tensor_scalar(out=dst1, in0=dst1, scalar1=1.0 / D, scalar2=EPS, op0=ALU.mult, op1=ALU.add),
                         reads=(tag + "r",), writes=(tag + "r",))
                    P.op("act", lambda e: e.activation(out=dst1, in_=dst1, func=AF.Ln), reads=(tag + "r",), writes=(tag + "r",))
                    P.op("act", lambda e: e.activation(out=dst1, in_=dst1, func=AF.Exp, scale=-0.5), reads=(tag + "r",), writes=(tag + "r",))

                gcount = [0]
                def load_x2(tI):
                    P.dma("sp", lambda e, ti=tI % 3, t0=128 * tI: e.dma_start(out=x2t[ti][:, :], in_=x2_d[t0:t0 + 128, :]),
                          dsem(f"x2t{tI % 3}"), writes=(f"x2t{tI % 3}",))
                def body4a(tI):
                    ti = tI % 2
                    t3 = tI % 3
                    S_sb = S_sbs[tI % 2]
                    sk_ = f"S_sb{tI % 2}"
                    t0 = 128 * tI
                    load_x2(tI)
                    rms_rstd(x2t[t3][:, :], st1[:, :], f"x2t{t3}")
                    P.op("dve", lambda e: e.scalar_tensor_tensor(out=hn[t3][:, :], in0=x2t[t3][:, :], scalar=st1[:, 0:1], in1=gffn[:, :],
                                                                 op0=ALU.mult, op1=ALU.mult),
                         reads=(f"x2t{t3}", f"x2t{t3}r", "C:W"), writes=(f"hn{t3}",))
                    P.op("act", lambda e: e.copy(out=hnb[:, :], in_=hn[t3][:, :]), reads=(f"hn{t3}",), writes=("hnb",))

                    def tr_h(e):
                        ins = None
                        for c in range(8):
                            ins = e.transpose(ps_T[:, c, :], hnb[:, 128 * c:128 * (c + 1)], ident[:, :])
                        return ins
                    P.op("pe", tr_h, reads=("hnb", "C:W"), writes=("p_T",))
                    P.op("act", lambda e: e.copy(out=hnT[:, :, :], in_=ps_T[:, :, :]), reads=("p_T",), writes=("hnT",))
                    for qg in range(4):
                        pj = qg % 2

                        def mm_pq(e, qg=qg, pj=pj):
                            ins = None
                            for q4 in range(4):
                                hp = qg * 4 + q4
                                for c in range(8):
                                    ins = e.matmul(ps_q[pj][:, 128 * q4:128 * (q4 + 1)], lhsT=wq[:, c, 128 * hp:128 * (hp + 1)],
                                                   rhs=hnT[:, c, :], start=(c == 0), stop=(c == 7))
                            return ins
                        P.op("pe", mm_pq, reads=("hnT", "C:W"), writes=(f"p_q{pj}",))
                        P.op("act", lambda e, qg=qg, pj=pj: e.copy(out=qTp[:, 4 * qg:4 * qg + 4, :],
                                                                  in_=ps_q[pj][:, :].rearrange("p (a b) -> p a b", a=4)),
                             reads=(f"p_q{pj}",), writes=(f"qTp.{qg}",))
                    for qg in range(4):
                        def mm_ps(e, qg=qg):
                            ins = None
                            for q4 in range(4):
                                hp = qg * 4 + q4
                                ins = e.matmul(ps_Sc[qg % 2][:, 128 * q4:128 * (q4 + 1)], lhsT=qTp[:, hp, :], rhs=skT[:, hp, :],
                                               start=True, stop=True)
                            return ins
                        P.op("pe", mm_ps, reads=(f"qTp.{qg}", "C:W"), writes=(f"p_S{qg % 2}",))
                        P.op("act", lambda e, qg=qg: e.copy(out=S_sb[:, 4 * qg:4 * qg + 4, :],
                                                           in_=ps_Sc[qg % 2][:, :].rearrange("p (a b) -> p a b", a=4)),
                             reads=(f"p_S{qg % 2}",), writes=(f"{sk_}.{qg}",))
                    P.mark()
                    for hp in range(16):
                        kq = f"{sk_}.{hp // 4}"
                        P.op("dve", lambda e, hp=hp: e.max(out=tops[:, hp, 0:8], in_=S_sb[:, hp, :]), reads=(kq,), writes=(f"top{hp}a",))
                        P.op("dve", lambda e, hp=hp: e.match_replace(out=S2[:, hp, :], in_to_replace=tops[:, hp, 0:8], in_values=S_sb[:, hp, :],
                                                                    imm_value=-1e30),
                             reads=(kq, f"top{hp}a"), writes=(f"S2.{hp}",))
                        P.op("dve", lambda e, hp=hp: e.max(out=tops[:, hp, 8:16], in_=S2[:, hp, :]), reads=(f"S2.{hp}",), writes=(f"top{hp}b",))
                        P.op("dve", lambda e, hp=hp: e.max_index(out=idxu[:, hp, 0:8], in_max=tops[:, hp, 0:8], in_values=S_sb[:, hp, :]),
                             reads=(kq, f"top{hp}a"), writes=(f"idx{hp}a",))
                        P.op("dve", lambda e, hp=hp: e.max_index(out=idxu[:, hp, 8:16], in_max=tops[:, hp, 8:16], in_values=S_sb[:, hp, :]),
                             reads=(kq, f"top{hp}b"), writes=(f"idx{hp}b",))
                    allidx = tuple(f"idx{hp}{x}" for hp in range(16) for x in "ab")
                    alltop = tuple(f"top{hp}{x}" for hp in range(16) for x in "ab")
                    P.op("dve", lambda e: e.tensor_copy(out=idxf[:, :, :], in_=idxu[:, :, :]), reads=allidx, writes=("idxf",))
                    topv = tops[:, :, :].rearrange("p (h t) k -> p h t k", t=2)
                    P.op("dve", lambda e: e.tensor_tensor(
                        out=cand[:, :, :].rearrange("p h (a b) -> p h a b", a=16),
                        in0=topv[:, :, 0, :].unsqueeze(3).to_broadcast([128, 8, 16, 16]),
                        in1=topv[:, :, 1, :].unsqueeze(2).to_broadcast([128, 8, 16, 16]), op=ALU.add),
                        reads=alltop, writes=("cand",))
                    for h in range(8):
                        P.op("dve", lambda e, h=h: e.max(out=best[:, h, 0:8], in_=cand[:, h, :]), reads=("cand",), writes=(f"best{h}a",))
                        P.op("dve", lambda e, h=h: e.match_replace(out=cand2[:, h, :], in_to_replace=best[:, h, 0:8], in_values=cand[:, h, :],
                                                                  imm_value=-1e30),
                             reads=("cand", f"best{h}a"), writes=(f"cand2.{h}",))
                        P.op("dve", lambda e, h=h: e.max(out=best[:, h, 8:16], in_=cand2[:, h, :]), reads=(f"cand2.{h}",), writes=(f"best{h}b",))
                        P.op("dve", lambda e, h=h: e.max_index(out=posu[:, h, 0:8], in_max=best[:, h, 0:8], in_values=cand[:, h, :]),
                             reads=("cand", f"best{h}a"), writes=(f"pos{h}a",))
                        P.op("dve", lambda e, h=h: e.max_index(out=posu[:, h, 8:16], in_max=best[:, h, 8:16], in_values=cand[:, h, :]),
                             reads=("cand", f"best{h}b"), writes=(f"pos{h}b",))
                    allpos = tuple(f"pos{h}{x}" for h in range(8) for x in "ab")
                    allbest = tuple(f"best{h}{x}" for h in range(8) for x in "ab")
                    gi = tI % 2
                    P.op("dve", lambda e, gi=gi: e.tensor_tensor(out=gate[gi][:, :, :], in0=best[:, :, :],
                                                                in1=best[:, :, 0:1].to_broadcast([128, 8, 16]), op=ALU.subtract),
                         reads=allbest, writes=(f"gate{gi}",))
                    P.op("act", lambda e, gi=gi: e.activation(out=gate[gi][:, :, :], in_=gate[gi][:, :, :], func=AF.Exp),
                         reads=(f"gate{gi}",), writes=(f"gate{gi}",))
                    P.op("dve", lambda e, gi=gi: e.tensor_reduce(out=gsum[:, :, :], in_=gate[gi][:, :, :], axis=AX.X, op=ALU.add),
                         reads=(f"gate{gi}",), writes=("gsum",))
                    P.op("dve", lambda e: e.reciprocal(out=gsum[:, :, :], in_=gsum[:, :, :]), reads=("gsum",), writes=("gsum",))
                    P.op("dve", lambda e, gi=gi: e.tensor_tensor(out=gate[gi][:, :, :], in0=gate[gi][:, :, :],
                                                                in1=gsum[:, :, :].to_broadcast([128, 8, 16]), op=ALU.mult),
                         reads=(f"gate{gi}", "gsum"), writes=(f"gate{gi}",))
                    P.op("dve", lambda e: e.tensor_copy(out=posf[:, :, :], in_=posu[:, :, :]), reads=allpos, writes=("posf",))
                    P.op("dve", lambda e: e.tensor_tensor(
                        out=eq[:, :, :], in0=posf[:, :, :].rearrange("p h k -> p (h k)").unsqueeze(2).to_broadcast([128, 128, 16]),
                        in1=thr16[:, :].unsqueeze(1).to_broadcast([128, 128, 16]), op=ALU.is_ge),
                        reads=("posf", "C:W"), writes=("eq",))
                    P.op("dve", lambda e: e.tensor_reduce(out=pa_f[:, :, :].rearrange("p h k -> p (h k)"), in_=eq[:, :, :], axis=AX.X, op=ALU.add),
                         reads=("eq",), writes=("pa_f",))
                    P.op("dve", lambda e: e.scalar_tensor_tensor(out=pb_f[:, :, :], in0=pa_f[:, :, :], scalar=-16.0, in1=posf[:, :, :],
                                                                 op0=ALU.mult, op1=ALU.add),
                         reads=("pa_f", "posf"), writes=("pb_f",))
                    idxv = idxf[:, :, :].rearrange("p (h t) k -> p h t k", t=2)
                    for (pf, half, dstk, dst) in ((pa_f, 0, "i1s", i1s), (pb_f, 1, "i2s", i2s)):
                        pk = "pa_f" if half == 0 else "pb_f"
                        P.op("dve", lambda e, pf=pf: e.tensor_tensor(
                            out=eq[:, :, :], in0=pf[:, :, :].rearrange("p h k -> p (h k)").unsqueeze(2).to_broadcast([128, 128, 16]),
                            in1=iota16[:, :].unsqueeze(1).to_broadcast([128, 128, 16]), op=ALU.is_equal),
                            reads=(pk, "C:W"), writes=("eq",))
                        P.op("dve", lambda e, half=half: e.tensor_tensor(
                            out=eq[:, :, :].rearrange("p (h k) a -> p h k a", h=8),
                            in0=eq[:, :, :].rearrange("p (h k) a -> p h k a", h=8),
                            in1=idxv[:, :, half, :].unsqueeze(2).to_broadcast([128, 8, 16, 16]), op=ALU.mult),
                            reads=("eq", "idxf"), writes=("eq",))
                        P.op("dve", lambda e, dst=dst: e.tensor_reduce(out=dst[:, :], in_=eq[:, :, :], axis=AX.X, op=ALU.add),
                             reads=("eq",), writes=(dstk,))
                    P.op("dve", lambda e: e.scalar_tensor_tensor(out=expf[:, :], in0=i1s[:, :], scalar=128.0, in1=i2s[:, :], op0=ALU.mult, op1=ALU.add),
                         reads=("i1s", "i2s"), writes=("expf",))
                    P.op("dve", lambda e, gi=gi: e.tensor_copy(out=expu[gi][:, :], in_=expf[:, :]), reads=("expf",), writes=(f"expu{gi}",))

                def body4b(tI):
                    ti = tI % 2
                    gi = tI % 2
                    t3 = tI % 3
                    t0 = 128 * tI
                    for k in range(nslot):
                        gcount[0] += 1
                        b = gcount[0] % NB
                        db = gcount[0] % NDG
                        P.dma("pool", lambda e, b=b, k=k, gi=gi: e.indirect_dma_start(
                            out=gbuf[b][:, :], out_offset=None, in_=uv16_d,
                            in_offset=bass.IndirectOffsetOnAxis(ap=expu[gi][:, k:k + 1], axis=0)),
                            dsem(f"gb{b}", "pool"), reads=(f"expu{gi}",), writes=(f"gbuf{b}",))
                        ji = nj()
                        P.op("dve", lambda e, b=b, k=k, t3=t3, ji=ji: e.scalar_tensor_tensor(
                            out=junkr[ji][:, :], in0=gbuf[b][:, 0:1024], scalar=1.0, in1=hn[t3][:, :], op0=ALU.mult, op1=ALU.mult,
                            accum_out=aval[:, k:k + 1]),
                            reads=(f"gbuf{b}", f"hn{t3}"), writes=(f"av{k}", f"junk{ji}"))
                        P.op("act", lambda e, k=k: e.activation(out=gval[:, k:k + 1], in_=aval[:, k:k + 1], func=AF.Gelu),
                             reads=(f"av{k}",), writes=(f"gv{k}",))
                        P.op("act", lambda e, k=k, gi=gi: e.mul(out=wval[:, k:k + 1], in_=gval[:, k:k + 1],
                                                               mul=gate[gi][:, :, :].rearrange("p h k -> p (h k)")[:, k:k + 1]),
                             reads=(f"gv{k}", f"gate{gi}"), writes=(f"wv{k}",))
                        P.op("act", lambda e, k=k, db=db: e.activation(out=dg[db][:, :], in_=ident_f[:, :], func=AF.Copy,
                                                                      scale=wval[:, k:k + 1]),
                             reads=(f"wv{k}", "C:W"), writes=(f"dg{db}",))

                        def mm_acc(e, k=k, b=b, db=db):
                            ins = None
                            for hh in range(2):
                                ins = e.matmul(ps_acc[hh][:, :], lhsT=dg[db][:, :], rhs=gbuf[b][:, 1024 + 512 * hh:1024 + 512 * (hh + 1)],
                                               start=(k == 0), stop=(k == nslot - 1))
                            return ins
                        P.op("pe", mm_acc, reads=(f"dg{db}", f"gbuf{b}"), writes=("p_acc",))
                    for hh in range(2):
                        P.op("dve", lambda e, t3=t3, hh=hh: e.tensor_tensor(out=x3[:, 512 * hh:512 * (hh + 1)], in0=ps_acc[hh][:, :],
                                                                           in1=x2t[t3][:, 512 * hh:512 * (hh + 1)], op=ALU.add),
                             reads=(f"x2t{t3}", "p_acc"), writes=(f"x3.{hh}",))
                    P.op("act", lambda e, ti=ti: e.activation(out=yo[ti][:, :], in_=x3[:, :], func=AF.Square, accum_out=st2[:, :]),
                         reads=("x3.0", "x3.1"), writes=("st2", f"yo{ti}"))
                    P.op("dve", lambda e: e.tensor_scalar(out=st2[:, :], in0=st2[:, :], scalar1=1.0 / D, scalar2=EPS, op0=ALU.mult, op1=ALU.add),
                         reads=("st2",), writes=("st2",))
                    P.op("act", lambda e: e.activation(out=st2[:, :], in_=st2[:, :], func=AF.Ln), reads=("st2",), writes=("st2",))
                    P.op("act", lambda e: e.activation(out=st2[:, :], in_=st2[:, :], func=AF.Exp, scale=-0.5), reads=("st2",), writes=("st2",))
                    P.op("dve", lambda e, ti=ti: e.scalar_tensor_tensor(out=yo[ti][:, :], in0=x3[:, :], scalar=st2[:, 0:1], in1=gfin[:, :],
                                                                       op0=ALU.mult, op1=ALU.mult),
                         reads=("x3.0", "x3.1", "st2", "C:W"), writes=(f"yo{ti}",))
                    P.dma("sp", lambda e, ti=ti, t0=t0: e.dma_start(out=y_h[t0:t0 + 128, :], in_=yo[ti][:, :]),
                          dsem(f"yo{ti}"), reads=(f"yo{ti}",), writes=())

                recA = [P.split(P.capture(body4a, t_)) for t_ in range(ntile)]
                recB = [P.capture(body4b, t_) for t_ in range(ntile)]
                P.pipeline([recA[t_] + [recB[t_]] for t_ in range(ntile)], spans=[0.9, 0.9, 1.0])
                P.drain_dma("sp")
                P.drain_dma("pool")
                P.emit(nc, "p3")
    return nc


def _seg_tokens():
    out = np.zeros((NSEG, 384), np.int64)
    kk = np.arange(384)
    for s in range(NSEG):
        g, sp_ = s // 16, s % 16
        d = DIL[g]
        nsub = 16 // d
        r, sub = sp_ // nsub, sp_ % nsub
        m = 256 * sub - 64 + kk
        out[s] = d * m + r
    return out


def prepare_inputs(inp):
    f = lambda a: np.ascontiguousarray(np.asarray(a, dtype=np.float32))
    x = f(inp["x"])
    lay = lambda w, nch: np.ascontiguousarray(w.reshape(nch, 128, -1).transpose(1, 0, 2))
    shared = {
        "w_in_l": lay(f(inp["w_in"])[0], 8),
        "gmix": np.ascontiguousarray(f(inp["norm_mix_g"])[0].reshape(8, 128).T),
        "gsgu_bc": np.ascontiguousarray(np.broadcast_to(f(inp["sgu_norm_g"])[0][None, :], (128, 1024))),
        "wsT": np.ascontiguousarray(f(inp["sgu_w"])[0].transpose(2, 0, 1)),
        "bsgu_bc": np.ascontiguousarray(np.broadcast_to(f(inp["sgu_b"])[0].reshape(1, 1024), (128, 1024))),
        "wa_l": lay(f(inp["w_branch_a"])[0], 8),
        "wb_l": lay(f(inp["w_branch_b"])[0], 4),
        "wo_l": lay(f(inp["w_out"])[0], 8),
        "gffn_bc": np.ascontiguousarray(np.broadcast_to(f(inp["norm_ffn_g"])[0][None, :], (128, 1024))),
        "gfin_bc": np.ascontiguousarray(np.broadcast_to(f(inp["norm_final_g"])[None, :], (128, 1024))),
        "wq_l": lay(f(inp["peer_wq"])[0], 8),
        "skT": np.ascontiguousarray(f(inp["peer_subkeys"])[0].reshape(16, 128, 128).transpose(2, 0, 1)),
        "uv_tab": np.ascontiguousarray(np.concatenate([f(inp["peer_u"])[0], f(inp["peer_v"])[0]], axis=1)),
        "ident": np.eye(128, dtype=np.float32),
        "iota16": np.ascontiguousarray(np.broadcast_to(np.arange(16, dtype=np.float32)[None, :], (128, 16))),
        "thr16": np.ascontiguousarray(np.broadcast_to((16.0 * np.arange(1, 17, dtype=np.float32))[None, :], (128, 16))),
    }
    jl = np.arange(128)[:, None]
    il = np.arange(128)[None, :]
    relA = jl - il - 64
    relB = jl - il + 64
    A = np.where(np.abs(relA) <= 64, -np.abs(relA), -1e9).astype(np.float32)
    Bm = np.where(np.abs(relB) <= 64, -np.abs(relB), -1e9).astype(np.float32)
    shared["negabs"] = np.ascontiguousarray(np.concatenate([A, Bm, A, Bm], axis=1))
    segtok = _seg_tokens()
    maps = []
    for c in range(8):
        b, hf = c // 2, c % 2
        T0 = hf * NTOK
        gtok = segtok + T0
        valid = (gtok >= 0) & (gtok < SEQ)
        xg = x[b][np.clip(gtok, 0, SEQ - 1)]
        xg = xg * valid[..., None].astype(np.float32) if False else np.where(valid[..., None], xg, np.float32(0))
        xs = np.ascontiguousarray(xg.reshape(NSEG, 384, 8, 128).transpose(0, 3, 2, 1))
        kv = valid.reshape(NSEG, 3, 128).transpose(2, 0, 1).reshape(128, NSEG * 3).astype(np.float32)
        m = dict(shared)
        m["xseg"] = xs
        m["kval"] = np.ascontiguousarray(kv)
        m["xtok"] = np.ascontiguousarray(x[b, T0:T0 + NTOK])
        maps.append(m)
    return maps


def kernel(**inputs):
    maps = prepare_inputs(inputs)
    nc = build_program()
    res = run_bass_kernel_spmd(nc, maps, core_ids=list(range(8)))
    out = np.zeros((4, SEQ, D), np.float32)
    for c in range(8):
        b, hf = c // 2, c % 2
        out[b, hf * NTOK:(hf + 1) * NTOK] = res.results[c]["y"]
    return out
```

```python
from contextlib import ExitStack
import numpy as np
import concourse.bass as bass
import concourse.mybir as mybir
from concourse.bass_utils import run_bass_kernel_spmd

F32 = mybir.dt.float32
BF16 = mybir.dt.bfloat16
U32 = mybir.dt.uint32
AF = mybir.ActivationFunctionType
ALU = mybir.AluOpType
AX = mybir.AxisListType

D = 1024
SEQ = 8192
NTOK = 4096
NSEG = 48
EPS = 1e-6
DIL = (1, 4, 16)
ENG = ("pe", "act", "dve", "pool", "sp")


class Prog:
    def __init__(self, semobj):
        self.semobj = semobj
        self.cnt = {e: 0 for e in ENG}
        self.dcnt = {}
        self.q = {e: [] for e in ENG}
        self.lastw = {}
        self.readers = {}
        self.known = {e: {} for e in ENG}
        self._rec = None

    def capture(self, fn, *args):
        saved = self._rec
        self._rec = []
        fn(*args)
        rec, self._rec = self._rec, saved
        return rec

    def replay(self, rec):
        for kind, a in rec:
            if kind == "op":
                self.op(*a)
            else:
                self.dma(*a)

    def mark(self):
        if self._rec is not None:
            self._rec.append(("mark", None))

    @staticmethod
    def split(rec):
        out = [[]]
        for it in rec:
            if it[0] == "mark":
                out.append([])
            else:
                out[-1].append(it)
        return out

    @staticmethod
    def interleave_n(lists, spans=None):
        items = []
        for li, L in enumerate(lists):
            n = len(L)
            f = 1.0 if spans is None else spans[li]
            for i, it in enumerate(L):
                items.append(((i + 0.5) / n * f, li, i, it))
        items.sort(key=lambda t: (t[0], t[1], t[2]))
        return [t[3] for t in items]

    def pipeline(self, stages_by_item, spans=None):
        n = len(stages_by_item)
        S = max(len(x) for x in stages_by_item)
        for step in range(n + S - 1):
            sts = [st for st in range(S - 1, -1, -1) if 0 <= step - st < n and st < len(stages_by_item[step - st])]
            lists = [stages_by_item[step - st][st] for st in sts]
            self.replay(self.interleave_n(lists, None if spans is None else [spans[st] for st in sts]))

    @staticmethod
    def interleave(A, B):
        out = []
        na, nb = len(A), len(B)
        ia = ib = 0
        while ia < na or ib < nb:
            if ib >= nb or (ia < na and ia * nb <= ib * na):
                out.append(A[ia])
                ia += 1
            else:
                out.append(B[ib])
                ib += 1
        return out

    def _deps(self, eng, reads, writes):
        need = {}

        def add(tok):
            if tok is None:
                return
            s, v = tok
            if need.get(s, 0) < v:
                need[s] = v

        def addw(k):
            for s, v in self.lastw.get(k, {}).items():
                add((s, v))
        for k in reads:
            addw(k)
        for k in writes:
            addw(k)
            for t in self.readers.get(k, ()):
                add(t)
        out = []
        kn = self.known[eng]
        for s, v in need.items():
            if kn.get(s, 0) < v:
                kn[s] = v
                out.append((s, v))
        return out

    def _commit(self, tok, reads, writes):
        for k in reads:
            if not k.startswith("C:"):
                self.readers.setdefault(k, []).append(tok)
        for k in writes:
            if k.startswith("C:"):
                self.lastw.setdefault(k, {})[tok[0]] = tok[1]
            else:
                self.lastw[k] = {tok[0]: tok[1]}
            self.readers[k] = []

    def op(self, eng, fn, reads=(), writes=()):
        if self._rec is not None:
            self._rec.append(("op", (eng, fn, reads, writes)))
            return None
        waits = self._deps(eng, reads, writes)
        self.cnt[eng] += 1
        tok = (eng, self.cnt[eng])
        self.q[eng].append((waits, fn, (eng, 1)))
        self._commit(tok, reads, writes)
        return tok

    def dma(self, eng, fn, semkey, reads=(), writes=()):
        if self._rec is not None:
            self._rec.append(("dma", (eng, fn, semkey, reads, writes)))
            return None
        waits = self._deps(eng, reads, writes)
        self.dcnt[semkey] = self.dcnt.get(semkey, 0) + 16
        tok = (semkey, self.dcnt[semkey])
        self.q[eng].append((waits, fn, (semkey, 16)))
        self._commit(tok, reads, writes)
        return tok

    def drain_dma(self, eng="sp"):
        waits = []
        for s, v in self.dcnt.items():
            if self.known[eng].get(s, 0) < v:
                self.known[eng][s] = v
                waits.append((s, v))
        if waits:
            self.q[eng].append((waits, None, None))

    def emit(self, nc, name):
        q = self.q
        semobj = self.semobj

        def run(eng_name):
            def f(e):
                for waits, fn, inc in q[eng_name]:
                    for s, v in waits:
                        e.wait_ge(semobj[s], v)
                    if fn is not None:
                        ins = fn(e)
                        ins.then_inc(semobj[inc[0]], inc[1])
            return f
        with nc.Block() as block:
            block.tensor(run("pe"))
            block.scalar(run("act"))
            block.vector(run("dve"))
            block.gpsimd(run("pool"))
            block.sync(run("sp"))
        self.q = {e: [] for e in ENG}
        self.lastw = {}
        self.readers = {}


def _slopes():
    return [2.0 ** (-(h + 1)) for h in range(8)]


def build_program(cfg=None):
    cfg = cfg or {}
    segs = cfg.get("segs", list(range(NSEG)))
    nseg = len(segs)
    nblk = cfg.get("nblk", 16)
    ntile = cfg.get("ntile", 32)
    nslot = cfg.get("nslot", 128)
    debug = cfg.get("debug", False)
    pool_rms = cfg.get("pool_rms", 4)
    scratch_kind = "ExternalOutput" if debug else "Internal"

    nc = bass.Bass("TRN2", target_bir_lowering=False)
    dt = nc.dram_tensor
    xseg = dt("xseg", [NSEG, 128, 8, 384], F32, kind="ExternalInput").ap()
    kval_h = dt("kval", [128, NSEG * 3], F32, kind="ExternalInput").ap()
    xtok_h = dt("xtok", [NTOK, D], F32, kind="ExternalInput").ap()
    w_in_h = dt("w_in_l", [128, 8, 6656], F32, kind="ExternalInput").ap()
    gmix_h = dt("gmix", [128, 8], F32, kind="ExternalInput").ap()
    gsgu_h = dt("gsgu_bc", [128, 1024], F32, kind="ExternalInput").ap()
    wsT_h = dt("wsT", [128, 8, 128], F32, kind="ExternalInput").ap()
    bsbc_h = dt("bsgu_bc", [128, 1024], F32, kind="ExternalInput").ap()
    wa_h = dt("wa_l", [128, 8, 1024], F32, kind="ExternalInput").ap()
    wb_h = dt("wb_l", [128, 4, 1024], F32, kind="ExternalInput").ap()
    wo_h = dt("wo_l", [128, 8, 1024], F32, kind="ExternalInput").ap()
    gffn_h = dt("gffn_bc", [128, 1024], F32, kind="ExternalInput").ap()
    gfin_h = dt("gfin_bc", [128, 1024], F32, kind="ExternalInput").ap()
    wq_h = dt("wq_l", [128, 8, 2048], F32, kind="ExternalInput").ap()
    skT_h = dt("skT", [128, 16, 128], F32, kind="ExternalInput").ap()
    uv_h = dt("uv_tab", [16384, 2048], F32, kind="ExternalInput").ap()
    negabs_h = dt("negabs", [128, 512], F32, kind="ExternalInput").ap()
    ident_h = dt("ident", [128, 128], F32, kind="ExternalInput").ap()
    iota_h = dt("iota16", [128, 16], F32, kind="ExternalInput").ap()
    thr_h = dt("thr16", [128, 16], F32, kind="ExternalInput").ap()
    y_h = dt("y", [NTOK, D], F32, kind="ExternalOutput").ap()
    att_d = dt("att_d", [3, NTOK, 520], F32, kind=scratch_kind).ap()
    at_d = dt("at_d", [128, 8, NTOK], F32, kind=scratch_kind).ap()
    x2_d = dt("x2_d", [NTOK, D], F32, kind=scratch_kind).ap()
    uv16_d = dt("uv16_d", [16384, 2048], BF16, kind="Internal").ap()
    rstd_d = dt("rstd_d", [128, NTOK], F32, kind="Internal").ap()

    slopes = _slopes()

    with ExitStack() as top:
        semobj = {}
        for e in ENG:
            semobj[e] = top.enter_context(nc.semaphore("s_" + e))

        def dsem(name, eng="sp"):
            name = name + "@" + eng
            if name not in semobj:
                semobj[name] = top.enter_context(nc.semaphore("d_" + name))
            return name
        P = Prog(semobj)

        def load_cast(dst, src, eng="pool", sem="w", piece=1024):
            n = src.shape[-1]
            pre = (slice(None),) * (len(src.shape) - 1)
            for a in range(0, n, piece):
                b = min(n, a + piece)
                P.dma(eng, (lambda e, d_=dst[pre + (slice(a, b),)], s_=src[pre + (slice(a, b),)]: e.dma_start(out=d_, in_=s_)),
                      dsem(sem, eng), writes=("C:W",))

        def rmsnorm_block(es_bufs, src_ap, bi, n, tagp, rs_src=None, rs_loaded=False):
            xst, sq, rstd, xn, ps_ss, gmix, ones_bf, rtmp = es_bufs[:8]
            mhalf = None
            kx = f"{tagp}xst{bi}"
            if src_ap is not None:
                P.dma("sp", lambda e: e.dma_start(out=xst[bi][:, :, 0:n], in_=src_ap), dsem(kx), writes=(kx,))
                if rs_src is not None:
                    P.dma("sp", lambda e: e.dma_start(out=rstd[bi][:, 0:n], in_=rs_src), dsem(f"{tagp}rsl{bi}"),
                          writes=(f"{tagp}rs{bi}",))
                return None
            if rs_loaded:
                return scale_chunks(es_bufs, bi, n, tagp)
            P.op("act", lambda e: e.activation(out=sq[bi][:, :, 0:n], in_=xst[bi][:, :, 0:n], func=AF.Square),
                 reads=(kx,), writes=(f"{tagp}sq{bi}",))

            def mm_ss(e):
                ins = None
                for c in range(8):
                    ins = e.matmul(ps_ss[:, 0:n], lhsT=ones_bf[:, :], rhs=sq[bi][:, c, 0:n], start=(c == 0), stop=(c == 7))
                return ins
            P.op("pe", mm_ss, reads=(f"{tagp}sq{bi}", "C:W", "C:ones"), writes=(f"{tagp}ps_ss",))
            P.op("dve", lambda e: e.tensor_scalar(out=rstd[bi][:, 0:n], in0=ps_ss[:, 0:n], scalar1=1.0 / D, scalar2=EPS,
                                                  op0=ALU.mult, op1=ALU.add),
                 reads=(f"{tagp}ps_ss",), writes=(f"{tagp}rs{bi}",))
            if mhalf is not None:
                P.op("pool", lambda e: e.tensor_tensor(out=rstd[bi][:, 0:n], in0=rstd[bi][:, 0:n], in1=mhalf[:, 0:n], op=ALU.pow),
                     reads=(f"{tagp}rs{bi}", "C:mhalf"), writes=(f"{tagp}rs{bi}",))
            else:
                P.op("act", lambda e: e.activation(out=rstd[bi][:, 0:n], in_=rstd[bi][:, 0:n], func=AF.Ln),
                     reads=(f"{tagp}rs{bi}",), writes=(f"{tagp}rs{bi}",))
                P.op("act", lambda e: e.activation(out=rstd[bi][:, 0:n], in_=rstd[bi][:, 0:n], func=AF.Exp, scale=-0.5),
                     reads=(f"{tagp}rs{bi}",), writes=(f"{tagp}rs{bi}",))
            return scale_chunks(es_bufs, bi, n, tagp)

        def scale_chunks(es_bufs, bi, n, tagp):
            xst, sq, rstd, xn, ps_ss, gmix, ones_bf, rtmp = es_bufs[:8]
            kx = f"{tagp}xst{bi}"
            for c in range(8):
                if c < pool_rms:
                    tb = c % 2
                    P.op("pool", lambda e, c=c, tb=tb: e.tensor_tensor(
                        out=rtmp[tb][:, 0:n], in0=xst[bi][:, c, 0:n], in1=rstd[bi][:, 0:n], op=ALU.mult),
                        reads=(kx, f"{tagp}rs{bi}"), writes=(f"{tagp}rtmp{tb}",))
                    P.op("pool", lambda e, c=c, tb=tb: e.tensor_scalar(
                        out=xn[bi][:, c, 0:n], in0=rtmp[tb][:, 0:n], scalar1=gmix[:, c:c + 1], scalar2=1.0,
                        op0=ALU.mult, op1=ALU.mult),
                        reads=(f"{tagp}rtmp{tb}", "C:W"), writes=(f"{tagp}xn{bi}.{c}",))
                    continue
                P.op("dve", lambda e, c=c: e.scalar_tensor_tensor(
                    out=xn[bi][:, c, 0:n], in0=xst[bi][:, c, 0:n], scalar=gmix[:, c:c + 1], in1=rstd[bi][:, 0:n],
                    op0=ALU.mult, op1=ALU.mult),
                    reads=(kx, f"{tagp}rs{bi}", "C:W"), writes=(f"{tagp}xn{bi}.{c}",))
            return [f"{tagp}xn{bi}.{c}" for c in range(8)]

        if nseg > 0:
            with ExitStack() as es:
                sb = lambda name, shape, dtp: es.enter_context(nc.sbuf_tensor("sb_" + name, shape, dtp))
                ps = lambda name, shape, dtp=F32: es.enter_context(nc.psum_tensor("pp_" + name, shape, dtp))
                wqkv = sb("wqkv", [128, 8, 2560], BF16)
                gmix = sb("gmix1", [128, 8], F32)
                negabs = sb("negabs", [128, 512], F32)
                ones_bf = sb("ones1", [128, 128], BF16)
                ones8 = sb("ones8", [128, 8, 1], F32)
                kval = sb("kval", [128, NSEG * 3], F32)
                xst = [sb(f"xst{i}", [128, 8, 384], F32) for i in range(2)]
                sq = [sb(f"sq{i}", [128, 8, 384], BF16) for i in range(2)]
                rstd = [sb(f"rstd{i}", [128, 384], F32) for i in range(2)]
                xn = [sb(f"xn{i}", [128, 8, 384], BF16) for i in range(2)]
                kT = [sb(f"kT{i}", [128, 4, 384], BF16) for i in range(2)]
                qT = [sb(f"qT{i}", [128, 4, 256], BF16) for i in range(2)]
                vaug = [sb(f"vaug{i}", [128, 3, 8, 65], BF16) for i in range(2)]
                tt = [sb(f"tt{i}", [128, 512], F32) for i in range(2)]
                pT = [sb(f"pT{i}", [128, 512], BF16) for i in range(2)]
                osb = [sb(f"osb{i}", [128, 2, 520], F32) for i in range(2)]
                cst = [sb(f"cst{i}", [128, 2, 2048], BF16) for i in range(2)]
                castn = [0 if cfg.get("cast", True) else 64]
                ps_ss1 = ps("ps_ss1", [128, 512])
                ps_pr = [ps(f"ps_pr{i}", [128, 512]) for i in range(2)]
                ps_S = [ps(f"ps_S{i}", [128, 512]) for i in range(2)]
                ps_O = [ps(f"ps_O{i}", [128, 512]) for i in range(3)]
                ocnt = [0]

                load_cast(wqkv[:, :, :], w_in_h[:, :, 2048:4608], piece=640)
                P.dma("sp", lambda e: e.dma_start(out=gmix[:, :], in_=gmix_h), dsem("w"), writes=("C:W",))
                P.dma("sp", lambda e: e.dma_start(out=negabs[:, :], in_=negabs_h), dsem("w"), writes=("C:W",))
                P.dma("sp", lambda e: e.dma_start(out=kval[:, :], in_=kval_h), dsem("w"), writes=("C:W",))
                P.op("dve", lambda e: e.memset(ones_bf[:, :], 1.0), writes=("C:ones",))
                P.op("dve", lambda e: e.memset(ones8[:, :, :], 1.0), writes=("C:ones8",))

                prn = [0]

                def next_pr():
                    prn[0] += 1
                    return prn[0] % 2

                rtmp = [sb(f"rtmp{i}", [128, 384], F32) for i in range(2)]
                bufs1 = (xst, sq, rstd, xn, ps_ss1, gmix, ones_bf, rtmp)
                evn = [0]

                def evac_copy(out_ap, in_ap, reads, writes):
                    evn[0] += 1
                    if evn[0] % 2:
                        P.op("act", lambda e: e.copy(out=out_ap, in_=in_ap), reads=reads, writes=writes)
                    else:
                        P.op("dve", lambda e: e.tensor_copy(out=out_ap, in_=in_ap), reads=reads, writes=writes)

                def seg_geo(si, s):
                    g = s // 16
                    d = DIL[g]
                    sp_ = s % 16
                    nsub = 16 // d
                    return g, d, sp_ // nsub, sp_ % nsub, si % 2

                def cast_chunk(ci):
                    cb = ci % 2
                    srcv = uv_h[256 * ci:256 * (ci + 1), :].rearrange("(p r) c -> p r c", r=2)
                    dstv = uv16_d[256 * ci:256 * (ci + 1), :].rearrange("(p r) c -> p r c", r=2)
                    P.dma("pool", lambda e: e.dma_start(out=cst[cb][:, :, :], in_=srcv), dsem(f"cst{cb}", "pool"), writes=(f"cst{cb}",))
                    P.dma("sp", lambda e: e.dma_start(out=dstv, in_=cst[cb][:, :, :]), dsem(f"cso{cb}"), reads=(f"cst{cb}",))

                def body1a(si, s):
                    g, d, r, sub, bi = seg_geo(si, s)
                    if si == 0:
                        rmsnorm_block(bufs1, xseg[s], bi, 384, "a")
                    if si + 1 < nseg:
                        rmsnorm_block(bufs1, xseg[segs[si + 1]], (si + 1) % 2, 384, "a")
                    for _ in range(2 if si % 3 == 0 else 1):
                        if castn[0] < 64:
                            cast_chunk(castn[0])
                            castn[0] += 1
                    xk = rmsnorm_block(bufs1, None, bi, 384, "a")
                    P.mark()
                    if g == 0:
                        P.dma("sp", lambda e: e.dma_start(out=rstd_d[:, 256 * s:256 * (s + 1)], in_=rstd[bi][:, 64:320]),
                              dsem(f"rsd{bi}"), reads=(f"ars{bi}",))

                    for j in range(4):
                        pj = next_pr()

                        def mm_k(e, j=j, pj=pj):
                            ins = None
                            for c in range(8):
                                ins = e.matmul(ps_pr[pj][:, 0:384], lhsT=wqkv[:, c, 1536 + 128 * j:1536 + 128 * (j + 1)],
                                               rhs=xn[bi][:, c, :], start=(c == 0), stop=(c == 7))
                            return ins
                        P.op("pe", mm_k, reads=tuple(xk) + ("C:W",), writes=(f"ps_pr{pj}",))
                        evac_copy(kT[bi][:, j, :], ps_pr[pj][:, 0:384], (f"ps_pr{pj}",), (f"kT{bi}.{j}",))
                    for j in range(4):
                        pj = next_pr()

                        def mm_q(e, j=j, pj=pj):
                            ins = None
                            for c in range(8):
                                ins = e.matmul(ps_pr[pj][:, 0:256], lhsT=wqkv[:, c, g * 512 + 128 * j:g * 512 + 128 * (j + 1)],
                                               rhs=xn[bi][:, c, 64:320], start=(c == 0), stop=(c == 7))
                            return ins
                        P.op("pe", mm_q, reads=tuple(xk) + ("C:W",), writes=(f"ps_pr{pj}",))
                        evac_copy(qT[bi][:, j, :], ps_pr[pj][:, 0:256], (f"ps_pr{pj}",), (f"qT{bi}.{j}",))
                    for j in range(3):
                        pj = next_pr()

                        def mm_v(e, j=j, pj=pj):
                            ins = None
                            for c in range(8):
                                ins = e.matmul(ps_pr[pj][:, :], lhsT=xn[bi][:, c, 128 * j:128 * (j + 1)],
                                               rhs=wqkv[:, c, 2048:2560], start=(c == 0), stop=(c == 7))
                            return ins
                        P.op("pe", mm_v, reads=tuple(xk) + ("C:W",), writes=(f"ps_pr{pj}",))
                        col = 3 * s + j
                        P.op("act", lambda e, j=j, pj=pj, col=col: e.activation(
                            out=vaug[bi][:, j, :, 0:64], in_=ps_pr[pj][:, :].rearrange("p (h e) -> p h e", h=8),
                            func=AF.Copy, scale=kval[:, col:col + 1]),
                            reads=(f"ps_pr{pj}", "C:W"), writes=(f"va{bi}.{j}",))
                        P.op("dve", lambda e, j=j, col=col: e.tensor_scalar(
                            out=vaug[bi][:, j, :, 64:65], in0=ones8[:, :, :],
                            scalar1=kval[:, col:col + 1], scalar2=None, op0=ALU.mult),
                            reads=("C:W", "C:ones8"), writes=(f"vb{bi}.{j}",))

                def body1b(si, s):
                    g, d, r, sub, bi = seg_geo(si, s)
                    obm = {}

                    def front(h):
                        jc, pb = h // 2, 64 * (h % 2)
                        ri = h % 2
                        coef = slopes[h] * d * 8.0

                        def mm_s(e, jc=jc, pb=pb, ri=ri):
                            kk = kT[bi][pb:pb + 64, jc, :]
                            qq = qT[bi][pb:pb + 64, jc, :]
                            e.matmul(ps_S[ri][:, 0:128], lhsT=kk[:, 0:128], rhs=qq[:, 0:128], start=True, stop=True)
                            e.matmul(ps_S[ri][:, 128:384], lhsT=kk[:, 128:256], rhs=qq[:, 0:256], start=True, stop=True)
                            return e.matmul(ps_S[ri][:, 384:512], lhsT=kk[:, 256:384], rhs=qq[:, 128:256], start=True, stop=True)
                        P.op("pe", mm_s, reads=(f"kT{bi}.{jc}", f"qT{bi}.{jc}"), writes=(f"ps_S{ri}",))
                        P.op("dve", lambda e, ri=ri, coef=coef: e.scalar_tensor_tensor(
                            out=tt[ri][:, :], in0=negabs[:, :], scalar=coef, in1=ps_S[ri][:, :], op0=ALU.mult, op1=ALU.add),
                            reads=(f"ps_S{ri}", "C:W"), writes=(f"tt{ri}",))
                        P.op("act", lambda e, ri=ri: e.activation(out=pT[ri][:, :], in_=tt[ri][:, :], func=AF.Exp, scale=0.125),
                             reads=(f"tt{ri}",), writes=(f"pT{ri}",))

                    def back(h):
                        ri = h % 2
                        hg = h // 4
                        if h % 4 == 0:
                            for qt in range(2):
                                ocnt[0] += 1
                                obm[qt] = ocnt[0] % 3
                        for qt in range(2):
                            ob = obm[qt]
                            c0 = (h % 4) * 65

                            def mm_o(e, qt=qt, ob=ob, c0=c0, h=h, ri=ri):
                                e.matmul(ps_O[ob][:, c0:c0 + 65], lhsT=pT[ri][:, 256 * qt:256 * qt + 128],
                                         rhs=vaug[bi][:, qt, h, :], start=True, stop=False)
                                return e.matmul(ps_O[ob][:, c0:c0 + 65], lhsT=pT[ri][:, 256 * qt + 128:256 * qt + 256],
                                                rhs=vaug[bi][:, qt + 1, h, :], start=False, stop=True)
                            P.op("pe", mm_o, reads=(f"pT{ri}", f"va{bi}.{qt}", f"vb{bi}.{qt}", f"va{bi}.{qt + 1}", f"vb{bi}.{qt + 1}"),
                                 writes=(f"ps_O{ob}",))
                        if h % 4 == 3:
                            for qt in range(2):
                                ob = obm[qt]
                                evac_copy(osb[bi][:, qt, 260 * hg:260 * (hg + 1)], ps_O[ob][:, 0:260],
                                          (f"ps_O{ob}",), (f"osb{bi}.{qt}.{hg}",))

                    front(0)
                    for h in range(8):
                        if h + 1 < 8:
                            front(h + 1)
                        back(h)
                    P.mark()
                    for qt in range(2):
                        m0 = 256 * sub + 128 * qt
                        lo = d * m0 + r
                        dst = att_d[g][lo:lo + d * 127 + 1:d, :]
                        P.dma("sp", lambda e, dst=dst, qt=qt: e.dma_start(out=dst, in_=osb[bi][:, qt, :]),
                              dsem(f"osb{bi}.{qt}"), reads=(f"osb{bi}.{qt}.0", f"osb{bi}.{qt}.1"), writes=())
                recA = [P.split(P.capture(body1a, si_, s_)) for si_, s_ in enumerate(segs)]
                extras = []
                while castn[0] < 64:
                    extras.append(P.capture(cast_chunk, castn[0]))
                    castn[0] += 1
                recB = [P.split(P.capture(body1b, si_, s_)) for si_, s_ in enumerate(segs)]
                P.pipeline([recA[i] + recB[i] for i in range(nseg)])
                for extra in extras:
                    P.replay(extra)
                P.drain_dma("sp")
                P.drain_dma("pool")
                P.emit(nc, "p1")

        if nblk > 0:
            with ExitStack() as es:
                sb = lambda name, shape, dtp: es.enter_context(nc.sbuf_tensor("sb_" + name, shape, dtp))
                ps = lambda name, shape, dtp=F32: es.enter_context(nc.psum_tensor("pp_" + name, shape, dtp))
                w_uv = sb("w_uv", [128, 8, 2048], BF16)
                w_a = sb("w_a", [128, 8, 1024], BF16)
                wsT = sb("wsT", [128, 8, 128], BF16)
                bsbc = sb("bsbc", [128, 1024], F32)
                mxb = [sb(f"mxb{i}", [128, 512], F32) for i in range(2)]
                gsgu = sb("gsgu", [128, 1024], F32)
                gmix = sb("gmix2", [128, 8], F32)
                ones_bf = sb("ones2", [128, 128], BF16)
                xst = [sb(f"bxst{i}", [128, 8, 256], F32) for i in range(2)]
                sq = [sb(f"bsq{i}", [128, 8, 256], BF16) for i in range(2)]
                rstd = [sb(f"brstd{i}", [128, 256], F32) for i in range(2)]
                xn = [sb(f"bxn{i}", [128, 8, 256], BF16) for i in range(2)]
                uT = [sb(f"uT{i}", [128, 8, 256], BF16) for i in range(2)]
                vg = [sb(f"vg{i}", [128, 1024], F32) for i in range(4)]
                junkr = [sb(f"junk{i}", [128, 1024], BF16) for i in range(4)]
                jn = [0]

                def nj():
                    jn[0] += 1
                    return jn[0] % 4
                ssv4 = sb("ssv4", [128, 4], F32)
                vn = [sb(f"vn{i}", [128, 1024], BF16) for i in range(4)]
                yaT = [sb(f"yaT{i}", [128, 8, 256], BF16) for i in range(2)]
                atsb = [sb(f"atsb{i}", [128, 8, 256], F32) for i in range(2)]
                ps_ss = ps("b_ss", [128, 512])
                ps_pr = [ps(f"b_pr{i}", [128, 512]) for i in range(2)]
                ps_mx = [ps(f"b_mx{i}", [128, 512]) for i in range(2)]
                ps_at = [ps(f"b_at{i}", [128, 512]) for i in range(2)]

                load_cast(w_uv[:, :, :], w_in_h[:, :, 0:2048], piece=1024)
                load_cast(w_a[:, :, :], wa_h[:, :, :], piece=1024)
                load_cast(wsT[:, :, :], wsT_h[:, :, :], piece=128)
                for dst_, src_ in ((bsbc[:, :], bsbc_h), (gsgu[:, :], gsgu_h), (gmix[:, :], gmix_h)):
                    P.dma("sp", lambda e, d_=dst_, s_=src_: e.dma_start(out=d_, in_=s_), dsem("w"), writes=("C:W",))
                P.op("dve", lambda e: e.memset(ones_bf[:, :], 1.0), writes=("C:ones",))
                prn = [0]
                rtmp = [sb(f"brtmp{i}", [128, 256], F32) for i in range(2)]
                bufs2 = (xst, sq, rstd, xn, ps_ss, gmix, ones_bf, rtmp)

                def body2(nb):
                    bi = nb % 2
                    if nb == 0:
                        rmsnorm_block(bufs2, xseg[nb][:, :, 64:320], bi, 256, "b", rs_src=rstd_d[:, 256 * nb:256 * (nb + 1)])
                    if nb + 1 < nblk:
                        rmsnorm_block(bufs2, xseg[nb + 1][:, :, 64:320], (nb + 1) % 2, 256, "b",
                                      rs_src=rstd_d[:, 256 * (nb + 1):256 * (nb + 2)])
                    xk = rmsnorm_block(bufs2, None, bi, 256, "b", rs_loaded=True)
                    P.mark()
                    for j in range(8):
                        prn[0] += 1
                        pj = prn[0] % 2

                        def mm_u(e, j=j, pj=pj):
                            ins = None
                            for c in range(8):
                                ins = e.matmul(ps_pr[pj][:, 0:256], lhsT=w_uv[:, c, 128 * j:128 * (j + 1)],
                                               rhs=xn[bi][:, c, :], start=(c == 0), stop=(c == 7))
                            return ins
                        P.op("pe", mm_u, reads=tuple(xk) + ("C:W",), writes=(f"b_pr{pj}",))
                        P.op("act", lambda e, j=j, pj=pj: e.activation(out=uT[bi][:, j, :], in_=ps_pr[pj][:, 0:256], func=AF.Gelu),
                             reads=(f"b_pr{pj}",), writes=(f"uT{bi}.{j}",))
                    for t2 in range(2):
                        vi = (nb * 2 + t2) % 4
                        for hh in range(2):
                            prn[0] += 1
                            pj = prn[0] % 2

                            def mm_v(e, t2=t2, hh=hh, pj=pj):
                                ins = None
                                for c in range(8):
                                    ins = e.matmul(ps_pr[pj][:, :], lhsT=xn[bi][:, c, 128 * t2:128 * (t2 + 1)],
                                                   rhs=w_uv[:, c, 1024 + 512 * hh:1024 + 512 * (hh + 1)],
                                                   start=(c == 0), stop=(c == 7))
                                return ins
                            P.op("pe", mm_v, reads=tuple(xk) + ("C:W",), writes=(f"b_pr{pj}",))
                            P.op("act", lambda e, hh=hh, pj=pj, vi=vi: e.activation(
                                out=vg[vi][:, 512 * hh:512 * (hh + 1)], in_=ps_pr[pj][:, :], func=AF.Gelu),
                                reads=(f"b_pr{pj}",), writes=(f"vg{vi}.{hh}",))
                        ji = nj()
                        P.op("dve", lambda e, vi=vi, ji=ji: e.scalar_tensor_tensor(
                            out=junkr[ji][:, :], in0=vg[vi][:, :], scalar=1.0, in1=vg[vi][:, :], op0=ALU.mult, op1=ALU.mult,
                            accum_out=ssv4[:, vi:vi + 1]),
                            reads=(f"vg{vi}.0", f"vg{vi}.1"), writes=(f"ssv{vi}", f"junk{ji}"))
                    v0 = (nb * 2) % 4
                    sk = (f"ssv{v0}", f"ssv{v0 + 1}")
                    P.op("dve", lambda e: e.tensor_scalar(out=ssv4[:, v0:v0 + 2], in0=ssv4[:, v0:v0 + 2], scalar1=1.0 / D, scalar2=EPS,
                                                          op0=ALU.mult, op1=ALU.add), reads=sk, writes=sk)
                    P.op("act", lambda e: e.activation(out=ssv4[:, v0:v0 + 2], in_=ssv4[:, v0:v0 + 2], func=AF.Ln), reads=sk, writes=sk)
                    P.op("act", lambda e: e.activation(out=ssv4[:, v0:v0 + 2], in_=ssv4[:, v0:v0 + 2], func=AF.Exp, scale=-0.5),
                         reads=sk, writes=sk)
                    for t2 in range(2):
                        vi = (nb * 2 + t2) % 4
                        P.op("dve", lambda e, vi=vi: e.scalar_tensor_tensor(
                            out=vn[vi][:, :], in0=vg[vi][:, :], scalar=ssv4[:, vi:vi + 1], in1=gsgu[:, :], op0=ALU.mult, op1=ALU.mult),
                            reads=(f"vg{vi}.0", f"vg{vi}.1", f"ssv{vi}", "C:W"), writes=(f"vn{vi}",))
                    P.mark()
                    for t2 in range(2):
                        vi = (nb * 2 + t2) % 4
                        for gq in range(2):
                            mi = gq

                            def mm_mix(e, gq=gq, mi=mi, vi=vi):
                                ins = None
                                for g4 in range(4):
                                    gg = gq * 4 + g4
                                    ins = e.matmul(ps_mx[mi][:, 128 * g4:128 * (g4 + 1)], lhsT=vn[vi][:, 128 * gg:128 * (gg + 1)],
                                                   rhs=wsT[:, gg, :], start=True, stop=True)
                                return ins
                            P.op("pe", mm_mix, reads=(f"vn{vi}", "C:W"), writes=(f"b_mx{mi}",))
                            P.op("dve", lambda e, gq=gq, mi=mi: e.tensor_tensor(
                                out=mxb[mi][:, :], in0=ps_mx[mi][:, :], in1=bsbc[:, 512 * gq:512 * (gq + 1)], op=ALU.add),
                                reads=(f"b_mx{mi}", "C:W"), writes=(f"mxb{mi}",))
                            P.op("dve", lambda e, gq=gq, mi=mi, t2=t2: e.tensor_tensor(
                                out=yaT[bi][:, 4 * gq:4 * gq + 4, 128 * t2:128 * (t2 + 1)],
                                in0=mxb[mi][:, :].rearrange("p (g t) -> p g t", g=4),
                                in1=uT[bi][:, 4 * gq:4 * gq + 4, 128 * t2:128 * (t2 + 1)], op=ALU.mult),
                                reads=(f"mxb{mi}",) + tuple(f"uT{bi}.{4 * gq + q}" for q in range(4)),
                                writes=(f"yaT{bi}.{gq}.{t2}",))
                    yk = tuple(f"yaT{bi}.{gq}.{t2}" for gq in range(2) for t2 in range(2))
                    for j in range(8):
                        pj = j % 2

                        def mm_a(e, j=j, pj=pj):
                            ins = None
                            for c in range(8):
                                ins = e.matmul(ps_at[pj][:, 0:256], lhsT=w_a[:, c, 128 * j:128 * (j + 1)],
                                               rhs=yaT[bi][:, c, :], start=(c == 0), stop=(c == 7))
                            return ins
                        P.op("pe", mm_a, reads=yk + ("C:W",), writes=(f"b_at{pj}",))
                        if j % 2:
                            P.op("act", lambda e, j=j, pj=pj: e.copy(out=atsb[bi][:, j, :], in_=ps_at[pj][:, 0:256]),
                                 reads=(f"b_at{pj}",), writes=(f"atsb{bi}.{j}",))
                        else:
                            P.op("dve", lambda e, j=j, pj=pj: e.tensor_copy(out=atsb[bi][:, j, :], in_=ps_at[pj][:, 0:256]),
                                 reads=(f"b_at{pj}",), writes=(f"atsb{bi}.{j}",))
                    P.mark()
                    P.dma("sp", lambda e, nb=nb: e.dma_start(out=at_d[:, :, 256 * nb:256 * (nb + 1)], in_=atsb[bi][:, :, :]),
                          dsem(f"atsb{bi}"), reads=tuple(f"atsb{bi}.{j}" for j in range(8)), writes=())
                P.pipeline([P.split(P.capture(body2, nb_)) for nb_ in range(nblk)])
                P.drain_dma("sp")
                P.drain_dma("pool")
                P.emit(nc, "p2a")

        if nblk > 0:
            with ExitStack() as es:
                sb = lambda name, shape, dtp: es.enter_context(nc.sbuf_tensor("sb_" + name, shape, dtp))
                ps = lambda name, shape, dtp=F32: es.enter_context(nc.psum_tensor("pp_" + name, shape, dtp))
                w_g = sb("w_g", [128, 8, 2048], BF16)
                w_b = sb("w_b", [128, 4, 1024], BF16)
                w_o = sb("w_o", [128, 8, 1024], BF16)
                ident = sb("identb", [128, 128], BF16)
                gmix = sb("gmix3", [128, 8], F32)
                ones_bf = sb("ones3", [128, 128], BF16)
                xst = [sb(f"cxst{i}", [128, 8, 256], F32) for i in range(2)]
                sq = [sb(f"csq{i}", [128, 8, 256], BF16) for i in range(2)]
                rstd = [sb(f"crstd{i}", [128, 256], F32) for i in range(2)]
                xn = [sb(f"cxn{i}", [128, 8, 256], BF16) for i in range(2)]
                sg = [sb(f"sg{i}", [128, 16, 256], BF16) for i in range(2)]
                a3 = [sb(f"a3{i}", [128, 3, 520], F32) for i in range(2)]
                s2 = [sb(f"s2{i}", [128, 520], F32) for i in range(2)]
                rden = [sb(f"rden{i}", [128, 8, 1], F32) for i in range(2)]
                yb = [sb(f"yb{i}", [128, 512], BF16) for i in range(2)]
                ybT = [sb(f"ybT{i}", [128, 4, 256], BF16) for i in range(2)]
                atl = [sb(f"atl{i}", [128, 8, 256], F32) for i in range(2)]
                tmpa = [sb(f"tmpa{i}", [128, 256], F32) for i in range(2)]
                tmpb = [sb(f"tmpb{i}", [128, 256], F32) for i in range(2)]
                mT = [sb(f"mT{i}", [128, 8, 256], BF16) for i in range(2)]
                xtk = [sb(f"xtk{i}", [128, 1024], F32) for i in range(4)]
                x2 = [sb(f"x2{i}", [128, 1024], F32) for i in range(4)]
                ps_ss = ps("c_ss", [128, 512])
                ps_pr = [ps(f"c_pr{i}", [128, 512]) for i in range(2)]
                ps_T = ps("c_T", [128, 4, 128], BF16)
                ps_B = [ps(f"c_B{i}", [128, 512]) for i in range(2)]
                ps_o = [ps(f"c_o{i}", [128, 512]) for i in range(2)]

                load_cast(w_g[:, :, :], w_in_h[:, :, 4608:6656], piece=1024)
                load_cast(w_b[:, :, :], wb_h[:, :, :], piece=1024)
                load_cast(w_o[:, :, :], wo_h[:, :, :], piece=1024)
                load_cast(ident[:, :], ident_h, piece=128)
                P.dma("sp", lambda e: e.dma_start(out=gmix[:, :], in_=gmix_h), dsem("w"), writes=("C:W",))
                P.op("dve", lambda e: e.memset(ones_bf[:, :], 1.0), writes=("C:ones",))
                prn = [0]
                rtmp = [sb(f"crtmp{i}", [128, 256], F32) for i in range(2)]
                bufs3 = (xst, sq, rstd, xn, ps_ss, gmix, ones_bf, rtmp)

                def body3(nb):
                    bi = nb % 2
                    if nb == 0:
                        rmsnorm_block(bufs3, xseg[nb][:, :, 64:320], bi, 256, "c", rs_src=rstd_d[:, 256 * nb:256 * (nb + 1)])
                    if nb + 1 < nblk:
                        rmsnorm_block(bufs3, xseg[nb + 1][:, :, 64:320], (nb + 1) % 2, 256, "c",
                                      rs_src=rstd_d[:, 256 * (nb + 1):256 * (nb + 2)])
                    xk = rmsnorm_block(bufs3, None, bi, 256, "c", rs_loaded=True)
                    P.mark()
                    for j in range(16):
                        prn[0] += 1
                        pj = prn[0] % 2

                        def mm_g(e, j=j, pj=pj):
                            ins = None
                            for c in range(8):
                                ins = e.matmul(ps_pr[pj][:, 0:256], lhsT=w_g[:, c, 128 * j:128 * (j + 1)],
                                               rhs=xn[bi][:, c, :], start=(c == 0), stop=(c == 7))
                            return ins
                        P.op("pe", mm_g, reads=tuple(xk) + ("C:W",), writes=(f"c_pr{pj}",))
                        P.op("act", lambda e, j=j, pj=pj: e.activation(out=sg[bi][:, j, :], in_=ps_pr[pj][:, 0:256], func=AF.Sigmoid),
                             reads=(f"c_pr{pj}",), writes=(f"sg{bi}.{j}",))
                    P.dma("sp", lambda e, nb=nb: e.dma_start(out=atl[bi][:, :, :], in_=at_d[:, :, 256 * nb:256 * (nb + 1)]),
                          dsem(f"atl{bi}"), writes=(f"atl{bi}",))
                    for t2 in range(2):
                        ti = (nb * 2 + t2) % 2
                        t0 = 256 * nb + 128 * t2
                        P.dma("sp", lambda e, ti=ti, t0=t0: e.dma_start(
                            out=a3[ti][:, :, :], in_=att_d[:, t0:t0 + 128, :].rearrange("g t c -> t g c")),
                            dsem(f"a3{ti}"), writes=(f"a3{ti}",))
                        xi = (nb * 2 + t2) % 4
                        P.dma("sp", lambda e, xi=xi, t0=t0: e.dma_start(out=xtk[xi][:, :], in_=xtok_h[t0:t0 + 128, :]),
                              dsem(f"xtk{xi}"), writes=(f"xtk{xi}",))
                        P.op("dve", lambda e, ti=ti: e.tensor_tensor(out=s2[ti][:, :], in0=a3[ti][:, 0, :], in1=a3[ti][:, 1, :], op=ALU.add),
                             reads=(f"a3{ti}",), writes=(f"s2{ti}",))
                        P.op("dve", lambda e, ti=ti: e.tensor_tensor(out=s2[ti][:, :], in0=s2[ti][:, :], in1=a3[ti][:, 2, :], op=ALU.add),
                             reads=(f"a3{ti}", f"s2{ti}"), writes=(f"s2{ti}",))
                        P.op("dve", lambda e, ti=ti: e.reciprocal(
                            out=rden[ti][:, :, :], in_=s2[ti][:, :].rearrange("p (h e) -> p h e", h=8)[:, :, 64:65]),
                            reads=(f"s2{ti}",), writes=(f"rden{ti}",))
                        P.op("dve", lambda e, ti=ti: e.tensor_tensor(
                            out=yb[ti][:, :].rearrange("p (h e) -> p h e", h=8),
                            in0=s2[ti][:, :].rearrange("p (h e) -> p h e", h=8)[:, :, 0:64],
                            in1=rden[ti][:, :, :].to_broadcast([128, 8, 64]), op=ALU.mult),
                            reads=(f"s2{ti}", f"rden{ti}"), writes=(f"yb{ti}",))

                        def tr_y(e, ti=ti):
                            ins = None
                            for j in range(4):
                                ins = e.transpose(ps_T[:, j, :], yb[ti][:, 128 * j:128 * (j + 1)], ident[:, :])
                            return ins
                        P.op("pe", tr_y, reads=(f"yb{ti}", "C:W"), writes=("c_T",))
                        P.op("act", lambda e, t2=t2: e.copy(out=ybT[bi][:, :, 128 * t2:128 * (t2 + 1)], in_=ps_T[:, :, :]),
                             reads=("c_T",), writes=(f"ybT{bi}.{t2}",))
                    P.mark()
                    for j in range(8):
                        pj = j % 2

                        def mm_b(e, j=j, pj=pj):
                            ins = None
                            for c in range(4):
                                ins = e.matmul(ps_B[pj][:, 0:256], lhsT=w_b[:, c, 128 * j:128 * (j + 1)],
                                               rhs=ybT[bi][:, c, :], start=(c == 0), stop=(c == 3))
                            return ins
                        P.op("pe", mm_b, reads=(f"ybT{bi}.0", f"ybT{bi}.1", "C:W"), writes=(f"c_B{pj}",))
                        P.op("dve", lambda e, j=j, pj=pj: e.tensor_tensor(out=tmpa[pj][:, :], in0=atl[bi][:, j, :], in1=sg[bi][:, j, :], op=ALU.mult),
                             reads=(f"atl{bi}", f"sg{bi}.{j}"), writes=(f"tmpa{pj}",))
                        P.op("dve", lambda e, j=j, pj=pj: e.tensor_tensor(out=tmpb[pj][:, :], in0=ps_B[pj][:, 0:256], in1=sg[bi][:, 8 + j, :], op=ALU.mult),
                             reads=(f"c_B{pj}", f"sg{bi}.{8 + j}"), writes=(f"tmpb{pj}",))
                        P.op("dve", lambda e, j=j, pj=pj: e.tensor_tensor(out=mT[bi][:, j, :], in0=tmpa[pj][:, :], in1=tmpb[pj][:, :], op=ALU.add),
                             reads=(f"tmpa{pj}", f"tmpb{pj}"), writes=(f"mT{bi}.{j}",))
                    mk = tuple(f"mT{bi}.{j}" for j in range(8))
                    for t2 in range(2):
                        ti = (nb * 2 + t2) % 2
                        t0 = 256 * nb + 128 * t2
                        for hh in range(2):
                            def mm_o2(e, t2=t2, hh=hh):
                                ins = None
                                for c in range(8):
                                    ins = e.matmul(ps_o[hh][:, :], lhsT=mT[bi][:, c, 128 * t2:128 * (t2 + 1)],
                                                   rhs=w_o[:, c, 512 * hh:512 * (hh + 1)], start=(c == 0), stop=(c == 7))
                                return ins
                            P.op("pe", mm_o2, reads=mk + ("C:W",), writes=(f"c_o{hh}",))
                            xi = (nb * 2 + t2) % 4
                            P.op("dve", lambda e, hh=hh, xi=xi: e.tensor_tensor(
                                out=x2[xi][:, 512 * hh:512 * (hh + 1)], in0=ps_o[hh][:, :], in1=xtk[xi][:, 512 * hh:512 * (hh + 1)], op=ALU.add),
                                reads=(f"c_o{hh}", f"xtk{xi}"), writes=(f"x2{xi}.{hh}",))
                    P.mark()
                    for t2 in range(2):
                        xi = (nb * 2 + t2) % 4
                        t0 = 256 * nb + 128 * t2
                        P.dma("sp", lambda e, xi=xi, t0=t0: e.dma_start(out=x2_d[t0:t0 + 128, :], in_=x2[xi][:, :]),
                              dsem(f"x2{xi}"), reads=(f"x2{xi}.0", f"x2{xi}.1"), writes=())
                P.pipeline([P.split(P.capture(body3, nb_)) for nb_ in range(nblk)])
                P.drain_dma("sp")
                P.drain_dma("pool")
                P.emit(nc, "p2b")

        if ntile > 0:
            with ExitStack() as es:
                sb = lambda name, shape, dtp: es.enter_context(nc.sbuf_tensor("sb_" + name, shape, dtp))
                ps = lambda name, shape, dtp=F32: es.enter_context(nc.psum_tensor("pp_" + name, shape, dtp))
                NB = 15
                NDG = 4
                wq = sb("wq", [128, 8, 2048], BF16)
                skT = sb("skT", [128, 16, 128], BF16)
                ident = sb("identp", [128, 128], BF16)
                gffn = sb("gffn", [128, 1024], F32)
                gfin = sb("gfin", [128, 1024], F32)
                iota16 = sb("iota16", [128, 16], F32)
                thr16 = sb("thr16", [128, 16], F32)
                posf = sb("posf", [128, 8, 16], F32)
                x2t = [sb(f"x2t{i}", [128, 1024], F32) for i in range(3)]
                hn = [sb(f"hn{i}", [128, 1024], F32) for i in range(3)]
                hnb = sb("hnb", [128, 1024], BF16)
                hnT = sb("hnT", [128, 8, 128], BF16)
                qTp = sb("qTp", [128, 16, 128], BF16)
                junkr = [sb(f"junkp{i}", [128, 1024], BF16) for i in range(2)]
                jn = [0]

                def nj():
                    jn[0] += 1
                    return jn[0] % 2
                st1 = sb("st1", [128, 1], F32)
                S_sbs = [sb(f"S_sb{i}", [128, 16, 128], F32) for i in range(2)]
                S2 = sb("S2", [128, 16, 128], F32)
                tops = sb("top", [128, 16, 16], F32)
                idxu = sb("idxu", [128, 16, 16], U32)
                idxf = sb("idxf", [128, 16, 16], F32)
                cand = sb("cand", [128, 8, 256], F32)
                cand2 = S2[:, :, :].rearrange("p (h t) n -> p h (t n)", t=2)
                best = sb("best", [128, 8, 16], F32)
                posu = sb("posu", [128, 8, 16], U32)
                pa_u = sb("pa_u", [128, 8, 16], U32)
                pb_u = sb("pb_u", [128, 8, 16], U32)
                pa_f = sb("pa_f", [128, 8, 16], F32)
                pb_f = sb("pb_f", [128, 8, 16], F32)
                eq = sb("eq", [128, 128, 16], F32)
                i1s = sb("i1s", [128, 128], F32)
                i2s = sb("i2s", [128, 128], F32)
                expf = sb("expf", [128, 128], F32)
                expu = [sb(f"expu{i}", [128, 128], U32) for i in range(2)]
                gate = [sb(f"gate{i}", [128, 8, 16], F32) for i in range(2)]
                gsum = sb("gsum", [128, 8, 1], F32)
                aval = sb("aval", [128, 128], F32)
                gval = sb("gval", [128, 128], F32)
                wval = sb("wval", [128, 128], F32)
                gbuf = [sb(f"gbuf{i}", [128, 2048], BF16) for i in range(NB)]
                dg = [sb(f"dg{i}", [128, 128], BF16) for i in range(NDG)]
                ident_f = sb("ident_f", [128, 128], F32)
                st2 = sb("st2", [128, 1], F32)
                x3 = sb("x3", [128, 1024], F32)
                yo = [sb(f"yo{i}", [128, 1024], F32) for i in range(2)]
                ps_T = ps("p_T", [128, 8, 128], BF16)
                ps_q = [ps(f"p_q{i}", [128, 512]) for i in range(2)]
                ps_Sc = [ps(f"p_S{i}", [128, 512]) for i in range(2)]
                ps_acc = [ps(f"p_acc{i}", [128, 512]) for i in range(2)]

                load_cast(wq[:, :, :], wq_h[:, :, :], piece=1024)
                load_cast(skT[:, :, :], skT_h[:, :, :], piece=128)
                load_cast(ident[:, :], ident_h, piece=128)
                for dst_, src_ in ((gffn[:, :], gffn_h), (gfin[:, :], gfin_h), (iota16[:, :], iota_h), (thr16[:, :], thr_h),
                                   (ident_f[:, :], ident_h)):
                    P.dma("sp", lambda e, d_=dst_, s_=src_: e.dma_start(out=d_, in_=s_), dsem("w"), writes=("C:W",))

                def rms_rstd(src, dst1, tag):
                    P.op("act", lambda e: e.activation(out=hnb[:, :], in_=src, func=AF.Square, accum_out=dst1),
                         reads=(tag,), writes=(tag + "r", "hnb"))
                    P.op("dve", lambda e: e.tensor_scalar(out=dst1, in0=dst1, scalar1=1.0 / D, scalar2=EPS, op0=ALU.mult, op1=ALU.add),
                         reads=(tag + "r",), writes=(tag + "r",))
                    P.op("act", lambda e: e.activation(out=dst1, in_=dst1, func=AF.Ln), reads=(tag + "r",), writes=(tag + "r",))
                    P.op("act", lambda e: e.activation(out=dst1, in_=dst1, func=AF.Exp, scale=-0.5), reads=(tag + "r",), writes=(tag + "r",))

                gcount = [0]
                def load_x2(tI):
                    P.dma("sp", lambda e, ti=tI % 3, t0=128 * tI: e.dma_start(out=x2t[ti][:, :], in_=x2_d[t0:t0 + 128, :]),
                          dsem(f"x2t{tI % 3}"), writes=(f"x2t{tI % 3}",))
                def body4a(tI):
                    ti = tI % 2
                    t3 = tI % 3
                    S_sb = S_sbs[tI % 2]
                    sk_ = f"S_sb{tI % 2}"
                    t0 = 128 * tI
                    load_x2(tI)
                    rms_rstd(x2t[t3][:, :], st1[:, :], f"x2t{t3}")
                    P.op("dve", lambda e: e.scalar_tensor_tensor(out=hn[t3][:, :], in0=x2t[t3][:, :], scalar=st1[:, 0:1], in1=gffn[:, :],
                                                                 op0=ALU.mult, op1=ALU.mult),
                         reads=(f"x2t{t3}", f"x2t{t3}r", "C:W"), writes=(f"hn{t3}",))
                    P.op("act", lambda e: e.copy(out=hnb[:, :], in_=hn[t3][:, :]), reads=(f"hn{t3}",), writes=("hnb",))

                    def tr_h(e):
                        ins = None
                        for c in range(8):
                            ins = e.transpose(ps_T[:, c, :], hnb[:, 128 * c:128 * (c + 1)], ident[:, :])
                        return ins
                    P.op("pe", tr_h, reads=("hnb", "C:W"), writes=("p_T",))
                    P.op("act", lambda e: e.copy(out=hnT[:, :, :], in_=ps_T[:, :, :]), reads=("p_T",), writes=("hnT",))
                    for qg in range(4):
                        pj = qg % 2

                        def mm_pq(e, qg=qg, pj=pj):
                            ins = None
                            for q4 in range(4):
                                hp = qg * 4 + q4
                                for c in range(8):
                                    ins = e.matmul(ps_q[pj][:, 128 * q4:128 * (q4 + 1)], lhsT=wq[:, c, 128 * hp:128 * (hp + 1)],
                                                   rhs=hnT[:, c, :], start=(c == 0), stop=(c == 7))
                            return ins
                        P.op("pe", mm_pq, reads=("hnT", "C:W"), writes=(f"p_q{pj}",))
                        P.op("act", lambda e, qg=qg, pj=pj: e.copy(out=qTp[:, 4 * qg:4 * qg + 4, :],
                                                                  in_=ps_q[pj][:, :].rearrange("p (a b) -> p a b", a=4)),
                             reads=(f"p_q{pj}",), writes=(f"qTp.{qg}",))
                    for qg in range(4):
                        def mm_ps(e, qg=qg):
                            ins = None
                            for q4 in range(4):
                                hp = qg * 4 + q4
                                ins = e.matmul(ps_Sc[qg % 2][:, 128 * q4:128 * (q4 + 1)], lhsT=qTp[:, hp, :], rhs=skT[:, hp, :],
                                               start=True, stop=True)
                            return ins
                        P.op("pe", mm_ps, reads=(f"qTp.{qg}", "C:W"), writes=(f"p_S{qg % 2}",))
                        P.op("act", lambda e, qg=qg: e.copy(out=S_sb[:, 4 * qg:4 * qg + 4, :],
                                                           in_=ps_Sc[qg % 2][:, :].rearrange("p (a b) -> p a b", a=4)),
                             reads=(f"p_S{qg % 2}",), writes=(f"{sk_}.{qg}",))
                    P.mark()
                    for hp in range(16):
                        kq = f"{sk_}.{hp // 4}"
                        P.op("dve", lambda e, hp=hp: e.max(out=tops[:, hp, 0:8], in_=S_sb[:, hp, :]), reads=(kq,), writes=(f"top{hp}a",))
                        P.op("dve", lambda e, hp=hp: e.match_replace(out=S2[:, hp, :], in_to_replace=tops[:, hp, 0:8], in_values=S_sb[:, hp, :],
                                                                    imm_value=-1e30),
                             reads=(kq, f"top{hp}a"), writes=(f"S2.{hp}",))
                        P.op("dve", lambda e, hp=hp: e.max(out=tops[:, hp, 8:16], in_=S2[:, hp, :]), reads=(f"S2.{hp}",), writes=(f"top{hp}b",))
                        P.op("dve", lambda e, hp=hp: e.max_index(out=idxu[:, hp, 0:8], in_max=tops[:, hp, 0:8], in_values=S_sb[:, hp, :]),
                             reads=(kq, f"top{hp}a"), writes=(f"idx{hp}a",))
                        P.op("dve", lambda e, hp=hp: e.max_index(out=idxu[:, hp, 8:16], in_max=tops[:, hp, 8:16], in_values=S_sb[:, hp, :]),
                             reads=(kq, f"top{hp}b"), writes=(f"idx{hp}b",))
                    allidx = tuple(f"idx{hp}{x}" for hp in range(16) for x in "ab")
                    alltop = tuple(f"top{hp}{x}" for hp in range(16) for x in "ab")
                    P.op("dve", lambda e: e.tensor_copy(out=idxf[:, :, :], in_=idxu[:, :, :]), reads=allidx, writes=("idxf",))
                    topv = tops[:, :, :].rearrange("p (h t) k -> p h t k", t=2)
                    P.op("dve", lambda e: e.tensor_tensor(
                        out=cand[:, :, :].rearrange("p h (a b) -> p h a b", a=16),
                        in0=topv[:, :, 0, :].unsqueeze(3).to_broadcast([128, 8, 16, 16]),
                        in1=topv[:, :, 1, :].unsqueeze(2).to_broadcast([128, 8, 16, 16]), op=ALU.add),
                        reads=alltop, writes=("cand",))
                    for h in range(8):
                        P.op("dve", lambda e, h=h: e.max(out=best[:, h, 0:8], in_=cand[:, h, :]), reads=("cand",), writes=(f"best{h}a",))
                        P.op("dve", lambda e, h=h: e.match_replace(out=cand2[:, h, :], in_to_replace=best[:, h, 0:8], in_values=cand[:, h, :],
                                                                  imm_value=-1e30),
                             reads=("cand", f"best{h}a"), writes=(f"cand2.{h}", f"S2.{2 * h}", f"S2.{2 * h + 1}"))
                        P.op("dve", lambda e, h=h: e.max(out=best[:, h, 8:16], in_=cand2[:, h, :]),
                             reads=(f"cand2.{h}", f"S2.{2 * h}", f"S2.{2 * h + 1}"), writes=(f"best{h}b",))
                        P.op("dve", lambda e, h=h: e.max_index(out=posu[:, h, 0:8], in_max=best[:, h, 0:8], in_values=cand[:, h, :]),
                             reads=("cand", f"best{h}a"), writes=(f"pos{h}a",))
                        P.op("dve", lambda e, h=h: e.max_index(out=posu[:, h, 8:16], in_max=best[:, h, 8:16], in_values=cand[:, h, :]),
                             reads=("cand", f"best{h}b"), writes=(f"pos{h}b",))
                    allpos = tuple(f"pos{h}{x}" for h in range(8) for x in "ab")
                    allbest = tuple(f"best{h}{x}" for h in range(8) for x in "ab")
                    gi = tI % 2
                    P.op("dve", lambda e, gi=gi: e.tensor_tensor(out=gate[gi][:, :, :], in0=best[:, :, :],
                                                                in1=best[:, :, 0:1].to_broadcast([128, 8, 16]), op=ALU.subtract),
                         reads=allbest, writes=(f"gate{gi}",))
                    P.op("act", lambda e, gi=gi: e.activation(out=gate[gi][:, :, :], in_=gate[gi][:, :, :], func=AF.Exp),
                         reads=(f"gate{gi}",), writes=(f"gate{gi}",))
                    P.op("dve", lambda e, gi=gi: e.tensor_reduce(out=gsum[:, :, :], in_=gate[gi][:, :, :], axis=AX.X, op=ALU.add),
                         reads=(f"gate{gi}",), writes=("gsum",))
                    P.op("dve", lambda e: e.reciprocal(out=gsum[:, :, :], in_=gsum[:, :, :]), reads=("gsum",), writes=("gsum",))
                    P.op("dve", lambda e, gi=gi: e.tensor_tensor(out=gate[gi][:, :, :], in0=gate[gi][:, :, :],
                                                                in1=gsum[:, :, :].to_broadcast([128, 8, 16]), op=ALU.mult),
                         reads=(f"gate{gi}", "gsum"), writes=(f"gate{gi}",))
                    P.op("dve", lambda e: e.tensor_copy(out=posf[:, :, :], in_=posu[:, :, :]), reads=allpos, writes=("posf",))
                    P.op("dve", lambda e: e.tensor_tensor(
                        out=eq[:, :, :], in0=posf[:, :, :].rearrange("p h k -> p (h k)").unsqueeze(2).to_broadcast([128, 128, 16]),
                        in1=thr16[:, :].unsqueeze(1).to_broadcast([128, 128, 16]), op=ALU.is_ge),
                        reads=("posf", "C:W"), writes=("eq",))
                    P.op("dve", lambda e: e.tensor_reduce(out=pa_f[:, :, :].rearrange("p h k -> p (h k)"), in_=eq[:, :, :], axis=AX.X, op=ALU.add),
                         reads=("eq",), writes=("pa_f",))
                    P.op("dve", lambda e: e.scalar_tensor_tensor(out=pb_f[:, :, :], in0=pa_f[:, :, :], scalar=-16.0, in1=posf[:, :, :],
                                                                 op0=ALU.mult, op1=ALU.add),
                         reads=("pa_f", "posf"), writes=("pb_f",))
                    idxv = idxf[:, :, :].rearrange("p (h t) k -> p h t k", t=2)
                    for (pf, half, dstk, dst) in ((pa_f, 0, "i1s", i1s), (pb_f, 1, "i2s", i2s)):
                        pk = "pa_f" if half == 0 else "pb_f"
                        P.op("dve", lambda e, pf=pf: e.tensor_tensor(
                            out=eq[:, :, :], in0=pf[:, :, :].rearrange("p h k -> p (h k)").unsqueeze(2).to_broadcast([128, 128, 16]),
                            in1=iota16[:, :].unsqueeze(1).to_broadcast([128, 128, 16]), op=ALU.is_equal),
                            reads=(pk, "C:W"), writes=("eq",))
                        P.op("dve", lambda e, half=half: e.tensor_tensor(
                            out=eq[:, :, :].rearrange("p (h k) a -> p h k a", h=8),
                            in0=eq[:, :, :].rearrange("p (h k) a -> p h k a", h=8),
                            in1=idxv[:, :, half, :].unsqueeze(2).to_broadcast([128, 8, 16, 16]), op=ALU.mult),
                            reads=("eq", "idxf"), writes=("eq",))
                        P.op("dve", lambda e, dst=dst: e.tensor_reduce(out=dst[:, :], in_=eq[:, :, :], axis=AX.X, op=ALU.add),
                             reads=("eq",), writes=(dstk,))
                    P.op("dve", lambda e: e.scalar_tensor_tensor(out=expf[:, :], in0=i1s[:, :], scalar=128.0, in1=i2s[:, :], op0=ALU.mult, op1=ALU.add),
                         reads=("i1s", "i2s"), writes=("expf",))
                    P.op("dve", lambda e, gi=gi: e.tensor_copy(out=expu[gi][:, :], in_=expf[:, :]), reads=("expf",), writes=(f"expu{gi}",))

                def body4b(tI):
                    ti = tI % 2
                    gi = tI % 2
                    t3 = tI % 3
                    t0 = 128 * tI
                    for k in range(nslot):
                        gcount[0] += 1
                        b = gcount[0] % NB
                        db = gcount[0] % NDG
                        P.dma("pool", lambda e, b=b, k=k, gi=gi: e.indirect_dma_start(
                            out=gbuf[b][:, :], out_offset=None, in_=uv16_d,
                            in_offset=bass.IndirectOffsetOnAxis(ap=expu[gi][:, k:k + 1], axis=0)),
                            dsem(f"gb{b}", "pool"), reads=(f"expu{gi}",), writes=(f"gbuf{b}",))
                        ji = nj()
                        P.op("dve", lambda e, b=b, k=k, t3=t3, ji=ji: e.scalar_tensor_tensor(
                            out=junkr[ji][:, :], in0=gbuf[b][:, 0:1024], scalar=1.0, in1=hn[t3][:, :], op0=ALU.mult, op1=ALU.mult,
                            accum_out=aval[:, k:k + 1]),
                            reads=(f"gbuf{b}", f"hn{t3}"), writes=(f"av{k}", f"junk{ji}"))
                        P.op("act", lambda e, k=k: e.activation(out=gval[:, k:k + 1], in_=aval[:, k:k + 1], func=AF.Gelu),
                             reads=(f"av{k}",), writes=(f"gv{k}",))
                        P.op("act", lambda e, k=k, gi=gi: e.mul(out=wval[:, k:k + 1], in_=gval[:, k:k + 1],
                                                               mul=gate[gi][:, :, :].rearrange("p h k -> p (h k)")[:, k:k + 1]),
                             reads=(f"gv{k}", f"gate{gi}"), writes=(f"wv{k}",))
                        P.op("act", lambda e, k=k, db=db: e.activation(out=dg[db][:, :], in_=ident_f[:, :], func=AF.Copy,
                                                                      scale=wval[:, k:k + 1]),
                             reads=(f"wv{k}", "C:W"), writes=(f"dg{db}",))

                        def mm_acc(e, k=k, b=b, db=db):
                            ins = None
                            for hh in range(2):
                                ins = e.matmul(ps_acc[hh][:, :], lhsT=dg[db][:, :], rhs=gbuf[b][:, 1024 + 512 * hh:1024 + 512 * (hh + 1)],
                                               start=(k == 0), stop=(k == nslot - 1))
                            return ins
                        P.op("pe", mm_acc, reads=(f"dg{db}", f"gbuf{b}"), writes=("p_acc",))
                    for hh in range(2):
                        P.op("dve", lambda e, t3=t3, hh=hh: e.tensor_tensor(out=x3[:, 512 * hh:512 * (hh + 1)], in0=ps_acc[hh][:, :],
                                                                           in1=x2t[t3][:, 512 * hh:512 * (hh + 1)], op=ALU.add),
                             reads=(f"x2t{t3}", "p_acc"), writes=(f"x3.{hh}",))
                    P.op("act", lambda e, ti=ti: e.activation(out=yo[ti][:, :], in_=x3[:, :], func=AF.Square, accum_out=st2[:, :]),
                         reads=("x3.0", "x3.1"), writes=("st2", f"yo{ti}"))
                    P.op("dve", lambda e: e.tensor_scalar(out=st2[:, :], in0=st2[:, :], scalar1=1.0 / D, scalar2=EPS, op0=ALU.mult, op1=ALU.add),
                         reads=("st2",), writes=("st2",))
                    P.op("act", lambda e: e.activation(out=st2[:, :], in_=st2[:, :], func=AF.Ln), reads=("st2",), writes=("st2",))
                    P.op("act", lambda e: e.activation(out=st2[:, :], in_=st2[:, :], func=AF.Exp, scale=-0.5), reads=("st2",), writes=("st2",))
                    P.op("dve", lambda e, ti=ti: e.scalar_tensor_tensor(out=yo[ti][:, :], in0=x3[:, :], scalar=st2[:, 0:1], in1=gfin[:, :],
                                                                       op0=ALU.mult, op1=ALU.mult),
                         reads=("x3.0", "x3.1", "st2", "C:W"), writes=(f"yo{ti}",))
                    P.dma("sp", lambda e, ti=ti, t0=t0: e.dma_start(out=y_h[t0:t0 + 128, :], in_=yo[ti][:, :]),
                          dsem(f"yo{ti}"), reads=(f"yo{ti}",), writes=())

                recA = [P.split(P.capture(body4a, t_)) for t_ in range(ntile)]
                recB = [P.capture(body4b, t_) for t_ in range(ntile)]
                P.pipeline([recA[t_] + [recB[t_]] for t_ in range(ntile)], spans=[0.9, 0.9, 1.0])
                P.drain_dma("sp")
                P.drain_dma("pool")
                P.emit(nc, "p3")
    return nc


def _seg_tokens():
    out = np.zeros((NSEG, 384), np.int64)
    kk = np.arange(384)
    for s in range(NSEG):
        g, sp_ = s // 16, s % 16
        d = DIL[g]
        nsub = 16 // d
        r, sub = sp_ // nsub, sp_ % nsub
        m = 256 * sub - 64 + kk
        out[s] = d * m + r
    return out


def prepare_inputs(inp):
    f = lambda a: np.ascontiguousarray(np.asarray(a, dtype=np.float32))
    x = f(inp["x"])
    lay = lambda w, nch: np.ascontiguousarray(w.reshape(nch, 128, -1).transpose(1, 0, 2))
    shared = {
        "w_in_l": lay(f(inp["w_in"])[0], 8),
        "gmix": np.ascontiguousarray(f(inp["norm_mix_g"])[0].reshape(8, 128).T),
        "gsgu_bc": np.ascontiguousarray(np.broadcast_to(f(inp["sgu_norm_g"])[0][None, :], (128, 1024))),
        "wsT": np.ascontiguousarray(f(inp["sgu_w"])[0].transpose(2, 0, 1)),
        "bsgu_bc": np.ascontiguousarray(np.broadcast_to(f(inp["sgu_b"])[0].reshape(1, 1024), (128, 1024))),
        "wa_l": lay(f(inp["w_branch_a"])[0], 8),
        "wb_l": lay(f(inp["w_branch_b"])[0], 4),
        "wo_l": lay(f(inp["w_out"])[0], 8),
        "gffn_bc": np.ascontiguousarray(np.broadcast_to(f(inp["norm_ffn_g"])[0][None, :], (128, 1024))),
        "gfin_bc": np.ascontiguousarray(np.broadcast_to(f(inp["norm_final_g"])[None, :], (128, 1024))),
        "wq_l": lay(f(inp["peer_wq"])[0], 8),
        "skT": np.ascontiguousarray(f(inp["peer_subkeys"])[0].reshape(16, 128, 128).transpose(2, 0, 1)),
        "uv_tab": np.ascontiguousarray(np.concatenate([f(inp["peer_u"])[0], f(inp["peer_v"])[0]], axis=1)),
        "ident": np.eye(128, dtype=np.float32),
        "iota16": np.ascontiguousarray(np.broadcast_to(np.arange(16, dtype=np.float32)[None, :], (128, 16))),
        "thr16": np.ascontiguousarray(np.broadcast_to((16.0 * np.arange(1, 17, dtype=np.float32))[None, :], (128, 16))),
    }
    jl = np.arange(128)[:, None]
    il = np.arange(128)[None, :]
    relA = jl - il - 64
    relB = jl - il + 64
    A = np.where(np.abs(relA) <= 64, -np.abs(relA), -1e9).astype(np.float32)
    Bm = np.where(np.abs(relB) <= 64, -np.abs(relB), -1e9).astype(np.float32)
    shared["negabs"] = np.ascontiguousarray(np.concatenate([A, Bm, A, Bm], axis=1))
    segtok = _seg_tokens()
    maps = []
    for c in range(8):
        b, hf = c // 2, c % 2
        T0 = hf * NTOK
        gtok = segtok + T0
        valid = (gtok >= 0) & (gtok < SEQ)
        xg = x[b][np.clip(gtok, 0, SEQ - 1)]
        xg = xg * valid[..., None].astype(np.float32) if False else np.where(valid[..., None], xg, np.float32(0))
        xs = np.ascontiguousarray(xg.reshape(NSEG, 384, 8, 128).transpose(0, 3, 2, 1))
        kv = valid.reshape(NSEG, 3, 128).transpose(2, 0, 1).reshape(128, NSEG * 3).astype(np.float32)
        m = dict(shared)
        m["xseg"] = xs
        m["kval"] = np.ascontiguousarray(kv)
        m["xtok"] = np.ascontiguousarray(x[b, T0:T0 + NTOK])
        maps.append(m)
    return maps


def kernel(**inputs):
    maps = prepare_inputs(inputs)
    nc = build_program()
    res = run_bass_kernel_spmd(nc, maps, core_ids=list(range(8)))
    out = np.zeros((4, SEQ, D), np.float32)
    for c in range(8):
        b, hf = c // 2, c % 2
        out[b, hf * NTOK:(hf + 1) * NTOK] = res.results[c]["y"]
    return out
```
